# Optimizing a Trainium2 kernel written in Bass

```python
import math
import jax
import jax.numpy as jnp
from jax import lax
import numpy as np

D_MODEL = 1024
BATCH = 8
SEQ = 4096
DEPTH = 4

GRID_W = 64
CTX_LEN = 256
N_MIXERS = 4
EPS = 1e-6
ROPE_BASE = 10000.0
Q_BLOCK = 128
NEG_INF = -1e30

NA_HEAD_DIM = 64
NA_HEADS = D_MODEL // NA_HEAD_DIM
NA_KH = 8
NA_KW = 16

SW_HEAD_DIM = 64
SW_HEADS = D_MODEL // SW_HEAD_DIM
SW_KV_HEADS = 4
SW_WINDOW = 128

MLA_HEADS = 16
MLA_Q_RANK = 384
MLA_KV_RANK = 256
MLA_NOPE = 64
MLA_ROPE = 32
MLA_V = 64

DIFF_HEAD_DIM = 64
DIFF_HEADS = D_MODEL // (2 * DIFF_HEAD_DIM)

N_GROUPS = 4
EXPERTS_PER_GROUP = 8
N_EXPERTS = N_GROUPS * EXPERTS_PER_GROUP
TOP_K = 2
D_EXPERT = 256
MOE_BLOCK = 128

kernel_name = 'hybrid_dit_na_swa_mla_diff_hmoe'


def _layers_of_kind(kind):
    return len(range(kind, DEPTH, N_MIXERS))


def rms_norm(x, g):
    xf = x.astype(jnp.float32)
    xf = xf * lax.rsqrt(jnp.mean(xf * xf, axis=-1, keepdims=True) + EPS)
    return (xf * g.astype(jnp.float32)).astype(x.dtype)


def modulate(x, g, shift, scale):
    return rms_norm(x, g) * (1.0 + scale) + shift


def split_heads(t, n_heads):
    b, n, _ = t.shape
    return t.reshape(b, n, n_heads, -1).transpose(0, 2, 1, 3)


def merge_heads(t):
    b, h, n, d = t.shape
    return t.transpose(0, 2, 1, 3).reshape(b, n, h * d)


def axial_rope(n_tokens, rot_dim):
    t = jnp.arange(n_tokens, dtype=jnp.int32)
    n_freq = rot_dim // 4
    inv_freq = ROPE_BASE ** (-jnp.arange(n_freq, dtype=jnp.float32) / n_freq)
    rows = (t // GRID_W).astype(jnp.float32)
    cols = (t % GRID_W).astype(jnp.float32)
    ang = jnp.concatenate([rows[:, None] * inv_freq, cols[:, None] * inv_freq], axis=-1)
    return jnp.cos(ang), jnp.sin(ang)


def apply_rope(x, cos, sin):
    x1, x2 = jnp.split(x.astype(jnp.float32), 2, axis=-1)
    return jnp.concatenate([x1 * cos - x2 * sin, x2 * cos + x1 * sin], axis=-1).astype(x.dtype)


def softmax_f32(logits, sink=None):
    if sink is None:
        return jax.nn.softmax(logits, axis=-1)
    sink_col = jnp.broadcast_to(sink, logits.shape[:-1] + (1,))
    return jax.nn.softmax(jnp.concatenate([logits, sink_col], axis=-1), axis=-1)[..., :-1]


def context_attention(q, k, v, scale, sink=None):
    b, h, n, dq = q.shape
    hk = k.shape[1]
    g = h // hk
    qg = q.reshape(b, hk, g, n, dq)
    logits = jnp.einsum('bkgqd,bkcd->bkgqc', qg, k).astype(jnp.float32) * scale
    sink_g = None if sink is None else sink.reshape(hk, g)[None, :, :, None, None].astype(jnp.float32)
    p = softmax_f32(logits, sink_g).astype(v.dtype)
    return jnp.einsum('bkgqc,bkcd->bkgqd', p, v).reshape(b, h, n, -1)


def dense_latent_attention(q, k, v, k_ctx, v_ctx, scale):
    b, h, s, dq = q.shape
    dv = v.shape[-1]
    nblk = s // Q_BLOCK
    qb = jnp.moveaxis(q.reshape(b, h, nblk, Q_BLOCK, dq), 2, 0)

    def one_block(qi):
        logits = jnp.concatenate([
            jnp.einsum('bhqd,bhkd->bhqk', qi, k),
            jnp.einsum('bhqd,bhcd->bhqc', qi, k_ctx)], axis=-1).astype(jnp.float32) * scale
        p = jax.nn.softmax(logits, axis=-1).astype(v.dtype)
        return (jnp.einsum('bhqk,bhkd->bhqd', p[..., :s], v)
                + jnp.einsum('bhqc,bhcd->bhqd', p[..., s:], v_ctx))

    o = lax.map(one_block, qb)
    return jnp.moveaxis(o, 0, 2).reshape(b, h, s, dv)


def window_latent_attention(q, k, v, k_ctx, v_ctx, sink, scale):
    b, h, s, d = q.shape
    hk = k.shape[1]
    g = h // hk
    nblk = s // Q_BLOCK
    span = Q_BLOCK + 2 * SW_WINDOW
    pad = ((0, 0), (0, 0), (SW_WINDOW, SW_WINDOW), (0, 0))
    kp, vp = jnp.pad(k, pad), jnp.pad(v, pad)
    qb = jnp.moveaxis(q.reshape(b, hk, g, nblk, Q_BLOCK, d), 3, 0)
    q_off = jnp.arange(Q_BLOCK)
    k_off = jnp.arange(span) - SW_WINDOW
    in_window = jnp.abs(k_off[None, :] - q_off[:, None]) <= SW_WINDOW
    sink_g = sink.reshape(hk, g)[None, :, :, None, None].astype(jnp.float32)

    def one_block(args):
        i, qi = args
        start = i * Q_BLOCK
        kb = lax.dynamic_slice_in_dim(kp, start, span, axis=2)
        vb = lax.dynamic_slice_in_dim(vp, start, span, axis=2)
        k_pos = start + k_off
        valid = in_window & ((k_pos >= 0) & (k_pos < s))[None, :]
        s_loc = jnp.einsum('bkgqd,bkcd->bkgqc', qi, kb).astype(jnp.float32) * scale
        s_loc = jnp.where(valid, s_loc, NEG_INF)
        s_ctx = jnp.einsum('bkgqd,bkcd->bkgqc', qi, k_ctx).astype(jnp.float32) * scale
        p = softmax_f32(jnp.concatenate([s_loc, s_ctx], axis=-1), sink_g).astype(v.dtype)
        return (jnp.einsum('bkgqc,bkcd->bkgqd', p[..., :span], vb)
                + jnp.einsum('bkgqc,bkcd->bkgqd', p[..., span:], v_ctx))

    o = lax.map(one_block, (jnp.arange(nblk), qb))
    return jnp.moveaxis(o, 0, 3).reshape(b, h, s, d)


def neighbourhood_latent_attention(q, k, v, k_ctx, v_ctx, rpb, scale):
    b, h, s, d = q.shape
    rows = s // GRID_W
    kh = min(NA_KH, rows)
    n_nb = kh * NA_KW
    q_col = jnp.arange(GRID_W)
    col0 = jnp.clip(q_col - NA_KW // 2, 0, GRID_W - NA_KW)
    key_col = col0[:, None] + jnp.arange(NA_KW)[None, :]
    col_off = key_col - q_col[:, None] + (NA_KW - 1)
    q_rows = jnp.moveaxis(q.reshape(b, h, rows, GRID_W, d), 2, 0)

    def one_row(args):
        r, qi = args
        row0 = jnp.clip(r - kh // 2, 0, rows - kh)
        key_row = row0 + jnp.arange(kh)
        row_off = key_row - r + (NA_KH - 1)
        key_idx = (key_row[None, :, None] * GRID_W + key_col[:, None, :]).reshape(GRID_W, n_nb)
        bias_idx = (row_off[None, :, None] * (2 * NA_KW - 1) + col_off[:, None, :]).reshape(GRID_W, n_nb)
        kg, vg = k[:, :, key_idx], v[:, :, key_idx]
        s_nb = (jnp.einsum('bhqd,bhqkd->bhqk', qi, kg).astype(jnp.float32) * scale
                + rpb[:, bias_idx].astype(jnp.float32)[None])
        s_ctx = jnp.einsum('bhqd,bhcd->bhqc', qi, k_ctx).astype(jnp.float32) * scale
        p = jax.nn.softmax(jnp.concatenate([s_nb, s_ctx], axis=-1), axis=-1).astype(v.dtype)
        return (jnp.einsum('bhqk,bhqkd->bhqd', p[..., :n_nb], vg)
                + jnp.einsum('bhqc,bhcd->bhqd', p[..., n_nb:], v_ctx))

    o = lax.map(one_row, (jnp.arange(rows), q_rows))
    return jnp.moveaxis(o, 0, 2).reshape(b, h, s, d)


def mixer_neighbourhood(h_lat, h_ctx, w_qkv, w_o, q_norm, k_norm, rpb, need_ctx):
    def project(h):
        q, k, v = jnp.split(h @ w_qkv, 3, axis=-1)
        return (rms_norm(split_heads(q, NA_HEADS), q_norm),
                rms_norm(split_heads(k, NA_HEADS), k_norm),
                split_heads(v, NA_HEADS))
    scale = NA_HEAD_DIM ** -0.5
    q, k, v = project(h_lat)
    qc, kc, vc = project(h_ctx)
    y_lat = merge_heads(neighbourhood_latent_attention(q, k, v, kc, vc, rpb, scale)) @ w_o
    y_ctx = merge_heads(context_attention(qc, kc, vc, scale)) @ w_o if need_ctx else None
    return y_lat, y_ctx


def mixer_window(h_lat, h_ctx, w_qkv, w_o, q_norm, k_norm, sink, rope, need_ctx):
    split_at = [SW_HEADS * SW_HEAD_DIM, (SW_HEADS + SW_KV_HEADS) * SW_HEAD_DIM]
    def project(h, rope_tab):
        q, k, v = jnp.split(h @ w_qkv, split_at, axis=-1)
        q = rms_norm(split_heads(q, SW_HEADS), q_norm)
        k = rms_norm(split_heads(k, SW_KV_HEADS), k_norm)
        if rope_tab is not None:
            q, k = apply_rope(q, *rope_tab), apply_rope(k, *rope_tab)
        return q, k, split_heads(v, SW_KV_HEADS)
    scale = SW_HEAD_DIM ** -0.5
    q, k, v = project(h_lat, rope)
    qc, kc, vc = project(h_ctx, None)
    y_lat = merge_heads(window_latent_attention(q, k, v, kc, vc, sink, scale)) @ w_o
    y_ctx = merge_heads(context_attention(qc, kc, vc, scale, sink)) @ w_o if need_ctx else None
    return y_lat, y_ctx


def mixer_mla(h_lat, h_ctx, w_dqkv, q_a_norm, kv_a_norm, w_uq, w_ukv, q_norm, k_norm, w_o, rope, need_ctx):
    def project(h, rope_tab):
        cq, ckv, k_rope = jnp.split(h @ w_dqkv, [MLA_Q_RANK, MLA_Q_RANK + MLA_KV_RANK], axis=-1)
        q = split_heads(rms_norm(cq, q_a_norm) @ w_uq, MLA_HEADS)
        kv = split_heads(rms_norm(ckv, kv_a_norm) @ w_ukv, MLA_HEADS)
        q_nope = rms_norm(q[..., :MLA_NOPE], q_norm[:MLA_NOPE])
        q_rope = rms_norm(q[..., MLA_NOPE:], q_norm[MLA_NOPE:])
        k_nope = rms_norm(kv[..., :MLA_NOPE], k_norm[:MLA_NOPE])
        v = kv[..., MLA_NOPE:]
        k_rope = rms_norm(k_rope, k_norm[MLA_NOPE:])[:, None]
        if rope_tab is not None:
            q_rope, k_rope = apply_rope(q_rope, *rope_tab), apply_rope(k_rope, *rope_tab)
        q = jnp.concatenate([q_nope, q_rope], axis=-1)
        k = jnp.concatenate([k_nope, jnp.broadcast_to(k_rope, k_nope.shape[:-1] + (MLA_ROPE,))], axis=-1)
        return q, k, v
    scale = (MLA_NOPE + MLA_ROPE) ** -0.5
    q, k, v = project(h_lat, rope)
    qc, kc, vc = project(h_ctx, None)
    y_lat = merge_heads(dense_latent_attention(q, k, v, kc, vc, scale)) @ w_o
    y_ctx = merge_heads(context_attention(qc, kc, vc, scale)) @ w_o if need_ctx else None
    return y_lat, y_ctx


def mixer_diff(h_lat, h_ctx, w_qkv, q_norm, k_norm, lam, subln, w_o, rope, lambda_init, need_ctx):
    qk_width = 2 * DIFF_HEADS * DIFF_HEAD_DIM
    def project(h, rope_tab):
        q, k, v = jnp.split(h @ w_qkv, [qk_width, 2 * qk_width], axis=-1)
        q = rms_norm(split_heads(q, 2 * DIFF_HEADS), q_norm)
        k = rms_norm(split_heads(k, 2 * DIFF_HEADS), k_norm)
        if rope_tab is not None:
            q, k = apply_rope(q, *rope_tab), apply_rope(k, *rope_tab)
        return q[:, 0::2], q[:, 1::2], k[:, 0::2], k[:, 1::2], split_heads(v, DIFF_HEADS)
    lf = lam.astype(jnp.float32)
    lam_full = jnp.exp(jnp.sum(lf[0] * lf[1])) - jnp.exp(jnp.sum(lf[2] * lf[3])) + lambda_init
    scale = DIFF_HEAD_DIM ** -0.5

    def combine(o1, o2):
        o = o1 - lam_full.astype(o1.dtype) * o2
        return merge_heads(rms_norm(o, subln) * (1.0 - lambda_init)) @ w_o

    q1, q2, k1, k2, v = project(h_lat, rope)
    q1c, q2c, k1c, k2c, vc = project(h_ctx, None)
    y_lat = combine(dense_latent_attention(q1, k1, v, k1c, vc, scale),
                    dense_latent_attention(q2, k2, v, k2c, vc, scale))
    y_ctx = (combine(context_attention(q1c, k1c, vc, scale), context_attention(q2c, k2c, vc, scale))
             if need_ctx else None)
    return y_lat, y_ctx


def hierarchical_moe(h, w_rg, b_rg, w_re, b_re, w_gu, w_down):
    t, d = h.shape
    hf = h.astype(jnp.float32)
    g_prob = jax.nn.softmax(hf @ w_rg.astype(jnp.float32) + b_rg.astype(jnp.float32), axis=-1)
    g_w, g_idx = lax.top_k(g_prob, 1)
    e_logits = (hf @ w_re.astype(jnp.float32) + b_re.astype(jnp.float32)).reshape(t, N_GROUPS, EXPERTS_PER_GROUP)
    e_logits = jnp.take_along_axis(e_logits, g_idx[:, :, None], axis=1)[:, 0]
    e_w, e_idx = lax.top_k(jax.nn.softmax(e_logits, axis=-1), TOP_K)
    gate = g_w * e_w / jnp.sum(e_w, axis=-1, keepdims=True)
    expert = (g_idx * EXPERTS_PER_GROUP + e_idx).astype(jnp.int32)
    n_rows = t * TOP_K
    flat_expert = expert.reshape(-1)
    order = jnp.argsort(flat_expert).astype(jnp.int32)
    sorted_expert = flat_expert[order]
    counts = jnp.zeros((N_EXPERTS,), jnp.int32).at[flat_expert].add(1)
    padded = (counts + MOE_BLOCK - 1) // MOE_BLOCK * MOE_BLOCK
    seg_end_p = jnp.cumsum(padded)
    seg_start_p = seg_end_p - padded
    seg_start = jnp.cumsum(counts) - counts
    dest = seg_start_p[sorted_expert] + jnp.arange(n_rows, dtype=jnp.int32) - seg_start[sorted_expert]
    n_pad_rows = -(-(n_rows + N_EXPERTS * (MOE_BLOCK - 1)) // MOE_BLOCK) * MOE_BLOCK
    n_blocks = n_pad_rows // MOE_BLOCK
    row_token = jnp.zeros((n_pad_rows,), jnp.int32).at[dest].set(order // TOP_K)
    row_gate = jnp.zeros((n_pad_rows,), jnp.float32).at[dest].set(gate.reshape(-1)[order])
    block_expert = jnp.minimum(
        jnp.searchsorted(seg_end_p, jnp.arange(n_blocks, dtype=jnp.int32) * MOE_BLOCK, side='right'),
        N_EXPERTS - 1)

    def one_block(args):
        e, tok, gw = args
        rows = h[tok]
        gte, up = jnp.split(rows @ w_gu[e], 2, axis=-1)
        return (jax.nn.silu(gte) * up * gw[:, None].astype(h.dtype)) @ w_down[e]

    out = lax.map(one_block, (block_expert,
                              row_token.reshape(n_blocks, MOE_BLOCK),
                              row_gate.reshape(n_blocks, MOE_BLOCK)))
    return jnp.zeros_like(h).at[row_token].add(out.reshape(n_pad_rows, d))


def setup_inputs(seed: int = 0) -> dict:
    keys = iter(jax.random.split(jax.random.key(seed), 40))

    def normal(shape, std):
        return std * jax.random.normal(next(keys), shape, jnp.float32)

    def gain(shape):
        return 1.0 + 0.1 * jax.random.normal(next(keys), shape, jnp.float32)

    d = D_MODEL
    n_a, n_b, n_c, n_d = (_layers_of_kind(kind) for kind in range(N_MIXERS))
    qk_diff = 2 * DIFF_HEADS * DIFF_HEAD_DIM
    return {
        'x': normal((BATCH, SEQ, d), 1.0),
        'c': normal((BATCH, d), 1.0),
        'ctx': normal((BATCH, CTX_LEN, d), 1.0),
        'c_ctx': normal((d,), 1.0),
        'ada_w': normal((DEPTH, d, 6 * d), 0.5 * d ** -0.5),
        'ada_b': normal((DEPTH, 6 * d), 0.02),
        'norm_g': gain((DEPTH, 2, d)),
        'moe_w_router_group': normal((DEPTH, d, N_GROUPS), d ** -0.5),
        'moe_b_router_group': normal((DEPTH, N_GROUPS), 0.01),
        'moe_w_router_expert': normal((DEPTH, d, N_EXPERTS), d ** -0.5),
        'moe_b_router_expert': normal((DEPTH, N_EXPERTS), 0.01),
        'moe_w_gate_up': normal((DEPTH, N_EXPERTS, d, 2 * D_EXPERT), d ** -0.5),
        'moe_w_down': normal((DEPTH, N_EXPERTS, D_EXPERT, d), D_EXPERT ** -0.5),
        'na_w_qkv': normal((n_a, d, 3 * NA_HEADS * NA_HEAD_DIM), d ** -0.5),
        'na_w_o': normal((n_a, NA_HEADS * NA_HEAD_DIM, d), (NA_HEADS * NA_HEAD_DIM) ** -0.5),
        'na_q_norm': gain((n_a, NA_HEAD_DIM)),
        'na_k_norm': gain((n_a, NA_HEAD_DIM)),
        'na_rpb': normal((n_a, NA_HEADS, (2 * NA_KH - 1) * (2 * NA_KW - 1)), 0.5),
        'sw_w_qkv': normal((n_b, d, (SW_HEADS + 2 * SW_KV_HEADS) * SW_HEAD_DIM), d ** -0.5),
        'sw_w_o': normal((n_b, SW_HEADS * SW_HEAD_DIM, d), (SW_HEADS * SW_HEAD_DIM) ** -0.5),
        'sw_q_norm': gain((n_b, SW_HEAD_DIM)),
        'sw_k_norm': gain((n_b, SW_HEAD_DIM)),
        'sw_sink': normal((n_b, SW_HEADS), 1.0),
        'mla_w_dqkv': normal((n_c, d, MLA_Q_RANK + MLA_KV_RANK + MLA_ROPE), d ** -0.5),
        'mla_q_a_norm': gain((n_c, MLA_Q_RANK)),
        'mla_kv_a_norm': gain((n_c, MLA_KV_RANK)),
        'mla_w_uq': normal((n_c, MLA_Q_RANK, MLA_HEADS * (MLA_NOPE + MLA_ROPE)), MLA_Q_RANK ** -0.5),
        'mla_w_ukv': normal((n_c, MLA_KV_RANK, MLA_HEADS * (MLA_NOPE + MLA_V)), MLA_KV_RANK ** -0.5),
        'mla_q_norm': gain((n_c, MLA_NOPE + MLA_ROPE)),
        'mla_k_norm': gain((n_c, MLA_NOPE + MLA_ROPE)),
        'mla_w_o': normal((n_c, MLA_HEADS * MLA_V, d), (MLA_HEADS * MLA_V) ** -0.5),
        'diff_w_qkv': normal((n_d, d, 2 * qk_diff + DIFF_HEADS * 2 * DIFF_HEAD_DIM), d ** -0.5),
        'diff_q_norm': gain((n_d, DIFF_HEAD_DIM)),
        'diff_k_norm': gain((n_d, DIFF_HEAD_DIM)),
        'diff_lambda': normal((n_d, 4, DIFF_HEAD_DIM), 0.1),
        'diff_subln': gain((n_d, 2 * DIFF_HEAD_DIM)),
        'diff_w_o': normal((n_d, DIFF_HEADS * 2 * DIFF_HEAD_DIM, d), (DIFF_HEADS * 2 * DIFF_HEAD_DIM) ** -0.5),
    }


def reference(x, c, ctx, c_ctx, ada_w, ada_b, norm_g,
              moe_w_router_group, moe_b_router_group, moe_w_router_expert, moe_b_router_expert,
              moe_w_gate_up, moe_w_down,
              na_w_qkv, na_w_o, na_q_norm, na_k_norm, na_rpb,
              sw_w_qkv, sw_w_o, sw_q_norm, sw_k_norm, sw_sink,
              mla_w_dqkv, mla_q_a_norm, mla_kv_a_norm, mla_w_uq, mla_w_ukv, mla_q_norm, mla_k_norm, mla_w_o,
              diff_w_qkv, diff_q_norm, diff_k_norm, diff_lambda, diff_subln, diff_w_o):
    b, s, d = x.shape
    n_ctx = ctx.shape[1]
    rope_head = axial_rope(s, SW_HEAD_DIM)
    rope_mla = axial_rope(s, MLA_ROPE)
    x_lat, x_ctx = x, ctx
    for l in range(DEPTH):
        kind, j = l % N_MIXERS, l // N_MIXERS
        need_ctx = l < DEPTH - 1
        mod_lat = (jax.nn.silu(c) @ ada_w[l] + ada_b[l])[:, None, :]
        mod_ctx = (jax.nn.silu(c_ctx) @ ada_w[l] + ada_b[l])[None, None, :]
        sh1, sc1, g1, sh2, sc2, g2 = jnp.split(mod_lat, 6, axis=-1)
        csh1, csc1, cg1, csh2, csc2, cg2 = jnp.split(mod_ctx, 6, axis=-1)
        h_lat = modulate(x_lat, norm_g[l, 0], sh1, sc1)
        h_ctx = modulate(x_ctx, norm_g[l, 0], csh1, csc1)
        if kind == 0:
            y_lat, y_ctx = mixer_neighbourhood(h_lat, h_ctx, na_w_qkv[j], na_w_o[j], na_q_norm[j],
                                               na_k_norm[j], na_rpb[j], need_ctx)
        elif kind == 1:
            y_lat, y_ctx = mixer_window(h_lat, h_ctx, sw_w_qkv[j], sw_w_o[j], sw_q_norm[j], sw_k_norm[j],
                                        sw_sink[j], rope_head, need_ctx)
        elif kind == 2:
            y_lat, y_ctx = mixer_mla(h_lat, h_ctx, mla_w_dqkv[j], mla_q_a_norm[j], mla_kv_a_norm[j],
                                     mla_w_uq[j], mla_w_ukv[j], mla_q_norm[j], mla_k_norm[j], mla_w_o[j],
                                     rope_mla, need_ctx)
        else:
            y_lat, y_ctx = mixer_diff(h_lat, h_ctx, diff_w_qkv[j], diff_q_norm[j], diff_k_norm[j],
                                      diff_lambda[j], diff_subln[j], diff_w_o[j], rope_head,
                                      0.8 - 0.6 * math.exp(-0.3 * l), need_ctx)
        x_lat = x_lat + g1 * y_lat
        h_lat = modulate(x_lat, norm_g[l, 1], sh2, sc2)
        moe_args = (moe_w_router_group[l], moe_b_router_group[l], moe_w_router_expert[l],
                    moe_b_router_expert[l], moe_w_gate_up[l], moe_w_down[l])
        if need_ctx:
            x_ctx = x_ctx + cg1 * y_ctx
            h_ctx = modulate(x_ctx, norm_g[l, 1], csh2, csc2)
            tokens = jnp.concatenate([h_lat.reshape(-1, d), h_ctx.reshape(-1, d)], axis=0)
            y = hierarchical_moe(tokens, *moe_args)
            x_lat = x_lat + g2 * y[:b * s].reshape(b, s, d)
            x_ctx = x_ctx + cg2 * y[b * s:].reshape(b, n_ctx, d)
        else:
            y = hierarchical_moe(h_lat.reshape(-1, d), *moe_args)
            x_lat = x_lat + g2 * y.reshape(b, s, d)
    return x_lat
```

```python
import math
from contextlib import ExitStack
import numpy as np
import concourse.bass as bass
import concourse.mybir as mybir
from concourse.bass_utils import run_bass_kernel_spmd

F32 = mybir.dt.float32
BF16 = mybir.dt.bfloat16
AF = mybir.ActivationFunctionType
ALU = mybir.AluOpType

D = 1024
SEQ = 4096
NCTX = 256
T = SEQ + NCTX
NKT = T // 128
DEPTH = 4
EPS = 1e-6
NEG = -30000.0
NE = 32


class Buf:
    __slots__ = ("ap", "w", "r")

    def __init__(self, ap):
        self.ap = ap
        self.w = {}
        self.r = {}


class Sched:
    NR = 8
    EPOCH = 1000000

    def __init__(self, nc):
        self.nc = nc
        self.E = {"pe": nc.tensor, "act": nc.scalar, "dve": nc.vector, "pool": nc.gpsimd, "sp": nc.sync}
        self.sems = {}
        self.ckey = {}
        self.ccnt = {}
        self.cep = {}
        for e in ("pe", "act", "dve", "pool"):
            self.cep[e] = 0
            self._new_epoch(e)
        self.seen = {e: {} for e in self.E}
        self.dkeys = {}
        self.dcnt = {}
        for q in ("sp", "pool", "act"):
            ks = []
            for i in range(self.NR):
                k = f"d_{q}_{i}"
                self.sems[k] = nc.alloc_semaphore(k)
                ks.append(k)
            self.dkeys[q] = ks
            self.dcnt[q] = 0
        self.dlast = {}
        self.n_inst = 0

    def _new_epoch(self, e):
        k = f"c_{e}_{self.cep[e]}"
        self.cep[e] += 1
        self.sems[k] = self.nc.alloc_semaphore(k)
        self.ckey[e] = k
        self.ccnt[e] = 0

    def _wait(self, e, k, v):
        if self.seen[e].get(k, 0) >= v:
            return
        self.E[e].wait_ge(self.sems[k], v)
        self.seen[e][k] = v

    def _collect(self, e, reads, writes, is_dma):
        own = f"c_{e}_"
        need = {}
        for b in reads:
            for k, v in b.w.items():
                if (not is_dma) and e == "pe" and k.startswith(own):
                    continue
                if need.get(k, 0) < v:
                    need[k] = v
        for b in writes:
            for src in (b.w, b.r):
                for k, v in src.items():
                    if (not is_dma) and k.startswith(own):
                        continue
                    if need.get(k, 0) < v:
                        need[k] = v
        for k, v in need.items():
            self._wait(e, k, v)

    def _commit(self, k, v, reads, writes):
        for b in reads:
            if b.r.get(k, 0) < v:
                b.r[k] = v
        for b in writes:
            if b.r:
                b.w = {k: v}
                b.r = {}
            else:
                b.w[k] = v

    def op(self, e, fn, reads=(), writes=()):
        self._collect(e, reads, writes, False)
        inst = fn(self.E[e])
        if self.ccnt[e] >= self.EPOCH:
            self._new_epoch(e)
        self.ccnt[e] += 1
        k, v = self.ckey[e], self.ccnt[e]
        inst.then_inc(self.sems[k], 1)
        self._commit(k, v, reads, writes)
        self.n_inst += 1

    def dma(self, q, out, in_, reads=(), writes=()):
        self._collect(q, reads, writes, True)
        i = self.dcnt[q]
        self.dcnt[q] += 1
        k = self.dkeys[q][i % self.NR]
        prev = 16 * (i // self.NR)
        if prev:
            self._wait(q, k, prev)
        inst = self.E[q].dma_start(out=out, in_=in_)
        inst.then_inc(self.sems[k], 16)
        v = prev + 16
        assert v < 60000
        self.dlast[k] = v
        self._commit(k, v, reads, writes)
        self.n_inst += 1

    def barrier(self):
        tick = {}
        for e in ("pe", "act", "dve", "pool"):
            if self.ccnt[e]:
                tick[self.ckey[e]] = self.ccnt[e]
        tick.update(self.dlast)
        for e in self.E:
            for k, v in tick.items():
                self._wait(e, k, v)


def _token_blocks(with_ctx=True):
    bl = [(i * 512, 512) for i in range(SEQ // 512)]
    if with_ctx:
        bl.append((SEQ, NCTX))
    return bl


class Builder:
    topk_eng = "dve"

    def __init__(self, layers, first, last):
        self.layers = layers
        self.first = first
        self.last = last
        nc = bass.Bass("TRN2", target_bir_lowering=False)
        self.nc = nc
        self.S = Sched(nc)
        self.rot = {}
        self._decl_dram()
        self._build()

    def din(self, name, shape, dt=F32, kinds=None):
        if kinds is not None and not (set(kinds) & set(l % 4 for l in self.layers)):
            return None
        if not hasattr(self, "in_names"):
            self.in_names = []
        self.in_names.append(name)
        return self.nc.dram_tensor(name, list(shape), dt, kind="ExternalInput").ap()

    def dscr(self, name, shape, dt):
        return self.nc.dram_tensor(name, list(shape), dt, kind="Internal").ap()

    def sb(self, es, name, shape, dt):
        self._uid = getattr(self, "_uid", 0) + 1
        t = es.enter_context(self.nc.sbuf_tensor(f"{name}_u{self._uid}", list(shape), dt))
        return t

    def nxt(self, key, n):
        i = self.rot.get(key, 0)
        self.rot[key] = i + 1
        return i % n

    def _decl_dram(self):
        nc = self.nc
        self.x_in = self.din("xT_in", [D, T])
        self.cc = self.din("cc", [128, 8, 2])
        self.ada_w = {l: self.din(f"ada_w{l}", [D, 6 * D]) for l in self.layers}
        self.ada_b = self.din("ada_b", [DEPTH, 128, 48, 2])
        self.norm_g = self.din("norm_g", [DEPTH, 2, 128, 8, 2])
        self.wr = self.din("wr", [DEPTH, D, 36])
        self.br = self.din("br", [DEPTH, 128, 36])
        self.w_gu = {l: self.din(f"w_gu{l}", [NE, D, 512]) for l in self.layers}
        self.w_dn = {l: self.din(f"w_dn{l}", [NE, 256, D]) for l in self.layers}
        self.wqkv = {0: self.din("na_w_qkv", [D, 3072], kinds=[0]), 1: self.din("sw_w_qkv", [D, 1536], kinds=[1]),
                     3: self.din("diff_w_qkv", [D, 3072], kinds=[3])}
        self.wo = {0: self.din("na_w_o", [D, D], kinds=[0]), 1: self.din("sw_w_o", [D, D], kinds=[1]),
                   2: self.din("mla_w_o", [D, D], kinds=[2]), 3: self.din("diff_w_o", [D, D], kinds=[3])}
        self.qkg = {0: self.din("na_qk_g", [128, 2], kinds=[0]), 1: self.din("sw_qk_g", [128, 2], kinds=[1]),
                    3: self.din("diff_qk_g", [128, 2], kinds=[3])}
        self.na_bias = self.din("na_bias", [16, 3, 128, 6, 512], kinds=[0])
        self.sw_sink = self.din("sw_sink", [128, 16], kinds=[1])
        self.sw_mask = self.din("sw_mask", [128, 6, 512], kinds=[1])
        self.rope64 = self.din("rope64", [2, 128, T], kinds=[1, 3])
        self.mla_w_dqkv = self.din("mla_w_dqkv", [D, 672], kinds=[2])
        self.mla_w_uq = self.din("mla_w_uq", [384, 1536], kinds=[2])
        self.mla_w_ukv = self.din("mla_w_ukv", [256, 2048], kinds=[2])
        self.mla_g = self.din("mla_g", [128, 8], kinds=[2])
        self.rope_mla = self.din("rope_mla", [2, 96, T], kinds=[2])
        self.diff_lam = self.din("diff_lam", [128, 4, 64], kinds=[3])
        self.diff_subln = self.din("diff_subln", [128, 1], kinds=[3])
        self.consts = self.din("consts", [128, 8, 128])
        self.sel_in = self.din("sel", [32, NE, 128])
        self.xs = self.dscr("xs", [D, T], F32)
        self.q_s = self.dscr("q_s", [16, 96, T], BF16)
        self.k_s = self.dscr("k_s", [16, 96, T], BF16)
        self.v_s = self.dscr("v_s", [16, 128, NKT, 64], BF16)
        self.kr_s = self.dscr("kr_s", [32, T], BF16)
        self.v_s2 = self.dscr("v_s2", [8, 128, NKT, 128], BF16)
        self.at_s = self.dscr("at_s", [D, T], BF16)
        self.h2_s = self.dscr("h2_s", [D, T], BF16)
        if self.last:
            self.out = nc.dram_tensor("outT", [D, SEQ], F32, kind="ExternalOutput").ap()
        else:
            self.out = nc.dram_tensor("xT_out", [D, T], F32, kind="ExternalOutput").ap()

    def _build(self):
        nc, S = self.nc, self.S
        with ExitStack() as es:
            self.ps = [nc.alloc_psum_tensor(f"ps{i}", [128, 512], F32) for i in range(8)]
            self.psb = [Buf(p) for p in self.ps]
            cst = self.sb(es, "cst_f", [128, 8, 128], F32)
            self.cst_f = cst
            cb = Buf(cst)
            S.dma("sp", cst[:, :, :], self.consts, writes=[cb])
            cstb = self.sb(es, "cst_b", [128, 8, 128], BF16)
            self.cstb_buf = Buf(cstb)
            self._cstb = cstb
            self.eps_col = self.sb(es, "eps_col", [128, 1], F32)
            self.eps_buf = Buf(self.eps_col)
            S.op("dve", lambda e: e.memset(self.eps_col[:, :], EPS), writes=[self.eps_buf])
            S.op("dve", lambda e: e.tensor_copy(out=cstb[:, :, :], in_=cst[:, :, :]), reads=[cb], writes=[self.cstb_buf])
            self.cst_buf = cb
            self.modT = self.sb(es, "modT", [128, DEPTH, 48, 2], F32)
            self.gsT = self.sb(es, "gsT", [128, DEPTH, 2, 8, 2], F32)
            self.mod_buf = Buf(self.modT)
            self.gs_buf = Buf(self.gsT)
            self.phase0()
            x_src = self.x_in
            for L in self.layers:
                kind = L % 4
                need_ctx = L < DEPTH - 1
                is_last_layer = (L == self.layers[-1])
                x_dst = self.xs
                S.barrier()
                with nc.named_scope(f"L{L}_p1"):
                    if kind == 2:
                        self.phase1_mla(L, x_src)
                    else:
                        self.phase1(L, x_src)
                    S.barrier()
                import os as _os
                _stop = int(_os.environ.get("K_STOP", "9"))
                with nc.named_scope(f"L{L}_p2"):
                    if _stop >= 2:
                        self.phase2(L, need_ctx)
                    S.barrier()
                if _stop >= 3:
                    self.phase3(L, x_src, x_dst, need_ctx, final=(is_last_layer))
                x_src = self.xs
            S.barrier()

    def phase0(self):
        nc, S = self.nc, self.S
        with ExitStack() as es:
            cc = self.sb(es, "cc", [128, 8, 2], F32)
            sc = self.sb(es, "scc", [128, 8, 2], F32)
            ccb, scb = Buf(cc), Buf(sc)
            S.dma("sp", cc[:, :, :], self.cc, writes=[ccb])
            S.op("act", lambda e: e.activation(out=sc[:, :, :], in_=cc[:, :, :], func=AF.Silu), reads=[ccb], writes=[scb])
            wm = [self.sb(es, f"wm{i}", [128, 8, 1024], F32) for i in range(2)]
            wmb = [Buf(w) for w in wm]
            adab = self.sb(es, "adab", [128, 48, 2], F32)
            adabb = Buf(adab)
            gn = self.sb(es, "gn", [128, 2, 8, 2], F32)
            gnb = Buf(gn)
            pm = self.ps[0]
            pmb = self.psb[0]
            for L in self.layers:
                S.dma("sp", adab[:, :, :], self.ada_b[L], writes=[adabb])
                S.dma("sp", gn[:, :, :, :], self.norm_g[L].rearrange("i p k t -> p i k t"), writes=[gnb])
                for which in range(6):
                    i = self.nxt("wm", 2)
                    src = self.ada_w[L].rearrange("(kc p) n -> p kc n", p=128)[:, :, which * 1024:(which + 1) * 1024]
                    S.dma("sp", wm[i][:, :, :], src, writes=[wmb[i]])
                    for fc in range(8):
                        j = which * 8 + fc
                        for kc in range(8):
                            S.op("pe", lambda e, i=i, fc=fc, kc=kc, j=j: e.matmul(
                                pm[:, 2 * j:2 * j + 2], lhsT=wm[i][:, kc, fc * 128:(fc + 1) * 128], rhs=sc[:, kc, :],
                                start=(kc == 0), stop=(kc == 7)), reads=[wmb[i], scb], writes=[pmb])
                mo = self.modT[:, L, :, :]
                S.op("dve", lambda e: e.tensor_tensor(out=mo, in0=pm[:, 0:96].rearrange("p (j t) -> p j t", t=2),
                                                      in1=adab[:, :, :], op=ALU.add),
                     reads=[pmb, adabb], writes=[self.mod_buf])
                for i2, sci in ((0, 1), (1, 4)):
                    g = self.gsT[:, L, i2, :, :]
                    S.op("dve", lambda e, g=g, sci=sci: e.tensor_scalar(
                        out=g, in0=self.modT[:, L, sci * 8:(sci + 1) * 8, :], scalar1=1.0, scalar2=None, op0=ALU.add),
                        reads=[self.mod_buf], writes=[self.gs_buf])
                    S.op("dve", lambda e, g=g, i2=i2: e.tensor_tensor(out=g, in0=g, in1=gn[:, i2, :, :], op=ALU.mult),
                         reads=[self.gs_buf, gnb], writes=[self.gs_buf])
            S.barrier()

    def modcol(self, L, which, fc, t):
        return self.modT[:, L, which * 8 + fc, t:t + 1]

    def gscol(self, L, i2, fc, t):
        return self.gsT[:, L, i2, fc, t:t + 1]

    def norm_mod(self, L, i2, xin, xb, n, t, rs, rsb, sq, sqb, tmp, tmpb, outs, pss):
        S = self.S
        ones_b = self.cstb[:, 0, :]
        pbank, pbuf = self.ps[pss], self.psb[pss]
        for kc in range(8):
            j = self.nxt("sq", 2)
            S.op("act", lambda e, j=j, kc=kc: e.activation(out=sq[j][:, :n], in_=xin[:, kc, :n], func=AF.Square),
                 reads=[xb], writes=[sqb[j]])
            S.op("pe", lambda e, j=j, kc=kc: e.matmul(pbank[:, :n], lhsT=ones_b, rhs=sq[j][:, :n],
                                                     start=(kc == 0), stop=(kc == 7)),
                 reads=[sqb[j], self.cstb_buf], writes=[pbuf])
        S.op("act", lambda e: e.activation(out=rs[:, :n], in_=pbank[:, :n], func=AF.Sqrt, bias=self.eps_col[:, 0:1], scale=1.0 / D),
             reads=[pbuf, self.eps_buf], writes=[rsb])
        S.op("dve", lambda e: e.reciprocal(out=rs[:, :n], in_=rs[:, :n]), reads=[rsb], writes=[rsb])
        sh = 0 if i2 == 0 else 3
        for kc in range(8):
            j = self.nxt("tmp", 2)
            S.op("dve", lambda e, j=j, kc=kc: e.tensor_tensor(out=tmp[j][:, :n], in0=xin[:, kc, :n], in1=rs[:, :n], op=ALU.mult),
                 reads=[xb, rsb], writes=[tmpb[j]])
            first = True
            for (ot, ob, eng) in outs:
                if first:
                    S.op("act", lambda e, j=j, kc=kc, ot=ot: e.activation(
                        out=ot[:, kc, :n], in_=tmp[j][:, :n], func=AF.Identity,
                        scale=self.gscol(L, i2, kc, t), bias=self.modcol(L, sh, kc, t)),
                        reads=[tmpb[j], self.gs_buf, self.mod_buf], writes=[ob])
                    first = False
                    prev_t, prev_b = ot, ob
                else:
                    S.op(eng, lambda e, kc=kc, ot=ot, prev_t=prev_t: e.tensor_copy(out=ot[:, kc, :n], in_=prev_t[:, kc, :n]),
                         reads=[prev_b], writes=[ob])

    @property
    def cstb(self):
        return self._cstb

    def phase1(self, L, x_src):
        nc, S = self.nc, self.S
        kind = L % 4
        ncol = {0: 3072, 1: 1536, 3: 3072}[kind]
        nq_ch = 8
        nk_ch = {0: 8, 1: 2, 3: 8}[kind]
        kcol0 = 1024
        vcol0 = {0: 2048, 1: 1280, 3: 2048}[kind]
        vw = {0: 1024, 1: 256, 3: 1024}[kind]
        dv = {0: 64, 1: 64, 3: 128}[kind]
        rope = kind in (1, 3)
        with ExitStack() as es:
            W = self.sb(es, "w1", [128, 8, ncol], BF16)
            Wb = [Buf(W) for _ in range(8)]
            wsrc = self.wqkv[kind].rearrange("(kc p) n -> p kc n", p=128)
            for kc in range(8):
                S.dma("pool", W[:, kc, :], wsrc[:, kc, :], writes=[Wb[kc]])
            qkg = self.sb(es, "qkg", [128, 2], F32)
            qkgb = Buf(qkg)
            S.dma("sp", qkg[:, :], self.qkg[kind], writes=[qkgb])
            xin = [self.sb(es, f"xin{i}", [128, 8, 512], F32) for i in range(2)]
            xinb = [Buf(t) for t in xin]
            hT = [self.sb(es, f"hT{i}", [128, 8, 512], BF16) for i in range(2)]
            hTb = [Buf(t) for t in hT]
            rs = self.sb(es, "rs", [128, 512], F32); rsb = Buf(rs)
            sq = [self.sb(es, f"sq{i}", [128, 512], BF16) for i in range(2)]; sqb = [Buf(t) for t in sq]
            tmp = [self.sb(es, f"tmp{i}", [128, 512], F32) for i in range(2)]; tmpb = [Buf(t) for t in tmp]
            rs2 = [self.sb(es, f"rs2{i}", [128, 512], F32) for i in range(2)]; rs2b = [Buf(t) for t in rs2]
            qn = [self.sb(es, f"qn{i}", [128, 512], BF16) for i in range(3)]; qnb = [Buf(t) for t in qn]
            qo = [self.sb(es, f"qo{i}", [128, 512], BF16) for i in range(3)]; qob = [Buf(t) for t in qo]
            t1 = [self.sb(es, f"t1{i}", [128, 512], F32) for i in range(2)]; t1b = [Buf(t) for t in t1]
            t2 = [self.sb(es, f"t2{i}", [128, 512], F32) for i in range(2)]; t2b = [Buf(t) for t in t2]
            vt = [self.sb(es, f"vt{i}", [128, 512], BF16) for i in range(3)]; vtb = [Buf(t) for t in vt]
            if rope:
                cs = [self.sb(es, f"cs{i}", [128, 2, 512], F32) for i in range(2)]
                csb = [Buf(t) for t in cs]
            blocks = _token_blocks(True)
            xsrc = x_src.rearrange("(kc p) t -> p kc t", p=128)

            def load(bi):
                t0, n = blocks[bi]
                i = bi % 2
                S.dma("sp", xin[i][:, :, :n], xsrc[:, :, t0:t0 + n], writes=[xinb[i]])
                if rope:
                    S.dma("sp", cs[i][:, :, :n], self.rope64.rearrange("c p t -> p c t")[:, :, t0:t0 + n], writes=[csb[i]])

            load(0)
            blk64 = self.cstb[:, 1, :]
            Rm = self.cstb[:, 2, :]
            for bi, (t0, n) in enumerate(blocks):
                if bi + 1 < len(blocks):
                    load(bi + 1)
                i = bi % 2
                tq = 0 if t0 < SEQ else 1
                self.norm_mod(L, 0, xin[i], xinb[i], n, tq, rs, rsb, sq, sqb, tmp, tmpb, [(hT[i], hTb[i], "act")], 7)
                nch = nq_ch + nk_ch
                cst = {}
                p1q_banks = [0, 1, 6]

                def stP(c):
                    isq = c < nq_ch
                    col0 = c * 128 if isq else kcol0 + (c - nq_ch) * 128
                    pb = p1q_banks[self.nxt("p1q3", 3)]
                    pq, pqb = self.ps[pb], self.psb[pb]
                    for kc in range(8):
                        S.op("pe", lambda e, kc=kc: e.matmul(
                            pq[:, :n], lhsT=W[:, kc, col0:col0 + 128], rhs=hT[i][:, kc, :n], start=(kc == 0), stop=(kc == 7)),
                            reads=[Wb[kc], hTb[i]], writes=[pqb])
                    cst[c] = dict(pq=pq, pqb=pqb, isq=isq)

                def stN(c):
                    d_ = cst[c]
                    pq, pqb, isq = d_["pq"], d_["pqb"], d_["isq"]
                    j = self.nxt("sq", 2)
                    S.op("act", lambda e: e.activation(out=sq[j][:, :n], in_=pq[:, :n], func=AF.Square),
                         reads=[pqb], writes=[sqb[j]])
                    pmi = 2 + self.nxt("p1m", 2)
                    pm, pmb = self.ps[pmi], self.psb[pmi]
                    S.op("pe", lambda e: e.matmul(pm[:, :n], lhsT=blk64, rhs=sq[j][:, :n], start=True, stop=True),
                         reads=[sqb[j], self.cstb_buf], writes=[pmb])
                    r = self.nxt("rs2", 2)
                    S.op("act", lambda e: e.activation(out=rs2[r][:, :n], in_=pm[:, :n], func=AF.Sqrt,
                                                       bias=self.eps_col[:, 0:1], scale=1.0),
                         reads=[pmb, self.eps_buf], writes=[rs2b[r]])
                    S.op("dve", lambda e: e.reciprocal(out=rs2[r][:, :n], in_=rs2[r][:, :n]), reads=[rs2b[r]], writes=[rs2b[r]])
                    gcol = qkg[:, 0:1] if isq else qkg[:, 1:2]
                    if rope and tq == 0:
                        a = self.nxt("qn", 3)
                        S.op("dve", lambda e: e.scalar_tensor_tensor(
                            out=qn[a][:, :n], in0=pq[:, :n], scalar=gcol, in1=rs2[r][:, :n], op0=ALU.mult, op1=ALU.mult),
                            reads=[pqb, rs2b[r], qkgb], writes=[qnb[a]])
                        d_["a"] = a
                    else:
                        o = self.nxt("qo", 3)
                        S.op("dve", lambda e: e.scalar_tensor_tensor(
                            out=qo[o][:, :n], in0=pq[:, :n], scalar=gcol, in1=rs2[r][:, :n], op0=ALU.mult, op1=ALU.mult),
                            reads=[pqb, rs2b[r], qkgb], writes=[qob[o]])
                        d_["o"] = o

                def stR(c):
                    d_ = cst[c]
                    isq = d_["isq"]
                    dst_s = self.q_s if isq else self.k_s
                    hh = (c if isq else c - nq_ch) * 2
                    if "a" in d_:
                        a = d_["a"]
                        pri = 4 + self.nxt("p1r", 2)
                        pr, prb = self.ps[pri], self.psb[pri]
                        S.op("pe", lambda e: e.matmul(pr[:, :n], lhsT=Rm, rhs=qn[a][:, :n], start=True, stop=True),
                             reads=[qnb[a], self.cstb_buf], writes=[prb])
                        u = self.nxt("t1", 2)
                        S.op("dve", lambda e: e.tensor_tensor(out=t1[u][:, :n], in0=qn[a][:, :n], in1=cs[i][:, 0, :n], op=ALU.mult),
                             reads=[qnb[a], csb[i]], writes=[t1b[u]])
                        S.op("dve", lambda e: e.tensor_tensor(out=t2[u][:, :n], in0=pr[:, :n], in1=cs[i][:, 1, :n], op=ALU.mult),
                             reads=[prb, csb[i]], writes=[t2b[u]])
                        o = self.nxt("qo", 3)
                        S.op("pool", lambda e: e.tensor_tensor(out=qo[o][:, :n], in0=t1[u][:, :n], in1=t2[u][:, :n], op=ALU.add),
                             reads=[t1b[u], t2b[u]], writes=[qob[o]])
                    else:
                        o = d_["o"]
                    for h2 in range(2):
                        S.dma("sp", dst_s[hh + h2, 0:64, t0:t0 + n], qo[o][h2 * 64:(h2 + 1) * 64, :n], reads=[qob[o]])

                for it_ in range(nch + 2):
                    if it_ < nch:
                        stP(it_)
                    if 0 <= it_ - 1 < nch:
                        stN(it_ - 1)
                    if 0 <= it_ - 2 < nch:
                        stR(it_ - 2)
                for tt in range(n // 128):
                    kt = (t0 // 128) + tt
                    for cbk in range((vw + 511) // 512):
                        w = min(512, vw - cbk * 512)
                        pvi = 4 + self.nxt("p1r", 2)
                        pv, pvb = self.ps[pvi], self.psb[pvi]
                        for kc in range(8):
                            S.op("pe", lambda e, kc=kc, pv=pv, tt=tt, cbk=cbk, w=w: e.matmul(
                                pv[:, :w], lhsT=hT[i][:, kc, tt * 128:(tt + 1) * 128],
                                rhs=W[:, kc, vcol0 + cbk * 512: vcol0 + cbk * 512 + w], start=(kc == 0), stop=(kc == 7)),
                                reads=[Wb[kc], hTb[i]], writes=[pvb])
                        vi = self.nxt("vt", 3)
                        S.op("act", lambda e, vi=vi, pv=pv, w=w: e.activation(out=vt[vi][:, :w], in_=pv[:, :w], func=AF.Copy),
                             reads=[pvb], writes=[vtb[vi]])
                        nh = w // dv
                        h0 = cbk * 512 // dv
                        if dv == 64:
                            dst = self.v_s[h0:h0 + nh, :, kt, :].rearrange("h p d -> p h d")
                        else:
                            dst = self.v_s2[h0:h0 + nh, :, kt, :].rearrange("h p d -> p h d")
                        S.dma("sp", dst, vt[vi][:, :w].rearrange("p (h d) -> p h d", d=dv), reads=[vtb[vi]])

    def phase1_mla(self, L, x_src):
        nc, S = self.nc, self.S
        with ExitStack() as es:
            W = self.sb(es, "w1", [128, 8, 672], BF16)
            Wb = [Buf(W) for _ in range(8)]
            wsrc = self.mla_w_dqkv.rearrange("(kc p) n -> p kc n", p=128)
            for kc in range(8):
                S.dma("pool", W[:, kc, :], wsrc[:, kc, :], writes=[Wb[kc]])
            Wuq = self.sb(es, "wuq", [128, 3, 1536], BF16); Wuqb = Buf(Wuq)
            S.dma("pool", Wuq[:, :, :], self.mla_w_uq.rearrange("(kc p) n -> p kc n", p=128), writes=[Wuqb])
            Wuk = self.sb(es, "wuk", [128, 2, 16, 64], BF16); Wukb = Buf(Wuk)
            Wuv = self.sb(es, "wuv", [128, 2, 16, 64], BF16); Wuvb = Buf(Wuv)
            ukv = self.mla_w_ukv.rearrange("(kc p) (h two d) -> p kc h two d", p=128, two=2, d=64)
            for kc in range(2):
                S.dma("pool", Wuk[:, kc, :, :], ukv[:, kc, :, 0, :], writes=[Wukb])
                S.dma("pool", Wuv[:, kc, :, :], ukv[:, kc, :, 1, :], writes=[Wuvb])
            mg = self.sb(es, "mg", [128, 8], F32); mgb = Buf(mg)
            S.dma("sp", mg[:, :], self.mla_g, writes=[mgb])
            xin = [self.sb(es, f"xin{i}", [128, 8, 512], F32) for i in range(2)]
            xinb = [Buf(t) for t in xin]
            hT = [self.sb(es, f"hT{i}", [128, 8, 512], BF16) for i in range(2)]
            hTb = [Buf(t) for t in hT]
            rs = self.sb(es, "rs", [128, 512], F32); rsb = Buf(rs)
            sq = [self.sb(es, f"sq{i}", [128, 512], BF16) for i in range(2)]; sqb = [Buf(t) for t in sq]
            tmp = [self.sb(es, f"tmp{i}", [128, 512], F32) for i in range(2)]; tmpb = [Buf(t) for t in tmp]
            rs2 = [self.sb(es, f"rs2{i}", [128, 512], F32) for i in range(2)]; rs2b = [Buf(t) for t in rs2]
            cf = self.sb(es, "cf", [128, 6, 512], F32); cfb = [Buf(cf) for _ in range(6)]
            cn = self.sb(es, "cn", [128, 5, 512], BF16); cnb = [Buf(cn) for _ in range(5)]
            qn = [self.sb(es, f"qn{i}", [128, 512], BF16) for i in range(3)]; qnb = [Buf(t) for t in qn]
            qo = [self.sb(es, f"qo{i}", [128, 512], BF16) for i in range(3)]; qob = [Buf(t) for t in qo]
            t1 = [self.sb(es, f"t1{i}", [128, 512], F32) for i in range(2)]; t1b = [Buf(t) for t in t1]
            t2 = [self.sb(es, f"t2{i}", [128, 512], F32) for i in range(2)]; t2b = [Buf(t) for t in t2]
            vt = [self.sb(es, f"vt{i}", [128, 512], BF16) for i in range(3)]; vtb = [Buf(t) for t in vt]
            cs = [self.sb(es, f"cs{i}", [96, 2, 512], F32) for i in range(2)]
            csb = [Buf(t) for t in cs]
            self.krt = [self.sb(es, f"krt{i}", [32, 2, 512], F32) for i in range(2)]
            self.krtb = [Buf(t) for t in self.krt]
            blocks = _token_blocks(True)
            xsrc = x_src.rearrange("(kc p) t -> p kc t", p=128)
            ones_b = self.cstb[:, 0, :]
            blk64 = self.cstb[:, 1, :]
            blkm = self.cstb[:, 5, :]
            Rmm = self.cstb[:, 6, :]

            def load(bi):
                t0, n = blocks[bi]
                i = bi % 2
                S.dma("sp", xin[i][:, :, :n], xsrc[:, :, t0:t0 + n], writes=[xinb[i]])
                S.dma("sp", cs[i][:, :, :n], self.rope_mla.rearrange("c p t -> p c t")[:, :, t0:t0 + n], writes=[csb[i]])
                S.dma("sp", self.krt[i][:, :, :n], self.rope_mla.rearrange("c p t -> p c t")[64:96, :, t0:t0 + n], writes=[self.krtb[i]])

            def rsq(pm, pmb, n, P, scale):
                r = self.nxt("rs2", 2)
                S.op("act", lambda e: e.activation(out=rs2[r][:P, :n], in_=pm[:P, :n], func=AF.Sqrt,
                                                   bias=self.eps_col[:P, 0:1], scale=scale),
                     reads=[pmb, self.eps_buf], writes=[rs2b[r]])
                S.op("dve", lambda e: e.reciprocal(out=rs2[r][:P, :n], in_=rs2[r][:P, :n]), reads=[rs2b[r]], writes=[rs2b[r]])
                return r

            load(0)
            for bi, (t0, n) in enumerate(blocks):
                if bi + 1 < len(blocks):
                    load(bi + 1)
                i = bi % 2
                tq = 0 if t0 < SEQ else 1
                self.norm_mod(L, 0, xin[i], xinb[i], n, tq, rs, rsb, sq, sqb, tmp, tmpb, [(hT[i], hTb[i], "act")], 7)
                for c in range(6):
                    M = 128 if c < 5 else 32
                    pb = self.nxt("p1q", 2)
                    pq, pqb = self.ps[pb], self.psb[pb]
                    for kc in range(8):
                        S.op("pe", lambda e, kc=kc, c=c, pq=pq, M=M: e.matmul(
                            pq[:M, :n], lhsT=W[:, kc, c * 128:c * 128 + M], rhs=hT[i][:, kc, :n], start=(kc == 0), stop=(kc == 7)),
                            reads=[Wb[kc], hTb[i]], writes=[pqb])
                    S.op("act", lambda e, c=c, pq=pq, M=M: e.activation(out=cf[:M, c, :n], in_=pq[:M, :n], func=AF.Copy),
                         reads=[pqb], writes=[cfb[c]])
                for (c0, c1, dim) in ((0, 3, 384), (3, 5, 256)):
                    pmi = 2 + self.nxt("p1m", 2)
                    pm, pmb = self.ps[pmi], self.psb[pmi]
                    for c in range(c0, c1):
                        j = self.nxt("sq", 2)
                        S.op("act", lambda e, j=j, c=c: e.activation(out=sq[j][:, :n], in_=cf[:, c, :n], func=AF.Square),
                             reads=[cfb[c]], writes=[sqb[j]])
                        S.op("pe", lambda e, j=j, c=c, pm=pm: e.matmul(pm[:, :n], lhsT=ones_b, rhs=sq[j][:, :n],
                                                                      start=(c == c0), stop=(c == c1 - 1)),
                             reads=[sqb[j], self.cstb_buf], writes=[pmb])
                    r = rsq(pm, pmb, n, 128, 1.0 / dim)
                    for c in range(c0, c1):
                        S.op("dve", lambda e, c=c, r=r: e.scalar_tensor_tensor(
                            out=cn[:, c, :n], in0=cf[:, c, :n], scalar=mg[:, c:c + 1], in1=rs2[r][:, :n], op0=ALU.mult, op1=ALU.mult),
                            reads=[cfb[c], rs2b[r], mgb], writes=[cnb[c]])
                j = self.nxt("sq", 2)
                S.op("act", lambda e, j=j: e.activation(out=sq[j][:32, :n], in_=cf[:32, 5, :n], func=AF.Square),
                     reads=[cfb[5]], writes=[sqb[j]])
                pmi = 2 + self.nxt("p1m", 2)
                pm, pmb = self.ps[pmi], self.psb[pmi]
                S.op("pe", lambda e, j=j, pm=pm: e.matmul(pm[:32, :n], lhsT=self.cstb[:32, 0, 0:32], rhs=sq[j][:32, :n], start=True, stop=True),
                     reads=[sqb[j], self.cstb_buf], writes=[pmb])
                r = rsq(pm, pmb, n, 32, 1.0 / 32)
                a = self.nxt("qn", 3)
                S.op("dve", lambda e, a=a, r=r: e.scalar_tensor_tensor(
                    out=qn[a][:32, :n], in0=cf[:32, 5, :n], scalar=mg[:32, 7:8], in1=rs2[r][:32, :n], op0=ALU.mult, op1=ALU.mult),
                    reads=[cfb[5], rs2b[r], mgb], writes=[qnb[a]])
                pri = 4 + self.nxt("p1r", 2)
                pr, prb = self.ps[pri], self.psb[pri]
                S.op("pe", lambda e, a=a, pr=pr: e.matmul(pr[:32, :n], lhsT=self.cstb[:32, 7, 0:32], rhs=qn[a][:32, :n], start=True, stop=True),
                     reads=[qnb[a], self.cstb_buf], writes=[prb])
                u = self.nxt("t1", 2)
                self._mla_krope(L, S, a, u, pr, prb, qn, qnb, t1, t1b, t2, t2b, qo, qob, n, t0, i)
                p1q_banks = [0, 1, 6]
                hst = {}

                def qP(h):
                    pb = p1q_banks[self.nxt("p1q3", 3)]
                    pq, pqb = self.ps[pb], self.psb[pb]
                    for kc in range(3):
                        S.op("pe", lambda e, kc=kc: e.matmul(
                            pq[:96, :n], lhsT=Wuq[:, kc, h * 96:(h + 1) * 96], rhs=cn[:, kc, :n], start=(kc == 0), stop=(kc == 2)),
                            reads=[Wuqb, cnb[kc]], writes=[pqb])
                    hst[h] = dict(pq=pq, pqb=pqb)

                def qN(h):
                    pq, pqb = hst[h]["pq"], hst[h]["pqb"]
                    j = self.nxt("sq", 2)
                    S.op("act", lambda e: e.activation(out=sq[j][:96, :n], in_=pq[:96, :n], func=AF.Square),
                         reads=[pqb], writes=[sqb[j]])
                    pmi = 2 + self.nxt("p1m", 2)
                    pm, pmb = self.ps[pmi], self.psb[pmi]
                    S.op("pe", lambda e: e.matmul(pm[:96, :n], lhsT=blkm[:96, 0:96], rhs=sq[j][:96, :n], start=True, stop=True),
                         reads=[sqb[j], self.cstb_buf], writes=[pmb])
                    r = rsq(pm, pmb, n, 96, 1.0)
                    a = self.nxt("qn", 3)
                    S.op("dve", lambda e: e.scalar_tensor_tensor(
                        out=qn[a][:96, :n], in0=pq[:96, :n], scalar=mg[:96, 5:6], in1=rs2[r][:96, :n], op0=ALU.mult, op1=ALU.mult),
                        reads=[pqb, rs2b[r], mgb], writes=[qnb[a]])
                    hst[h]["a"] = a

                def qR(h):
                    a = hst[h]["a"]
                    pri = 4 + self.nxt("p1r", 2)
                    pr, prb = self.ps[pri], self.psb[pri]
                    S.op("pe", lambda e: e.matmul(pr[:96, :n], lhsT=Rmm[:96, 0:96], rhs=qn[a][:96, :n], start=True, stop=True),
                         reads=[qnb[a], self.cstb_buf], writes=[prb])
                    u = self.nxt("t1", 2)
                    S.op("dve", lambda e: e.tensor_tensor(out=t1[u][:96, :n], in0=qn[a][:96, :n], in1=cs[i][:96, 0, :n], op=ALU.mult),
                         reads=[qnb[a], csb[i]], writes=[t1b[u]])
                    S.op("dve", lambda e: e.tensor_tensor(out=t2[u][:96, :n], in0=pr[:96, :n], in1=cs[i][:96, 1, :n], op=ALU.mult),
                         reads=[prb, csb[i]], writes=[t2b[u]])
                    o = self.nxt("qo", 3)
                    S.op("pool", lambda e: e.tensor_tensor(out=qo[o][:96, :n], in0=t1[u][:96, :n], in1=t2[u][:96, :n], op=ALU.add),
                         reads=[t1b[u], t2b[u]], writes=[qob[o]])
                    S.dma("sp", self.q_s[h, 0:96, t0:t0 + n], qo[o][:96, :n], reads=[qob[o]])

                for it_ in range(16 + 2):
                    if it_ < 16:
                        qP(it_)
                    if 0 <= it_ - 1 < 16:
                        qN(it_ - 1)
                    if 0 <= it_ - 2 < 16:
                        qR(it_ - 2)

                kst = {}

                def kP(c):
                    pb = p1q_banks[self.nxt("p1q3", 3)]
                    pq, pqb = self.ps[pb], self.psb[pb]
                    for kc in range(2):
                        S.op("pe", lambda e, kc=kc: e.matmul(
                            pq[:, :n], lhsT=Wuk[:, kc, 2 * c:2 * c + 2, :].rearrange("p h d -> p (h d)"), rhs=cn[:, 3 + kc, :n],
                            start=(kc == 0), stop=(kc == 1)),
                            reads=[Wukb, cnb[3 + kc]], writes=[pqb])
                    kst[c] = (pq, pqb)

                def kN(c):
                    pq, pqb = kst[c]
                    j = self.nxt("sq", 2)
                    S.op("act", lambda e: e.activation(out=sq[j][:, :n], in_=pq[:, :n], func=AF.Square),
                         reads=[pqb], writes=[sqb[j]])
                    pmi = 2 + self.nxt("p1m", 2)
                    pm, pmb = self.ps[pmi], self.psb[pmi]
                    S.op("pe", lambda e: e.matmul(pm[:, :n], lhsT=blk64, rhs=sq[j][:, :n], start=True, stop=True),
                         reads=[sqb[j], self.cstb_buf], writes=[pmb])
                    kst[c] = (pq, pqb, pm, pmb)

                def kR(c):
                    pq, pqb, pm, pmb = kst[c]
                    r = rsq(pm, pmb, n, 128, 1.0)
                    o = self.nxt("qo", 3)
                    S.op("dve", lambda e: e.scalar_tensor_tensor(
                        out=qo[o][:, :n], in0=pq[:, :n], scalar=mg[:, 6:7], in1=rs2[r][:, :n], op0=ALU.mult, op1=ALU.mult),
                        reads=[pqb, rs2b[r], mgb], writes=[qob[o]])
                    for h2 in range(2):
                        S.dma("sp", self.k_s[2 * c + h2, 0:64, t0:t0 + n], qo[o][h2 * 64:(h2 + 1) * 64, :n], reads=[qob[o]])

                for it_ in range(8 + 2):
                    if it_ < 8:
                        kP(it_)
                    if 0 <= it_ - 1 < 8:
                        kN(it_ - 1)
                    if 0 <= it_ - 2 < 8:
                        kR(it_ - 2)
                for tt in range(n // 128):
                    kt = (t0 // 128) + tt
                    for cbk in range(2):
                        pvi = 4 + self.nxt("p1r", 2)
                        pv, pvb = self.ps[pvi], self.psb[pvi]
                        for kc in range(2):
                            S.op("pe", lambda e, kc=kc, pv=pv, tt=tt, cbk=cbk: e.matmul(
                                pv[:, :512], lhsT=cn[:, 3 + kc, tt * 128:(tt + 1) * 128],
                                rhs=Wuv[:, kc, cbk * 8:(cbk + 1) * 8, :].rearrange("p h d -> p (h d)"), start=(kc == 0), stop=(kc == 1)),
                                reads=[Wuvb, cnb[3 + kc]], writes=[pvb])
                        vi = self.nxt("vt", 3)
                        S.op("act", lambda e, vi=vi, pv=pv: e.activation(out=vt[vi][:, :512], in_=pv[:, :512], func=AF.Copy),
                             reads=[pvb], writes=[vtb[vi]])
                        dst = self.v_s[cbk * 8:(cbk + 1) * 8, :, kt, :].rearrange("h p d -> p h d")
                        S.dma("sp", dst, vt[vi][:, :512].rearrange("p (h d) -> p h d", d=64), reads=[vtb[vi]])

    def _mla_krope(self, L, S, a, u, pr, prb, qn, qnb, t1, t1b, t2, t2b, qo, qob, n, t0, i):
        kt_ = self.krt[i]
        kb_ = self.krtb[i]
        S.op("pool", lambda e: e.tensor_tensor(out=t1[u][:32, :n], in0=qn[a][:32, :n], in1=kt_[:32, 0, :n], op=ALU.mult),
             reads=[qnb[a], kb_], writes=[t1b[u]])
        S.op("dve", lambda e: e.tensor_tensor(out=t2[u][:32, :n], in0=pr[:32, :n], in1=kt_[:32, 1, :n], op=ALU.mult),
             reads=[prb, kb_], writes=[t2b[u]])
        o = self.nxt("qo", 3)
        S.op("pool", lambda e: e.tensor_tensor(out=qo[o][:32, :n], in0=t1[u][:32, :n], in1=t2[u][:32, :n], op=ALU.add),
             reads=[t1b[u], t2b[u]], writes=[qob[o]])
        S.dma("sp", self.kr_s[:, t0:t0 + n], qo[o][:32, :n], reads=[qob[o]])

    def phase2(self, L, need_ctx):
        nc, S = self.nc, self.S
        kind = L % 4
        dq = 96 if kind == 2 else 64
        dv = 128 if kind == 3 else 64
        scale = dq ** -0.5
        ones_b = self.cstb[:, 0, :]
        with ExitStack() as es:
            nkv = 2 if kind == 3 else 1
            kT = [[self.sb(es, f"kT{i}_{j}", [128, T], BF16) for j in range(nkv)] for i in range(2)]
            kTb = [[Buf(t) for t in row] for row in kT]
            V = [self.sb(es, f"V{i}", [128, NKT, 128], BF16) for i in range(2)]
            Vb = [Buf(t) for t in V]
            qt = [self.sb(es, f"qt{i}", [128, 512], BF16) for i in range(3)]; qtb = [Buf(t) for t in qt]
            for row in kT:
                for t_ in row:
                    pass
            for i_, row in enumerate(kT):
                for j_, t_ in enumerate(row):
                    S.op("pool", lambda e, t_=t_: e.memset(t_[dq:128, :], 0.0), writes=[kTb[i_][j_]])
            for i_, t_ in enumerate(qt):
                S.op("pool", lambda e, t_=t_: e.memset(t_[dq:128, :], 0.0), writes=[qtb[i_]])
            LOOK = 4 if dv == 64 else 3
            sbanks = [0, 1, 2, 5, 6] if dv == 64 else [0, 1, 2, 7]
            NPT = LOOK + 2
            pt = [self.sb(es, f"pt{i}", [128, 512], BF16) for i in range(NPT)]; ptb = [Buf(t) for t in pt]
            rd = [self.sb(es, f"rd{i}", [128, 512], F32) for i in range(2)]; rdb = [Buf(t) for t in rd]
            ot = [self.sb(es, f"ot{i}", [128, 512], BF16) for i in range(2)]; otb = [Buf(t) for t in ot]
            if dv != 64:
                pp = [self.sb(es, f"pp{i}", [128, 512], BF16) for i in range(4)]; ppb = [Buf(t) for t in pp]
                pq4 = [self.sb(es, f"pq4{i}", [128, 512], BF16) for i in range(3)]; pq4b = [Buf(t) for t in pq4]
            if kind == 0:
                bt = [self.sb(es, f"bt{i}", [128, 6, 512], F32) for i in range(2)]; btb = [Buf(t) for t in bt]
                tb = [self.sb(es, f"tb{i}", [128, 512], F32) for i in range(3)]; tbb = [Buf(t) for t in tb]
            if kind == 1:
                mk = self.sb(es, "mk", [128, 6, 512], BF16); mkb = Buf(mk)
                S.dma("pool", mk[:, :, :], self.sw_mask, writes=[mkb])
                snk = self.sb(es, "snk", [128, 16], F32); snkb = Buf(snk)
                S.dma("sp", snk[:, :], self.sw_sink, writes=[snkb])
                S.op("act", lambda e: e.activation(out=snk[:, :], in_=snk[:, :], func=AF.Exp), reads=[snkb], writes=[snkb])
                p0 = [self.sb(es, f"p0{i}", [128, 512], BF16) for i in range(3)]; p0b = [Buf(t) for t in p0]
            if kind == 3:
                lam = self.sb(es, "lam", [128, 4, 64], F32); lamb = Buf(lam)
                S.dma("sp", lam[:, :, :], self.diff_lam, writes=[lamb])
                lt = self.sb(es, "lt", [128, 2, 64], F32); ltb = Buf(lt)
                ls = self.sb(es, "ls", [128, 4], F32); lsb = Buf(ls)
                S.op("dve", lambda e: e.tensor_tensor(out=lt[:, 0, :], in0=lam[:, 0, :], in1=lam[:, 1, :], op=ALU.mult), reads=[lamb], writes=[ltb])
                S.op("dve", lambda e: e.tensor_tensor(out=lt[:, 1, :], in0=lam[:, 2, :], in1=lam[:, 3, :], op=ALU.mult), reads=[lamb], writes=[ltb])
                S.op("dve", lambda e: e.tensor_reduce(out=ls[:, 0:2], in_=lt[:, :, :], axis=mybir.AxisListType.X, op=ALU.add), reads=[ltb], writes=[lsb])
                S.op("act", lambda e: e.activation(out=ls[:, 0:2], in_=ls[:, 0:2], func=AF.Exp), reads=[lsb], writes=[lsb])
                lam_init = 0.8 - 0.6 * math.exp(-0.3 * L)
                S.op("dve", lambda e: e.tensor_tensor(out=ls[:, 2:3], in0=ls[:, 1:2], in1=ls[:, 0:1], op=ALU.subtract), reads=[lsb], writes=[lsb])
                S.op("dve", lambda e: e.tensor_scalar(out=ls[:, 2:3], in0=ls[:, 2:3], scalar1=-lam_init, scalar2=None, op0=ALU.add), reads=[lsb], writes=[lsb])
                sg = self.sb(es, "sg", [128, 1], F32); sgb = Buf(sg)
                S.dma("sp", sg[:, :], self.diff_subln, writes=[sgb])
                S.op("dve", lambda e: e.tensor_scalar(out=sg[:, :], in0=sg[:, :], scalar1=1.0 - lam_init, scalar2=None, op0=ALU.mult), reads=[sgb], writes=[sgb])
                o1 = [self.sb(es, f"o1{i}", [128, 512], F32) for i in range(2)]; o1b = [Buf(t) for t in o1]
                od = [self.sb(es, f"od{i}", [128, 512], F32) for i in range(2)]; odb = [Buf(t) for t in od]
                sqd = [self.sb(es, f"sqd{i}", [128, 512], BF16) for i in range(2)]; sqdb = [Buf(t) for t in sqd]

            if kind in (0, 2):
                groups = [([h], h, [(h, 0)]) for h in range(16)]
            elif kind == 1:
                groups = [([g], g, [(4 * g + i, 0) for i in range(4)]) for g in range(4)]
            else:
                groups = [([2 * g, 2 * g + 1], g, [(2 * g, 0), (2 * g + 1, 1)]) for g in range(8)]

            qblocks = []
            for qi in range(8):
                tiles = []
                pat = 0
                if kind == 0:
                    r0 = 8 * qi
                    kr0 = min(max(r0 - 4, 0), 52)
                    pat = 0 if qi == 0 else (2 if qi == 7 else 1)
                    tiles = [(kr0 // 2 + j, "bias", j) for j in range(6)]
                elif kind == 1:
                    for j in range(6):
                        kt = 4 * qi - 1 + j
                        if 0 <= kt < 32:
                            tiles.append((kt, "mask", j))
                else:
                    tiles = [(kt, "plain", 0) for kt in range(32)]
                tiles += [(32, "plain", 0), (33, "plain", 0)]
                qblocks.append((qi * 512, 512, tiles, pat))
            if need_ctx:
                qblocks.append((SEQ, NCTX, [(32, "plain", 0), (33, "plain", 0)], -1))

            aug = (dv == 64)
            if aug:
                for i in range(2):
                    S.op("pool", lambda e, i=i: e.memset(V[i][:, :, 64:128], 1.0), writes=[Vb[i]])

            def load_group(gi):
                ks, vh, _ = groups[gi]
                i = gi % 2
                for j, kh in enumerate(ks):
                    S.dma("sp", kT[i][j][0:64, :], self.k_s[kh, 0:64, :], writes=[kTb[i][j]])
                    if kind == 2:
                        S.dma("sp", kT[i][j][64:96, :], self.kr_s[:, :], writes=[kTb[i][j]])
                if aug:
                    S.dma("sp", V[i][:, :, 0:64], self.v_s[vh], writes=[Vb[i]])
                else:
                    S.dma("sp", V[i][:, :, :], self.v_s2[vh], writes=[Vb[i]])

            load_group(0)
            for gi, (ks, vh, subs) in enumerate(groups):
                if gi + 1 < len(groups):
                    load_group(gi + 1)
                gb = gi % 2
                items = []
                for (q0, nq, tiles, pat) in qblocks:
                    for si, (qh, kvi) in enumerate(subs):
                        items.append(dict(q0=q0, nq=nq, tiles=tiles, pat=pat, si=si, qh=qh, kvi=kvi))
                work = [(ii, ti) for ii, it in enumerate(items) for ti in range(len(it["tiles"]))]
                st = dict(pat=None, bi=None, o1i=None)

                def prefetch(ii):
                    it = items[ii]
                    if kind == 0 and it["pat"] >= 0 and it["pat"] != st["pat"]:
                        st["pat"] = it["pat"]
                        st["bi"] = self.nxt("bt", 2)
                        S.dma("sp", bt[st["bi"]][:, :, :], self.na_bias[it["qh"], it["pat"]], writes=[btb[st["bi"]]])
                    it["bi"] = st["bi"]
                    it["qi"] = self.nxt("qt", 3)
                    S.dma("sp", qt[it["qi"]][0:dq, :it["nq"]], self.q_s[it["qh"], 0:dq, it["q0"]:it["q0"] + it["nq"]], writes=[qtb[it["qi"]]])

                def stageA(ii, ti):
                    it = items[ii]
                    nq = it["nq"]
                    if ti == 0:
                        if ii == 0:
                            prefetch(0)
                        if ii + 1 < len(items):
                            prefetch(ii + 1)
                        it["ni"] = 3 + self.nxt("p2n", 2)
                        it["di"] = 5 + self.nxt("p2d", 2)
                        it["pis"] = {}
                        it["pps"] = {}
                    kt, mode, ref = it["tiles"][ti]
                    qi_, bi_, kvi = it["qi"], it["bi"], it["kvi"]
                    si_ = sbanks[self.nxt("p2s", len(sbanks))]
                    sp_, spb = self.ps[si_], self.psb[si_]
                    S.op("pe", lambda e: e.matmul(
                        sp_[:, :nq], lhsT=kT[gb][kvi][:, kt * 128:(kt + 1) * 128], rhs=qt[qi_][:, :nq], start=True, stop=True),
                        reads=[kTb[gb][kvi], qtb[qi_]], writes=[spb])
                    pi = self.nxt("pt", NPT)
                    it["pis"][ti] = pi
                    if mode == "plain":
                        S.op("act", lambda e: e.activation(out=pt[pi][:, :nq], in_=sp_[:, :nq], func=AF.Exp, scale=scale),
                             reads=[spb], writes=[ptb[pi]])
                    elif mode == "mask":
                        zi = self.nxt("p0", 3)
                        S.op("act", lambda e: e.activation(out=p0[zi][:, :nq], in_=sp_[:, :nq], func=AF.Exp, scale=scale),
                             reads=[spb], writes=[p0b[zi]])
                        S.op("dve", lambda e: e.tensor_tensor(out=pt[pi][:, :nq], in0=p0[zi][:, :nq], in1=mk[:, ref, :nq], op=ALU.mult),
                             reads=[p0b[zi], mkb], writes=[ptb[pi]])
                    else:
                        zi = self.nxt("tb", 3)
                        S.op("dve", lambda e: e.scalar_tensor_tensor(
                            out=tb[zi][:, :nq], in0=sp_[:, :nq], scalar=scale, in1=bt[bi_][:, ref, :nq], op0=ALU.mult, op1=ALU.add),
                            reads=[spb, btb[bi_]], writes=[tbb[zi]])
                        S.op("act", lambda e: e.activation(out=pt[pi][:, :nq], in_=tb[zi][:, :nq], func=AF.Exp),
                             reads=[tbb[zi]], writes=[ptb[pi]])

                def pairsum(ii, ti):
                    it = items[ii]
                    nq = it["nq"]
                    if aug or ti % 2 == 0:
                        return
                    pa, pb_ = it["pis"][ti - 1], it["pis"][ti]
                    pj = self.nxt("pp", 4)
                    it["pps"][ti] = pj
                    S.op("dve", lambda e: e.tensor_tensor(out=pp[pj][:, :nq], in0=pt[pa][:, :nq], in1=pt[pb_][:, :nq], op=ALU.add),
                         reads=[ptb[pa], ptb[pb_]], writes=[ppb[pj]])

                def stageB(ii, ti):
                    it = items[ii]
                    nq, qh, si = it["nq"], it["qh"], it["si"]
                    q0 = it["q0"]
                    kt, mode, ref = it["tiles"][ti]
                    pi = it["pis"][ti]
                    ni, di = it["ni"], it["di"]
                    num, numb, den, denb = self.ps[ni], self.psb[ni], self.ps[di], self.psb[di]
                    st_, sp2 = (ti == 0), (ti == len(it["tiles"]) - 1)
                    S.op("pe", lambda e: e.matmul(num[:, :nq], lhsT=V[gb][:, kt, :], rhs=pt[pi][:, :nq], start=st_, stop=sp2),
                         reads=[Vb[gb], ptb[pi]], writes=[numb])
                    if (not aug) and ti % 2 == 1:
                        pj = it["pps"][ti]
                        S.op("pe", lambda e: e.matmul(den[:, :nq], lhsT=ones_b, rhs=pp[pj][:, :nq], start=(ti == 1), stop=sp2),
                             reads=[self.cstb_buf, ppb[pj]], writes=[denb])
                    if not sp2:
                        return None
                    return lambda: finish(it, num, numb, den, denb)

                def finish(it, num, numb, den, denb):
                    nq, qh, si, q0 = it["nq"], it["qh"], it["si"], it["q0"]
                    ri = self.nxt("rd", 2)
                    if aug:
                        if kind == 1:
                            S.op("dve", lambda e: e.tensor_scalar(out=rd[ri][0:64, :nq], in0=num[64:128, :nq], scalar1=snk[64:128, qh:qh + 1], scalar2=None, op0=ALU.add),
                                 reads=[numb, snkb], writes=[rdb[ri]])
                            S.op("dve", lambda e: e.reciprocal(out=rd[ri][0:64, :nq], in_=rd[ri][0:64, :nq]), reads=[rdb[ri]], writes=[rdb[ri]])
                        else:
                            S.op("dve", lambda e: e.reciprocal(out=rd[ri][0:64, :nq], in_=num[64:128, :nq]), reads=[numb], writes=[rdb[ri]])
                        oi = self.nxt("ot", 2)
                        S.op("dve", lambda e: e.tensor_tensor(out=ot[oi][0:64, :nq], in0=num[0:64, :nq], in1=rd[ri][0:64, :nq], op=ALU.mult),
                             reads=[numb, rdb[ri]], writes=[otb[oi]])
                        S.dma("sp", self.at_s[qh * 64:(qh + 1) * 64, q0:q0 + nq], ot[oi][0:64, :nq], reads=[otb[oi]])
                        return
                    S.op("dve", lambda e: e.reciprocal(out=rd[ri][:, :nq], in_=den[:, :nq]), reads=[denb], writes=[rdb[ri]])
                    if si == 0:
                        o1i = self.nxt("o1", 2)
                        st["o1i"] = o1i
                        S.op("dve", lambda e: e.tensor_tensor(out=o1[o1i][:, :nq], in0=num[:, :nq], in1=rd[ri][:, :nq], op=ALU.mult),
                             reads=[numb, rdb[ri]], writes=[o1b[o1i]])
                    else:
                        o1i = st["o1i"]
                        odi = self.nxt("od", 2)
                        S.op("dve", lambda e: e.tensor_tensor(out=od[odi][:, :nq], in0=num[:, :nq], in1=rd[ri][:, :nq], op=ALU.mult),
                             reads=[numb, rdb[ri]], writes=[odb[odi]])
                        S.op("dve", lambda e: e.scalar_tensor_tensor(
                            out=od[odi][:, :nq], in0=od[odi][:, :nq], scalar=ls[:, 2:3], in1=o1[o1i][:, :nq], op0=ALU.mult, op1=ALU.add),
                            reads=[odb[odi], o1b[o1i], lsb], writes=[odb[odi]])
                        qd = self.nxt("sqd", 2)
                        S.op("act", lambda e: e.activation(out=sqd[qd][:, :nq], in_=od[odi][:, :nq], func=AF.Square),
                             reads=[odb[odi]], writes=[sqdb[qd]])
                        pm, pmb = self.ps[7], self.psb[7]
                        S.op("pe", lambda e: e.matmul(pm[:, :nq], lhsT=ones_b, rhs=sqd[qd][:, :nq], start=True, stop=True),
                             reads=[sqdb[qd], self.cstb_buf], writes=[pmb])
                        r2 = self.nxt("rd", 2)
                        S.op("act", lambda e: e.activation(out=rd[r2][:, :nq], in_=pm[:, :nq], func=AF.Sqrt, bias=self.eps_col[:, 0:1], scale=1.0 / 128),
                             reads=[pmb, self.eps_buf], writes=[rdb[r2]])
                        S.op("dve", lambda e: e.reciprocal(out=rd[r2][:, :nq], in_=rd[r2][:, :nq]), reads=[rdb[r2]], writes=[rdb[r2]])
                        oi = self.nxt("ot", 2)
                        S.op("dve", lambda e: e.scalar_tensor_tensor(
                            out=ot[oi][:, :nq], in0=od[odi][:, :nq], scalar=sg[:, 0:1], in1=rd[r2][:, :nq], op0=ALU.mult, op1=ALU.mult),
                            reads=[odb[odi], rdb[r2], sgb], writes=[otb[oi]])
                        S.dma("sp", self.at_s[vh * 128:(vh + 1) * 128, q0:q0 + nq], ot[oi][:, :nq], reads=[otb[oi]])

                pending = []
                for idx in range(len(work) + LOOK):
                    if idx < len(work):
                        stageA(*work[idx])
                        pairsum(*work[idx])
                    while pending and pending[0][0] <= idx:
                        pending.pop(0)[1]()
                    if idx - LOOK >= 0:
                        fin = stageB(*work[idx - LOOK])
                        if fin is not None:
                            pending.append((idx + 3, fin))
                for _, fin in pending:
                    fin()

    def phase3(self, L, x_src, x_dst, need_ctx, final):
        nc, S = self.nc, self.S
        kind = L % 4
        ident = self.cst_f[:, 3, :]
        with ExitStack() as es0:
            GT = self.sb(es0, "GT", [32, T], BF16); GTb = Buf(GT)
            with ExitStack() as es, nc.named_scope(f"L{L}_p3a"):
                Wo = self.sb(es, "wo", [128, 8, D], BF16); Wob = [Buf(Wo) for _ in range(8)]
                wsrc = self.wo[kind].rearrange("(kc p) n -> p kc n", p=128)
                for kc in range(8):
                    S.dma("pool", Wo[:, kc, :], wsrc[:, kc, :], writes=[Wob[kc]])
                Wr = self.sb(es, "wr", [128, 8, 36], F32); Wrb = Buf(Wr)
                S.dma("sp", Wr[:, :, :], self.wr[L].rearrange("(kc p) n -> p kc n", p=128), writes=[Wrb])
                brt = self.sb(es, "brt", [128, 36], F32); brb = Buf(brt)
                S.dma("sp", brt[:, :], self.br[L], writes=[brb])
                xin = [self.sb(es, f"xin{i}", [128, 8, 512], F32) for i in range(2)]; xinb = [Buf(t) for t in xin]
                at = [self.sb(es, f"at{i}", [128, 8, 512], BF16) for i in range(2)]; atb = [Buf(t) for t in at]
                h2f = self.sb(es, "h2f", [128, 8, 512], F32); h2fb = Buf(h2f)
                h2b = [self.sb(es, f"h2b{i}", [128, 8, 512], BF16) for i in range(2)]; h2bb = [Buf(t) for t in h2b]
                rs = self.sb(es, "rs", [128, 512], F32); rsb = Buf(rs)
                sq = [self.sb(es, f"sq{i}", [128, 512], BF16) for i in range(2)]; sqb = [Buf(t) for t in sq]
                tmp = [self.sb(es, f"tmp{i}", [128, 512], F32) for i in range(2)]; tmpb = [Buf(t) for t in tmp]
                rt = self.sb(es, "rt", [128, 4, 256], F32); rtb = Buf(rt)
                blocks = _token_blocks(need_ctx)
                xsrc = x_src.rearrange("(kc p) t -> p kc t", p=128)
                xdst = x_dst.rearrange("(kc p) t -> p kc t", p=128)
                asrc = self.at_s.rearrange("(kc p) t -> p kc t", p=128)
                hdst = self.h2_s.rearrange("(kc p) t -> p kc t", p=128)

                def load(bi):
                    t0, n = blocks[bi]
                    i = bi % 2
                    S.dma("sp", xin[i][:, :, :n], xsrc[:, :, t0:t0 + n], writes=[xinb[i]])
                    S.dma("sp", at[i][:, :, :n], asrc[:, :, t0:t0 + n], writes=[atb[i]])

                def stX(bi):
                    t0, n = blocks[bi]
                    i = bi % 2
                    tq = 0 if t0 < SEQ else 1
                    for oc in range(8):
                        pb = self.nxt("p3y", 2)
                        py, pyb = self.ps[pb], self.psb[pb]
                        for kc in range(8):
                            S.op("pe", lambda e, kc=kc: e.matmul(
                                py[:, :n], lhsT=Wo[:, kc, oc * 128:(oc + 1) * 128], rhs=at[i][:, kc, :n], start=(kc == 0), stop=(kc == 7)),
                                reads=[Wob[kc], atb[i]], writes=[pyb])
                        S.op("dve", lambda e: e.scalar_tensor_tensor(
                            out=xin[i][:, oc, :n], in0=py[:, :n], scalar=self.modcol(L, 2, oc, tq), in1=xin[i][:, oc, :n], op0=ALU.mult, op1=ALU.add),
                            reads=[pyb, xinb[i], self.mod_buf], writes=[xinb[i]])
                    S.dma("sp", xdst[:, :, t0:t0 + n], xin[i][:, :, :n], reads=[xinb[i]])

                def stY(bi):
                    t0, n = blocks[bi]
                    i = bi % 2
                    tq = 0 if t0 < SEQ else 1
                    hb = h2b[i]
                    self.norm_mod(L, 1, xin[i], xinb[i], n, tq, rs, rsb, sq, sqb, tmp, tmpb,
                                  [(h2f, h2fb, "act"), (hb, h2bb[i], "pool")], 7)
                    S.dma("sp", hdst[:, :, t0:t0 + n], hb[:, :, :n], reads=[h2bb[i]])
                    nt = n // 128
                    pr, prb = self.ps[2], self.psb[2]
                    for tt in range(nt):
                        for kc in range(8):
                            S.op("pe", lambda e, kc=kc, tt=tt: e.matmul(
                                pr[:, tt * 36:(tt + 1) * 36], lhsT=h2f[:, kc, tt * 128:(tt + 1) * 128], rhs=Wr[:, kc, :], start=(kc == 0), stop=(kc == 7)),
                                reads=[h2fb, Wrb], writes=[prb])
                    self.topk(pr, prb, brt, brb, rt, rtb, nt)
                    pt_, ptb_ = self.ps[3], self.psb[3]
                    for tt in range(nt):
                        S.op("pe", lambda e, tt=tt: e.transpose(pt_[0:32, tt * 128:(tt + 1) * 128], rt[:, tt, 64:96], ident),
                             reads=[rtb, self.cst_buf], writes=[ptb_])
                    S.op("act", lambda e: e.activation(out=GT[:, t0:t0 + n], in_=pt_[0:32, 0:n], func=AF.Copy),
                         reads=[ptb_], writes=[GTb])

                nbk = len(blocks)
                load(0)
                if nbk > 1:
                    load(1)
                stX(0)
                for bi in range(nbk):
                    if bi + 1 < nbk:
                        stX(bi + 1)
                    stY(bi)
                    if bi + 2 < nbk:
                        load(bi + 2)
            S.barrier()
            with ExitStack() as es, nc.named_scope(f"L{L}_p3b"):
                sel = self.sb(es, "sel", [32, NE, 128], BF16); selb = Buf(sel)
                S.dma("pool", sel[:, :, :], self.sel_in, writes=[selb])
                SBW = 2048 + (NCTX if need_ctx else 0)
                h2 = self.sb(es, "h2", [128, 8, SBW], BF16); h2bf = Buf(h2)
                acc = self.sb(es, "acc", [128, 8, SBW], F32); accb = [Buf(acc) for _ in range(5)]
                NW = 3
                wgu = [self.sb(es, f"wgu{i}", [128, 8, 512], BF16) for i in range(NW)]; wgub = [Buf(t) for t in wgu]
                wdn = [self.sb(es, f"wdn{i}", [128, 2, D], BF16) for i in range(NW)]; wdnb = [Buf(t) for t in wdn]
                x1 = self.sb(es, "x1", [128, 8, 512], F32); x1b = Buf(x1)
                sgt = [self.sb(es, f"sgt{i}", [128, 512], F32) for i in range(2)]; sgtb = [Buf(t) for t in sgt]
                tt_ = [self.sb(es, f"tt{i}", [128, 512], F32) for i in range(2)]; ttb = [Buf(t) for t in tt_]
                aT = [self.sb(es, f"aT{i}", [128, 512], BF16) for i in range(4)]; aTb = [Buf(t) for t in aT]
                sbs = [(0, 2048), (2048, SBW)]
                hsrc = self.h2_s.rearrange("(kc p) t -> p kc t", p=128)
                xdst = x_dst.rearrange("(kc p) t -> p kc t", p=128)
                outd = self.out.rearrange("(kc p) t -> p kc t", p=128)

                def loadw(e):
                    i = e % NW
                    S.dma("pool", wgu[i][:, :, :], self.w_gu[L][e].rearrange("(kc p) n -> p kc n", p=128), writes=[wgub[i]])
                    S.dma("pool", wdn[i][:, :, :], self.w_dn[L][e].rearrange("(hc p) n -> p hc n", p=128), writes=[wdnb[i]])

                for (s0, sn) in sbs:
                    S.dma("sp", h2[:, :, :sn], hsrc[:, :, s0:s0 + sn], writes=[h2bf])
                    nb = (sn + 511) // 512
                    units = [(e, b) for e in range(NE) for b in range(nb)]

                    def gu_group(e, b, gidx):
                        wi = e % NW
                        b0 = b * 512
                        n = min(512, sn - b0)
                        j = (0, 2, 1, 3)[gidx]
                        pgj, pgbj = self.ps[j], self.psb[j]
                        for kc in range(8):
                            S.op("pe", lambda e_: e_.matmul(
                                pgj[:, :n], lhsT=wgu[wi][:, kc, j * 128:(j + 1) * 128], rhs=h2[:, kc, b0:b0 + n],
                                start=(kc == 0), stop=(kc == 7)), reads=[wgub[wi], h2bf], writes=[pgbj])

                    def chain(e, b):
                        b0 = b * 512
                        n = min(512, sn - b0)
                        pg = [self.ps[j] for j in range(4)]
                        pgb = [self.psb[j] for j in range(4)]
                        pgate, pgateb = self.ps[4], self.psb[4]
                        S.op("pe", lambda e_: e_.matmul(pgate[:, :n], lhsT=sel[:, e, :], rhs=GT[:, s0 + b0:s0 + b0 + n], start=True, stop=True),
                             reads=[selb, GTb], writes=[pgateb])
                        ai = []
                        for hc in range(2):
                            s_ = self.nxt("sgt", 2)
                            S.op("act", lambda e_: e_.activation(out=sgt[s_][:, :n], in_=pg[hc][:, :n], func=AF.Silu),
                                 reads=[pgb[hc]], writes=[sgtb[s_]])
                            t_ = self.nxt("tt", 2)
                            S.op("dve", lambda e_: e_.tensor_tensor(out=tt_[t_][:, :n], in0=pg[2 + hc][:, :n], in1=sgt[s_][:, :n], op=ALU.mult),
                                 reads=[pgb[2 + hc], sgtb[s_]], writes=[ttb[t_]])
                            a_ = self.nxt("aT", 4)
                            S.op("dve", lambda e_: e_.tensor_tensor(out=aT[a_][:, :n], in0=pgate[:, :n], in1=tt_[t_][:, :n], op=ALU.mult),
                                 reads=[pgateb, ttb[t_]], writes=[aTb[a_]])
                            ai.append(a_)
                        return ai

                    def down_part(e, b, ai, ocs):
                        wi = e % NW
                        b0 = b * 512
                        n = min(512, sn - b0)
                        for oc in ocs:
                            pdi = 5 + self.nxt("p3d", 3)
                            pd, pdb = self.ps[pdi], self.psb[pdi]
                            for hc in range(2):
                                S.op("pe", lambda e_: e_.matmul(
                                    pd[:, :n], lhsT=wdn[wi][:, hc, oc * 128:(oc + 1) * 128], rhs=aT[ai[hc]][:, :n],
                                    start=(hc == 0), stop=(hc == 1)), reads=[wdnb[wi], aTb[ai[hc]]], writes=[pdb])
                            if e == 0:
                                S.op("dve", lambda e_: e_.tensor_copy(out=acc[:, oc, b0:b0 + n], in_=pd[:, :n]),
                                     reads=[pdb], writes=[accb[b]])
                            else:
                                S.op("dve", lambda e_: e_.tensor_tensor(out=acc[:, oc, b0:b0 + n], in0=pd[:, :n], in1=acc[:, oc, b0:b0 + n], op=ALU.add),
                                     reads=[pdb, accb[b]], writes=[accb[b]])

                    loadw(0)
                    loadw(1)
                    loadw(2)
                    prev = None
                    for (e, b) in units:
                        for gidx in range(4):
                            gu_group(e, b, gidx)
                            if prev is not None:
                                down_part(prev[0], prev[1], prev[2], (2 * gidx, 2 * gidx + 1))
                        if prev is not None and prev[0] != e and prev[0] + NW < NE:
                            loadw(prev[0] + NW)
                        ai = chain(e, b)
                        prev = (e, b, ai)
                    down_part(prev[0], prev[1], prev[2], range(8))
                    for b in range(nb):
                        b0 = b * 512
                        n = min(512, sn - b0)
                        tq = 0 if s0 + b0 < SEQ else 1
                        S.dma("sp", x1[:, :, :n], xdst[:, :, s0 + b0:s0 + b0 + n], writes=[x1b])
                        for oc in range(8):
                            S.op("dve", lambda e_, oc=oc: e_.scalar_tensor_tensor(
                                out=x1[:, oc, :n], in0=acc[:, oc, b0:b0 + n], scalar=self.modcol(L, 5, oc, tq), in1=x1[:, oc, :n], op0=ALU.mult, op1=ALU.add),
                                reads=[accb[b], x1b, self.mod_buf], writes=[x1b])
                        if final:
                            if self.last:
                                if s0 + b0 < SEQ:
                                    S.dma("sp", outd[:, :, s0 + b0:s0 + b0 + n], x1[:, :, :n], reads=[x1b])
                            else:
                                S.dma("sp", outd[:, :, s0 + b0:s0 + b0 + n], x1[:, :, :n], reads=[x1b])
                        else:
                            S.dma("sp", xdst[:, :, s0 + b0:s0 + b0 + n], x1[:, :, :n], reads=[x1b])

    def topk(self, pr, prb, brt, brb, rt, rtb, nt):
        S = self.S
        X = mybir.AxisListType.X

        def op(fn, extra=()):
            S.op("dve", fn, reads=[rtb] + list(extra), writes=[rtb])

        def bc(ap, w):
            return ap.unsqueeze(2).to_broadcast([128, nt, w])

        Lg = rt[:, 0:nt, 0:36]
        S.op("dve", lambda e: e.tensor_tensor(out=Lg, in0=pr[:, 0:nt * 36].rearrange("p (t c) -> p t c", c=36),
                                              in1=brt[:, :].unsqueeze(1).to_broadcast([128, nt, 36]), op=ALU.add),
             reads=[prb, brb], writes=[rtb])
        lg, le = rt[:, 0:nt, 0:4], rt[:, 0:nt, 4:36]
        gmax, gsum, gw = rt[:, 0:nt, 40], rt[:, 0:nt, 42], rt[:, 0:nt, 43]
        m1, m2, dd, g1, g2 = rt[:, 0:nt, 52], rt[:, 0:nt, 53], rt[:, 0:nt, 54], rt[:, 0:nt, 55], rt[:, 0:nt, 56]
        gexp, pen = rt[:, 0:nt, 44:48], rt[:, 0:nt, 48:52]
        lem, mask1, mask2, lem2 = rt[:, 0:nt, 100:132], rt[:, 0:nt, 132:164], rt[:, 0:nt, 164:196], rt[:, 0:nt, 196:228]
        G = rt[:, 0:nt, 64:96]
        op(lambda e: e.tensor_reduce(out=gmax, in_=lg, axis=X, op=ALU.max))
        op(lambda e: e.tensor_tensor(out=gexp, in0=lg, in1=bc(gmax, 4), op=ALU.subtract))
        S.op("act", lambda e: e.activation(out=gexp, in_=gexp, func=AF.Exp), reads=[rtb], writes=[rtb])
        op(lambda e: e.tensor_reduce(out=gsum, in_=gexp, axis=X, op=ALU.add))
        op(lambda e: e.reciprocal(out=gw, in_=gsum))
        op(lambda e: e.tensor_tensor(out=pen, in0=lg, in1=bc(gmax, 4), op=ALU.is_equal))
        op(lambda e: e.tensor_scalar(out=pen, in0=pen, scalar1=-1.0, scalar2=30000.0, op0=ALU.add, op1=ALU.mult))
        op(lambda e: e.tensor_tensor(out=lem.rearrange("p t (g x) -> p t g x", g=4), in0=le.rearrange("p t (g x) -> p t g x", g=4),
                                     in1=pen.unsqueeze(3).to_broadcast([128, nt, 4, 8]), op=ALU.add))
        op(lambda e: e.tensor_reduce(out=m1, in_=lem, axis=X, op=ALU.max))
        op(lambda e: e.tensor_tensor(out=mask1, in0=lem, in1=bc(m1, 32), op=ALU.is_equal))
        op(lambda e: e.scalar_tensor_tensor(out=lem2, in0=mask1, scalar=-30000.0, in1=lem, op0=ALU.mult, op1=ALU.add))
        op(lambda e: e.tensor_reduce(out=m2, in_=lem2, axis=X, op=ALU.max))
        op(lambda e: e.tensor_tensor(out=mask2, in0=lem2, in1=bc(m2, 32), op=ALU.is_equal))
        op(lambda e: e.tensor_tensor(out=dd, in0=m2, in1=m1, op=ALU.subtract))
        S.op("act", lambda e: e.activation(out=dd, in_=dd, func=AF.Exp), reads=[rtb], writes=[rtb])
        op(lambda e: e.tensor_scalar(out=dd, in0=dd, scalar1=1.0, scalar2=None, op0=ALU.add))
        op(lambda e: e.reciprocal(out=dd, in_=dd))
        op(lambda e: e.tensor_tensor(out=g1, in0=dd, in1=gw, op=ALU.mult))
        op(lambda e: e.tensor_tensor(out=g2, in0=gw, in1=g1, op=ALU.subtract))
        op(lambda e: e.tensor_tensor(out=G, in0=mask1, in1=bc(g1, 32), op=ALU.mult))
        op(lambda e: e.tensor_tensor(out=mask2, in0=mask2, in1=bc(g2, 32), op=ALU.mult))
        op(lambda e: e.tensor_tensor(out=G, in0=G, in1=mask2, op=ALU.add))


def _rope_tables():
    t = np.arange(SEQ)
    rows = (t // 64).astype(np.float32)
    cols = (t % 64).astype(np.float32)

    def ang(rot_dim):
        nf = rot_dim // 4
        inv = (np.float32(10000.0) ** (-np.arange(nf, dtype=np.float32) / np.float32(nf))).astype(np.float32)
        return np.concatenate([rows[:, None] * inv, cols[:, None] * inv], axis=-1).astype(np.float32)

    a64 = ang(64)
    r64 = np.zeros((2, 128, T), np.float32)
    r64[0, :, SEQ:] = 1.0
    idx = (np.arange(128) % 64) % 32
    r64[0, :, :SEQ] = np.cos(a64).T[idx]
    r64[1, :, :SEQ] = np.sin(a64).T[idx]
    a32 = ang(32)
    rm = np.zeros((2, 96, T), np.float32)
    rm[0] = 1.0
    idx = np.arange(32) % 16
    rm[0, 64:96, :SEQ] = np.cos(a32).T[idx]
    rm[1, 64:96, :SEQ] = np.sin(a32).T[idx]
    return r64, rm


def _consts():
    c = np.zeros((128, 8, 128), np.float32)
    c[:, 0, :] = 1.0
    for b in (0, 64):
        c[b:b + 64, 1, b:b + 64] = 1.0 / 64
        for m in range(64):
            if m < 32:
                c[b + m + 32, 2, b + m] = -1.0
            else:
                c[b + m - 32, 2, b + m] = 1.0
    c[:, 3, :] = np.eye(128, dtype=np.float32)
    c[:, 4, :] = 1.0 / 128
    c[0:64, 5, 0:64] = 1.0 / 64
    c[64:96, 5, 64:96] = 1.0 / 32
    for m in range(32):
        if m < 16:
            c[64 + m + 16, 6, 64 + m] = -1.0
            c[m + 16, 7, m] = -1.0
        else:
            c[64 + m - 16, 6, 64 + m] = 1.0
            c[m - 16, 7, m] = 1.0
    return c


def _na_bias(rpb):
    out = np.full((16, 3, 128, 6, 512), NEG, np.float32)
    rl = np.arange(8)[:, None].repeat(64, 1).reshape(-1)
    c = np.arange(64)[None, :].repeat(8, 0).reshape(-1)
    krl = np.arange(2)[:, None].repeat(64, 1).reshape(-1)
    kc = np.arange(64)[None, :].repeat(2, 0).reshape(-1)
    for pi, r0 in enumerate((0, 8, 56)):
        kr0 = min(max(r0 - 4, 0), 52)
        r = r0 + rl
        row0 = np.clip(r - 4, 0, 56)
        col0 = np.clip(c - 8, 0, 48)
        for j in range(6):
            kr = kr0 + 2 * j + krl
            vr = (kr[:, None] >= row0[None, :]) & (kr[:, None] < row0[None, :] + 8)
            vc = (kc[:, None] >= col0[None, :]) & (kc[:, None] < col0[None, :] + 16)
            valid = vr & vc
            idx = (kr[:, None] - r[None, :] + 7) * 31 + (kc[:, None] - c[None, :] + 15)
            idx = np.where(valid, idx, 0)
            g = rpb[:, idx]
            out[:, pi, :, j, :] = np.where(valid[None], g, np.float32(NEG))
    return out


def _sw_mask():
    kl = np.arange(128)[:, None, None]
    j = np.arange(6)[None, :, None]
    ql = np.arange(512)[None, None, :]
    return (np.abs(ql - (kl + 128 * (j - 1))) <= 128).astype(np.float32)


def _col(v):
    return np.ascontiguousarray(v.reshape(-1, 128).T)


def _prep_shared(inp, layers):
    f = lambda a: np.ascontiguousarray(np.asarray(a, dtype=np.float32))
    kinds = set(l % 4 for l in layers)
    d = {}
    for l in layers:
        d[f"ada_w{l}"] = f(inp["ada_w"])[l]
        d[f"w_gu{l}"] = f(inp["moe_w_gate_up"])[l]
        d[f"w_dn{l}"] = f(inp["moe_w_down"])[l]
    ab = np.stack([_col(f(inp["ada_b"])[l]) for l in range(DEPTH)])
    d["ada_b"] = np.ascontiguousarray(np.repeat(ab[..., None], 2, axis=-1))
    ng = np.stack([np.stack([_col(f(inp["norm_g"])[l, i]) for i in range(2)]) for l in range(DEPTH)])
    d["norm_g"] = np.ascontiguousarray(np.repeat(ng[..., None], 2, axis=-1))
    d["wr"] = np.ascontiguousarray(np.concatenate([f(inp["moe_w_router_group"]), f(inp["moe_w_router_expert"])], axis=-1))
    br = np.concatenate([f(inp["moe_b_router_group"]), f(inp["moe_b_router_expert"])], axis=-1)
    d["br"] = np.ascontiguousarray(np.broadcast_to(br[:, None, :], (DEPTH, 128, 36)))
    d["consts"] = _consts()
    sel = np.zeros((32, NE, 128), np.float32)
    for e in range(NE):
        sel[e, e, :] = 1.0
    d["sel"] = sel
    r64, rm = _rope_tables()
    if 0 in kinds:
        d["na_w_qkv"] = f(inp["na_w_qkv"])[0]
        d["na_w_o"] = f(inp["na_w_o"])[0]
        d["na_qk_g"] = np.ascontiguousarray(np.stack([np.tile(f(inp["na_q_norm"])[0], 2), np.tile(f(inp["na_k_norm"])[0], 2)], axis=1))
        d["na_bias"] = _na_bias(f(inp["na_rpb"])[0])
    if 1 in kinds:
        d["sw_w_qkv"] = f(inp["sw_w_qkv"])[0]
        d["sw_w_o"] = f(inp["sw_w_o"])[0]
        d["sw_qk_g"] = np.ascontiguousarray(np.stack([np.tile(f(inp["sw_q_norm"])[0], 2), np.tile(f(inp["sw_k_norm"])[0], 2)], axis=1))
        d["sw_sink"] = np.ascontiguousarray(np.broadcast_to(f(inp["sw_sink"])[0][None, :], (128, 16)))
        d["sw_mask"] = _sw_mask()
    if 1 in kinds or 3 in kinds:
        d["rope64"] = r64
    if 2 in kinds:
        d["mla_w_dqkv"] = f(inp["mla_w_dqkv"])[0]
        d["mla_w_uq"] = f(inp["mla_w_uq"])[0]
        d["mla_w_ukv"] = f(inp["mla_w_ukv"])[0]
        d["mla_w_o"] = f(inp["mla_w_o"])[0]
        mg = np.zeros((128, 8), np.float32)
        mg[:, 0:3] = _col(f(inp["mla_q_a_norm"])[0])
        mg[:, 3:5] = _col(f(inp["mla_kv_a_norm"])[0])
        mg[:96, 5] = f(inp["mla_q_norm"])[0]
        mg[:, 6] = np.tile(f(inp["mla_k_norm"])[0][:64], 2)
        mg[:32, 7] = f(inp["mla_k_norm"])[0][64:96]
        d["mla_g"] = mg
        d["rope_mla"] = rm
    if 3 in kinds:
        d["diff_w_qkv"] = f(inp["diff_w_qkv"])[0]
        d["diff_w_o"] = f(inp["diff_w_o"])[0]
        d["diff_qk_g"] = np.ascontiguousarray(np.stack([np.tile(f(inp["diff_q_norm"])[0], 2), np.tile(f(inp["diff_k_norm"])[0], 2)], axis=1))
        d["diff_lam"] = np.ascontiguousarray(np.broadcast_to(f(inp["diff_lambda"])[0][None], (128, 4, 64)))
        d["diff_subln"] = np.ascontiguousarray(f(inp["diff_subln"])[0][:, None])
    return d


def _cc(inp, b):
    c = np.asarray(inp["c"], np.float32)[b]
    cx = np.asarray(inp["c_ctx"], np.float32)
    return np.ascontiguousarray(np.stack([_col(c), _col(cx)], axis=-1))


_CACHE = {}


def _get_builder(layers, last):
    key = (tuple(layers), last)
    if key not in _CACHE:
        _CACHE[key] = Builder(list(layers), layers[0] == 0, last)
    return _CACHE[key]


def run_layers(inp, layers, xT_list, cores):
    last = layers[-1] == DEPTH - 1
    B = _get_builder(layers, last)
    shared = _prep_shared(inp, layers)
    in_maps = []
    for ci, b in enumerate(cores):
        m = {"xT_in": np.ascontiguousarray(xT_list[ci]), "cc": _cc(inp, b)}
        for k in B.in_names:
            if k not in m:
                m[k] = shared[k]
        in_maps.append(m)
    res = run_bass_kernel_spmd(B.nc, in_maps, core_ids=list(range(len(cores))))
    name = "outT" if last else "xT_out"
    return [np.asarray(r[name]) for r in res.results]


FUSED = True


def kernel(**inp):
    x = np.asarray(inp["x"], np.float32)
    ctx = np.asarray(inp["ctx"], np.float32)
    nb = x.shape[0]
    xT = [np.ascontiguousarray(np.concatenate([x[b].T, ctx[b].T], axis=1)) for b in range(nb)]
    cores = list(range(nb))
    if FUSED:
        outs = run_layers(inp, [0, 1, 2, 3], xT, cores)
    else:
        cur = xT
        for L in range(DEPTH):
            cur = run_layers(inp, [L], cur, cores)
        outs = cur
    return np.ascontiguousarray(np.stack([o.T for o in outs], axis=0)).astype(np.float32)
```

```python
import math
from contextlib import ExitStack
import numpy as np
import concourse.bass as bass
import concourse.mybir as mybir
from concourse.bass_utils import run_bass_kernel_spmd

F32 = mybir.dt.float32
BF16 = mybir.dt.bfloat16
AF = mybir.ActivationFunctionType
ALU = mybir.AluOpType

D = 1024
SEQ = 4096
NCTX = 256
T = SEQ + NCTX
NKT = T // 128
DEPTH = 4
EPS = 1e-6
NEG = -30000.0
NE = 32


class Buf:
    __slots__ = ("ap", "w", "r")

    def __init__(self, ap):
        self.ap = ap
        self.w = {}
        self.r = {}


class Sched:
    NR = 8
    EPOCH = 1000000

    def __init__(self, nc):
        self.nc = nc
        self.E = {"pe": nc.tensor, "act": nc.scalar, "dve": nc.vector, "pool": nc.gpsimd, "sp": nc.sync}
        self.sems = {}
        self.ckey = {}
        self.ccnt = {}
        self.cep = {}
        for e in ("pe", "act", "dve", "pool"):
            self.cep[e] = 0
            self._new_epoch(e)
        self.seen = {e: {} for e in self.E}
        self.dkeys = {}
        self.dcnt = {}
        for q in ("sp", "pool", "act"):
            ks = []
            for i in range(self.NR):
                k = f"d_{q}_{i}"
                self.sems[k] = nc.alloc_semaphore(k)
                ks.append(k)
            self.dkeys[q] = ks
            self.dcnt[q] = 0
        self.dlast = {}
        self.n_inst = 0

    def _new_epoch(self, e):
        k = f"c_{e}_{self.cep[e]}"
        self.cep[e] += 1
        self.sems[k] = self.nc.alloc_semaphore(k)
        self.ckey[e] = k
        self.ccnt[e] = 0

    def _wait(self, e, k, v):
        if self.seen[e].get(k, 0) >= v:
            return
        self.E[e].wait_ge(self.sems[k], v)
        self.seen[e][k] = v

    def _collect(self, e, reads, writes, is_dma):
        own = f"c_{e}_"
        need = {}
        for b in reads:
            for k, v in b.w.items():
                if (not is_dma) and e == "pe" and k.startswith(own):
                    continue
                if need.get(k, 0) < v:
                    need[k] = v
        for b in writes:
            for src in (b.w, b.r):
                for k, v in src.items():
                    if (not is_dma) and k.startswith(own):
                        continue
                    if need.get(k, 0) < v:
                        need[k] = v
        for k, v in need.items():
            self._wait(e, k, v)

    def _commit(self, k, v, reads, writes):
        for b in reads:
            if b.r.get(k, 0) < v:
                b.r[k] = v
        for b in writes:
            if b.r:
                b.w = {k: v}
                b.r = {}
            else:
                b.w[k] = v

    def op(self, e, fn, reads=(), writes=()):
        self._collect(e, reads, writes, False)
        inst = fn(self.E[e])
        if self.ccnt[e] >= self.EPOCH:
            self._new_epoch(e)
        self.ccnt[e] += 1
        k, v = self.ckey[e], self.ccnt[e]
        inst.then_inc(self.sems[k], 1)
        self._commit(k, v, reads, writes)
        self.n_inst += 1

    def dma(self, q, out, in_, reads=(), writes=()):
        self._collect(q, reads, writes, True)
        i = self.dcnt[q]
        self.dcnt[q] += 1
        k = self.dkeys[q][i % self.NR]
        prev = 16 * (i // self.NR)
        if prev:
            self._wait(q, k, prev)
        inst = self.E[q].dma_start(out=out, in_=in_)
        inst.then_inc(self.sems[k], 16)
        v = prev + 16
        assert v < 60000
        self.dlast[k] = v
        self._commit(k, v, reads, writes)
        self.n_inst += 1

    def barrier(self):
        tick = {}
        for e in ("pe", "act", "dve", "pool"):
            if self.ccnt[e]:
                tick[self.ckey[e]] = self.ccnt[e]
        tick.update(self.dlast)
        for e in self.E:
            for k, v in tick.items():
                self._wait(e, k, v)


def _token_blocks(with_ctx=True):
    bl = [(i * 512, 512) for i in range(SEQ // 512)]
    if with_ctx:
        bl.append((SEQ, NCTX))
    return bl


class Builder:
    topk_eng = "dve"

    def __init__(self, layers, first, last):
        self.layers = layers
        self.first = first
        self.last = last
        nc = bass.Bass("TRN2", target_bir_lowering=False)
        self.nc = nc
        self.S = Sched(nc)
        self.rot = {}
        self._decl_dram()
        self._build()

    def din(self, name, shape, dt=F32, kinds=None):
        if kinds is not None and not (set(kinds) & set(l % 4 for l in self.layers)):
            return None
        if not hasattr(self, "in_names"):
            self.in_names = []
        self.in_names.append(name)
        return self.nc.dram_tensor(name, list(shape), dt, kind="ExternalInput").ap()

    def dscr(self, name, shape, dt):
        return self.nc.dram_tensor(name, list(shape), dt, kind="Internal").ap()

    def sb(self, es, name, shape, dt):
        self._uid = getattr(self, "_uid", 0) + 1
        t = es.enter_context(self.nc.sbuf_tensor(f"{name}_u{self._uid}", list(shape), dt))
        return t

    def nxt(self, key, n):
        i = self.rot.get(key, 0)
        self.rot[key] = i + 1
        return i % n

    def _decl_dram(self):
        nc = self.nc
        self.x_in = self.din("xT_in", [D, T])
        self.cc = self.din("cc", [128, 8, 2])
        self.ada_w = {l: self.din(f"ada_w{l}", [D, 6 * D]) for l in self.layers}
        self.ada_b = self.din("ada_b", [DEPTH, 128, 48, 2])
        self.norm_g = self.din("norm_g", [DEPTH, 2, 128, 8, 2])
        self.wr = self.din("wr", [DEPTH, D, 36])
        self.br = self.din("br", [DEPTH, 128, 36])
        self.w_gu = {l: self.din(f"w_gu{l}", [NE, D, 512]) for l in self.layers}
        self.w_dn = {l: self.din(f"w_dn{l}", [NE, 256, D]) for l in self.layers}
        self.wqkv = {0: self.din("na_w_qkv", [D, 3072], kinds=[0]), 1: self.din("sw_w_qkv", [D, 1536], kinds=[1]),
                     3: self.din("diff_w_qkv", [D, 3072], kinds=[3])}
        self.wo = {0: self.din("na_w_o", [D, D], kinds=[0]), 1: self.din("sw_w_o", [D, D], kinds=[1]),
                   2: self.din("mla_w_o", [D, D], kinds=[2]), 3: self.din("diff_w_o", [D, D], kinds=[3])}
        self.qkg = {0: self.din("na_qk_g", [128, 2], kinds=[0]), 1: self.din("sw_qk_g", [128, 2], kinds=[1]),
                    3: self.din("diff_qk_g", [128, 2], kinds=[3])}
        self.na_bias = self.din("na_bias", [16, 3, 128, 6, 512], kinds=[0])
        self.sw_sink = self.din("sw_sink", [128, 16], kinds=[1])
        self.sw_mask = self.din("sw_mask", [128, 6, 512], kinds=[1])
        self.rope64 = self.din("rope64", [2, 128, T], kinds=[1, 3])
        self.mla_w_dqkv = self.din("mla_w_dqkv", [D, 672], kinds=[2])
        self.mla_w_uq = self.din("mla_w_uq", [384, 1536], kinds=[2])
        self.mla_w_ukv = self.din("mla_w_ukv", [256, 2048], kinds=[2])
        self.mla_g = self.din("mla_g", [128, 8], kinds=[2])
        self.rope_mla = self.din("rope_mla", [2, 96, T], kinds=[2])
        self.diff_lam = self.din("diff_lam", [128, 4, 64], kinds=[3])
        self.diff_subln = self.din("diff_subln", [128, 1], kinds=[3])
        self.consts = self.din("consts", [128, 8, 128])
        self.sel_in = self.din("sel", [32, NE, 128])
        self.xs = self.dscr("xs", [D, T], F32)
        self.q_s = self.dscr("q_s", [16, 96, T], BF16)
        self.k_s = self.dscr("k_s", [16, 96, T], BF16)
        self.v_s = self.dscr("v_s", [16, 128, NKT, 64], BF16)
        self.kr_s = self.dscr("kr_s", [32, T], BF16)
        self.v_s2 = self.dscr("v_s2", [8, 128, NKT, 128], BF16)
        self.at_s = self.dscr("at_s", [D, T], BF16)
        self.h2_s = self.dscr("h2_s", [D, T], BF16)
        if self.last:
            self.out = nc.dram_tensor("outT", [D, SEQ], F32, kind="ExternalOutput").ap()
        else:
            self.out = nc.dram_tensor("xT_out", [D, T], F32, kind="ExternalOutput").ap()

    def _build(self):
        nc, S = self.nc, self.S
        with ExitStack() as es:
            self.ps = [nc.alloc_psum_tensor(f"ps{i}", [128, 512], F32) for i in range(8)]
            self.psb = [Buf(p) for p in self.ps]
            cst = self.sb(es, "cst_f", [128, 8, 128], F32)
            self.cst_f = cst
            cb = Buf(cst)
            S.dma("sp", cst[:, :, :], self.consts, writes=[cb])
            cstb = self.sb(es, "cst_b", [128, 8, 128], BF16)
            self.cstb_buf = Buf(cstb)
            self._cstb = cstb
            self.eps_col = self.sb(es, "eps_col", [128, 1], F32)
            self.eps_buf = Buf(self.eps_col)
            S.op("dve", lambda e: e.memset(self.eps_col[:, :], EPS), writes=[self.eps_buf])
            S.op("dve", lambda e: e.tensor_copy(out=cstb[:, :, :], in_=cst[:, :, :]), reads=[cb], writes=[self.cstb_buf])
            self.cst_buf = cb
            self.modT = self.sb(es, "modT", [128, DEPTH, 48, 2], F32)
            self.gsT = self.sb(es, "gsT", [128, DEPTH, 2, 8, 2], F32)
            self.mod_buf = Buf(self.modT)
            self.gs_buf = Buf(self.gsT)
            self.phase0()
            x_src = self.x_in
            for L in self.layers:
                kind = L % 4
                need_ctx = L < DEPTH - 1
                is_last_layer = (L == self.layers[-1])
                x_dst = self.xs
                S.barrier()
                with nc.named_scope(f"L{L}_p1"):
                    if kind == 2:
                        self.phase1_mla(L, x_src)
                    else:
                        self.phase1(L, x_src)
                    S.barrier()
                import os as _os
                _stop = int(_os.environ.get("K_STOP", "9"))
                with nc.named_scope(f"L{L}_p2"):
                    if _stop >= 2:
                        self.phase2(L, need_ctx)
                    S.barrier()
                if _stop >= 3:
                    self.phase3(L, x_src, x_dst, need_ctx, final=(is_last_layer))
                x_src = self.xs
            S.barrier()

    def phase0(self):
        nc, S = self.nc, self.S
        with ExitStack() as es:
            cc = self.sb(es, "cc", [128, 8, 2], F32)
            sc = self.sb(es, "scc", [128, 8, 2], F32)
            ccb, scb = Buf(cc), Buf(sc)
            S.dma("sp", cc[:, :, :], self.cc, writes=[ccb])
            S.op("act", lambda e: e.activation(out=sc[:, :, :], in_=cc[:, :, :], func=AF.Silu), reads=[ccb], writes=[scb])
            wm = [self.sb(es, f"wm{i}", [128, 8, 1024], F32) for i in range(2)]
            wmb = [Buf(w) for w in wm]
            adab = self.sb(es, "adab", [128, 48, 2], F32)
            adabb = Buf(adab)
            gn = self.sb(es, "gn", [128, 2, 8, 2], F32)
            gnb = Buf(gn)
            pm = self.ps[0]
            pmb = self.psb[0]
            for L in self.layers:
                S.dma("sp", adab[:, :, :], self.ada_b[L], writes=[adabb])
                S.dma("sp", gn[:, :, :, :], self.norm_g[L].rearrange("i p k t -> p i k t"), writes=[gnb])
                for which in range(6):
                    i = self.nxt("wm", 2)
                    src = self.ada_w[L].rearrange("(kc p) n -> p kc n", p=128)[:, :, which * 1024:(which + 1) * 1024]
                    S.dma("sp", wm[i][:, :, :], src, writes=[wmb[i]])
                    for fc in range(8):
                        j = which * 8 + fc
                        for kc in range(8):
                            S.op("pe", lambda e, i=i, fc=fc, kc=kc, j=j: e.matmul(
                                pm[:, 2 * j:2 * j + 2], lhsT=wm[i][:, kc, fc * 128:(fc + 1) * 128], rhs=sc[:, kc, :],
                                start=(kc == 0), stop=(kc == 7)), reads=[wmb[i], scb], writes=[pmb])
                mo = self.modT[:, L, :, :]
                S.op("dve", lambda e: e.tensor_tensor(out=mo, in0=pm[:, 0:96].rearrange("p (j t) -> p j t", t=2),
                                                      in1=adab[:, :, :], op=ALU.add),
                     reads=[pmb, adabb], writes=[self.mod_buf])
                for i2, sci in ((0, 1), (1, 4)):
                    g = self.gsT[:, L, i2, :, :]
                    S.op("dve", lambda e, g=g, sci=sci: e.tensor_scalar(
                        out=g, in0=self.modT[:, L, sci * 8:(sci + 1) * 8, :], scalar1=1.0, scalar2=None, op0=ALU.add),
                        reads=[self.mod_buf], writes=[self.gs_buf])
                    S.op("dve", lambda e, g=g, i2=i2: e.tensor_tensor(out=g, in0=g, in1=gn[:, i2, :, :], op=ALU.mult),
                         reads=[self.gs_buf, gnb], writes=[self.gs_buf])
            S.barrier()

    def modcol(self, L, which, fc, t):
        return self.modT[:, L, which * 8 + fc, t:t + 1]

    def gscol(self, L, i2, fc, t):
        return self.gsT[:, L, i2, fc, t:t + 1]

    def norm_mod(self, L, i2, xin, xb, n, t, rs, rsb, sq, sqb, tmp, tmpb, outs, pss):
        S = self.S
        ones_b = self.cstb[:, 0, :]
        pbank, pbuf = self.ps[pss], self.psb[pss]
        for kc in range(8):
            j = self.nxt("sq", 2)
            S.op("act", lambda e, j=j, kc=kc: e.activation(out=sq[j][:, :n], in_=xin[:, kc, :n], func=AF.Square),
                 reads=[xb], writes=[sqb[j]])
            S.op("pe", lambda e, j=j, kc=kc: e.matmul(pbank[:, :n], lhsT=ones_b, rhs=sq[j][:, :n],
                                                     start=(kc == 0), stop=(kc == 7)),
                 reads=[sqb[j], self.cstb_buf], writes=[pbuf])
        S.op("act", lambda e: e.activation(out=rs[:, :n], in_=pbank[:, :n], func=AF.Ln, bias=self.eps_col[:, 0:1], scale=1.0 / D),
             reads=[pbuf, self.eps_buf], writes=[rsb])
        S.op("act", lambda e: e.activation(out=rs[:, :n], in_=rs[:, :n], func=AF.Exp, scale=-0.5), reads=[rsb], writes=[rsb])
        sh = 0 if i2 == 0 else 3
        for kc in range(8):
            j = self.nxt("tmp", 2)
            S.op("dve", lambda e, j=j, kc=kc: e.tensor_tensor(out=tmp[j][:, :n], in0=xin[:, kc, :n], in1=rs[:, :n], op=ALU.mult),
                 reads=[xb, rsb], writes=[tmpb[j]])
            first = True
            for (ot, ob, eng) in outs:
                if first:
                    S.op("act", lambda e, j=j, kc=kc, ot=ot: e.activation(
                        out=ot[:, kc, :n], in_=tmp[j][:, :n], func=AF.Identity,
                        scale=self.gscol(L, i2, kc, t), bias=self.modcol(L, sh, kc, t)),
                        reads=[tmpb[j], self.gs_buf, self.mod_buf], writes=[ob])
                    first = False
                    prev_t, prev_b = ot, ob
                else:
                    S.op(eng, lambda e, kc=kc, ot=ot, prev_t=prev_t: e.tensor_copy(out=ot[:, kc, :n], in_=prev_t[:, kc, :n]),
                         reads=[prev_b], writes=[ob])

    @property
    def cstb(self):
        return self._cstb

    def phase1(self, L, x_src):
        nc, S = self.nc, self.S
        kind = L % 4
        ncol = {0: 3072, 1: 1536, 3: 3072}[kind]
        nq_ch = 8
        nk_ch = {0: 8, 1: 2, 3: 8}[kind]
        kcol0 = 1024
        vcol0 = {0: 2048, 1: 1280, 3: 2048}[kind]
        vw = {0: 1024, 1: 256, 3: 1024}[kind]
        dv = {0: 64, 1: 64, 3: 128}[kind]
        rope = kind in (1, 3)
        with ExitStack() as es:
            W = self.sb(es, "w1", [128, 8, ncol], BF16)
            Wb = [Buf(W) for _ in range(8)]
            wsrc = self.wqkv[kind].rearrange("(kc p) n -> p kc n", p=128)
            for kc in range(8):
                S.dma("pool", W[:, kc, :], wsrc[:, kc, :], writes=[Wb[kc]])
            qkg = self.sb(es, "qkg", [128, 2], F32)
            qkgb = Buf(qkg)
            S.dma("sp", qkg[:, :], self.qkg[kind], writes=[qkgb])
            xin = [self.sb(es, f"xin{i}", [128, 8, 512], F32) for i in range(2)]
            xinb = [Buf(t) for t in xin]
            hT = [self.sb(es, f"hT{i}", [128, 8, 512], BF16) for i in range(2)]
            hTb = [Buf(t) for t in hT]
            rs = self.sb(es, "rs", [128, 512], F32); rsb = Buf(rs)
            sq = [self.sb(es, f"sq{i}", [128, 512], BF16) for i in range(2)]; sqb = [Buf(t) for t in sq]
            tmp = [self.sb(es, f"tmp{i}", [128, 512], F32) for i in range(2)]; tmpb = [Buf(t) for t in tmp]
            rs2 = [self.sb(es, f"rs2{i}", [128, 512], F32) for i in range(2)]; rs2b = [Buf(t) for t in rs2]
            qn = [self.sb(es, f"qn{i}", [128, 512], BF16) for i in range(3)]; qnb = [Buf(t) for t in qn]
            qo = [self.sb(es, f"qo{i}", [128, 512], BF16) for i in range(3)]; qob = [Buf(t) for t in qo]
            t1 = [self.sb(es, f"t1{i}", [128, 512], F32) for i in range(2)]; t1b = [Buf(t) for t in t1]
            t2 = [self.sb(es, f"t2{i}", [128, 512], F32) for i in range(2)]; t2b = [Buf(t) for t in t2]
            vt = [self.sb(es, f"vt{i}", [128, 512], BF16) for i in range(3)]; vtb = [Buf(t) for t in vt]
            if rope:
                cs = [self.sb(es, f"cs{i}", [128, 2, 512], F32) for i in range(2)]
                csb = [Buf(t) for t in cs]
            blocks = _token_blocks(True)
            xsrc = x_src.rearrange("(kc p) t -> p kc t", p=128)

            def load(bi):
                t0, n = blocks[bi]
                i = bi % 2
                S.dma("sp", xin[i][:, :, :n], xsrc[:, :, t0:t0 + n], writes=[xinb[i]])
                if rope:
                    S.dma("sp", cs[i][:, :, :n], self.rope64.rearrange("c p t -> p c t")[:, :, t0:t0 + n], writes=[csb[i]])

            load(0)
            blk64 = self.cstb[:, 1, :]
            Rm = self.cstb[:, 2, :]
            for bi, (t0, n) in enumerate(blocks):
                if bi + 1 < len(blocks):
                    load(bi + 1)
                i = bi % 2
                tq = 0 if t0 < SEQ else 1
                self.norm_mod(L, 0, xin[i], xinb[i], n, tq, rs, rsb, sq, sqb, tmp, tmpb, [(hT[i], hTb[i], "act")], 7)
                nch = nq_ch + nk_ch
                cst = {}
                p1q_banks = [0, 1, 6]

                def stP(c):
                    isq = c < nq_ch
                    col0 = c * 128 if isq else kcol0 + (c - nq_ch) * 128
                    pb = p1q_banks[self.nxt("p1q3", 3)]
                    pq, pqb = self.ps[pb], self.psb[pb]
                    for kc in range(8):
                        S.op("pe", lambda e, kc=kc: e.matmul(
                            pq[:, :n], lhsT=W[:, kc, col0:col0 + 128], rhs=hT[i][:, kc, :n], start=(kc == 0), stop=(kc == 7)),
                            reads=[Wb[kc], hTb[i]], writes=[pqb])
                    cst[c] = dict(pq=pq, pqb=pqb, isq=isq)

                def stN(c):
                    d_ = cst[c]
                    pq, pqb, isq = d_["pq"], d_["pqb"], d_["isq"]
                    j = self.nxt("sq", 2)
                    S.op("act", lambda e: e.activation(out=sq[j][:, :n], in_=pq[:, :n], func=AF.Square),
                         reads=[pqb], writes=[sqb[j]])
                    pmi = 2 + self.nxt("p1m", 2)
                    pm, pmb = self.ps[pmi], self.psb[pmi]
                    S.op("pe", lambda e: e.matmul(pm[:, :n], lhsT=blk64, rhs=sq[j][:, :n], start=True, stop=True),
                         reads=[sqb[j], self.cstb_buf], writes=[pmb])
                    r = self.nxt("rs2", 2)
                    S.op("act", lambda e: e.activation(out=rs2[r][:, :n], in_=pm[:, :n], func=AF.Ln,
                                                       bias=self.eps_col[:, 0:1], scale=1.0),
                         reads=[pmb, self.eps_buf], writes=[rs2b[r]])
                    S.op("act", lambda e: e.activation(out=rs2[r][:, :n], in_=rs2[r][:, :n], func=AF.Exp, scale=-0.5), reads=[rs2b[r]], writes=[rs2b[r]])
                    gcol = qkg[:, 0:1] if isq else qkg[:, 1:2]
                    if rope and tq == 0:
                        a = self.nxt("qn", 3)
                        S.op("dve", lambda e: e.scalar_tensor_tensor(
                            out=qn[a][:, :n], in0=pq[:, :n], scalar=gcol, in1=rs2[r][:, :n], op0=ALU.mult, op1=ALU.mult),
                            reads=[pqb, rs2b[r], qkgb], writes=[qnb[a]])
                        d_["a"] = a
                    else:
                        o = self.nxt("qo", 3)
                        S.op("dve", lambda e: e.scalar_tensor_tensor(
                            out=qo[o][:, :n], in0=pq[:, :n], scalar=gcol, in1=rs2[r][:, :n], op0=ALU.mult, op1=ALU.mult),
                            reads=[pqb, rs2b[r], qkgb], writes=[qob[o]])
                        d_["o"] = o

                def stR(c):
                    d_ = cst[c]
                    isq = d_["isq"]
                    dst_s = self.q_s if isq else self.k_s
                    hh = (c if isq else c - nq_ch) * 2
                    if "a" in d_:
                        a = d_["a"]
                        pri = 4 + self.nxt("p1r", 2)
                        pr, prb = self.ps[pri], self.psb[pri]
                        S.op("pe", lambda e: e.matmul(pr[:, :n], lhsT=Rm, rhs=qn[a][:, :n], start=True, stop=True),
                             reads=[qnb[a], self.cstb_buf], writes=[prb])
                        u = self.nxt("t1", 2)
                        S.op("dve", lambda e: e.tensor_tensor(out=t1[u][:, :n], in0=qn[a][:, :n], in1=cs[i][:, 0, :n], op=ALU.mult),
                             reads=[qnb[a], csb[i]], writes=[t1b[u]])
                        S.op("dve", lambda e: e.tensor_tensor(out=t2[u][:, :n], in0=pr[:, :n], in1=cs[i][:, 1, :n], op=ALU.mult),
                             reads=[prb, csb[i]], writes=[t2b[u]])
                        o = self.nxt("qo", 3)
                        S.op("pool", lambda e: e.tensor_tensor(out=qo[o][:, :n], in0=t1[u][:, :n], in1=t2[u][:, :n], op=ALU.add),
                             reads=[t1b[u], t2b[u]], writes=[qob[o]])
                    else:
                        o = d_["o"]
                    for h2 in range(2):
                        S.dma("sp", dst_s[hh + h2, 0:64, t0:t0 + n], qo[o][h2 * 64:(h2 + 1) * 64, :n], reads=[qob[o]])

                for it_ in range(nch + 2):
                    if it_ < nch:
                        stP(it_)
                    if 0 <= it_ - 1 < nch:
                        stN(it_ - 1)
                    if 0 <= it_ - 2 < nch:
                        stR(it_ - 2)
                for tt in range(n // 128):
                    kt = (t0 // 128) + tt
                    for cbk in range((vw + 511) // 512):
                        w = min(512, vw - cbk * 512)
                        pvi = 4 + self.nxt("p1r", 2)
                        pv, pvb = self.ps[pvi], self.psb[pvi]
                        for kc in range(8):
                            S.op("pe", lambda e, kc=kc, pv=pv, tt=tt, cbk=cbk, w=w: e.matmul(
                                pv[:, :w], lhsT=hT[i][:, kc, tt * 128:(tt + 1) * 128],
                                rhs=W[:, kc, vcol0 + cbk * 512: vcol0 + cbk * 512 + w], start=(kc == 0), stop=(kc == 7)),
                                reads=[Wb[kc], hTb[i]], writes=[pvb])
                        vi = self.nxt("vt", 3)
                        S.op("act", lambda e, vi=vi, pv=pv, w=w: e.activation(out=vt[vi][:, :w], in_=pv[:, :w], func=AF.Copy),
                             reads=[pvb], writes=[vtb[vi]])
                        nh = w // dv
                        h0 = cbk * 512 // dv
                        if dv == 64:
                            dst = self.v_s[h0:h0 + nh, :, kt, :].rearrange("h p d -> p h d")
                        else:
                            dst = self.v_s2[h0:h0 + nh, :, kt, :].rearrange("h p d -> p h d")
                        S.dma("sp", dst, vt[vi][:, :w].rearrange("p (h d) -> p h d", d=dv), reads=[vtb[vi]])

    def phase1_mla(self, L, x_src):
        nc, S = self.nc, self.S
        with ExitStack() as es:
            W = self.sb(es, "w1", [128, 8, 672], BF16)
            Wb = [Buf(W) for _ in range(8)]
            wsrc = self.mla_w_dqkv.rearrange("(kc p) n -> p kc n", p=128)
            for kc in range(8):
                S.dma("pool", W[:, kc, :], wsrc[:, kc, :], writes=[Wb[kc]])
            Wuq = self.sb(es, "wuq", [128, 3, 1536], BF16); Wuqb = Buf(Wuq)
            S.dma("pool", Wuq[:, :, :], self.mla_w_uq.rearrange("(kc p) n -> p kc n", p=128), writes=[Wuqb])
            Wuk = self.sb(es, "wuk", [128, 2, 16, 64], BF16); Wukb = Buf(Wuk)
            Wuv = self.sb(es, "wuv", [128, 2, 16, 64], BF16); Wuvb = Buf(Wuv)
            ukv = self.mla_w_ukv.rearrange("(kc p) (h two d) -> p kc h two d", p=128, two=2, d=64)
            for kc in range(2):
                S.dma("pool", Wuk[:, kc, :, :], ukv[:, kc, :, 0, :], writes=[Wukb])
                S.dma("pool", Wuv[:, kc, :, :], ukv[:, kc, :, 1, :], writes=[Wuvb])
            mg = self.sb(es, "mg", [128, 8], F32); mgb = Buf(mg)
            S.dma("sp", mg[:, :], self.mla_g, writes=[mgb])
            xin = [self.sb(es, f"xin{i}", [128, 8, 512], F32) for i in range(2)]
            xinb = [Buf(t) for t in xin]
            hT = [self.sb(es, f"hT{i}", [128, 8, 512], BF16) for i in range(2)]
            hTb = [Buf(t) for t in hT]
            rs = self.sb(es, "rs", [128, 512], F32); rsb = Buf(rs)
            sq = [self.sb(es, f"sq{i}", [128, 512], BF16) for i in range(2)]; sqb = [Buf(t) for t in sq]
            tmp = [self.sb(es, f"tmp{i}", [128, 512], F32) for i in range(2)]; tmpb = [Buf(t) for t in tmp]
            rs2 = [self.sb(es, f"rs2{i}", [128, 512], F32) for i in range(2)]; rs2b = [Buf(t) for t in rs2]
            cf = self.sb(es, "cf", [128, 6, 512], F32); cfb = [Buf(cf) for _ in range(6)]
            cn = self.sb(es, "cn", [128, 5, 512], BF16); cnb = [Buf(cn) for _ in range(5)]
            qn = [self.sb(es, f"qn{i}", [128, 512], BF16) for i in range(3)]; qnb = [Buf(t) for t in qn]
            qo = [self.sb(es, f"qo{i}", [128, 512], BF16) for i in range(3)]; qob = [Buf(t) for t in qo]
            t1 = [self.sb(es, f"t1{i}", [128, 512], F32) for i in range(2)]; t1b = [Buf(t) for t in t1]
            t2 = [self.sb(es, f"t2{i}", [128, 512], F32) for i in range(2)]; t2b = [Buf(t) for t in t2]
            vt = [self.sb(es, f"vt{i}", [128, 512], BF16) for i in range(3)]; vtb = [Buf(t) for t in vt]
            cs = [self.sb(es, f"cs{i}", [96, 2, 512], F32) for i in range(2)]
            csb = [Buf(t) for t in cs]
            self.krt = [self.sb(es, f"krt{i}", [32, 2, 512], F32) for i in range(2)]
            self.krtb = [Buf(t) for t in self.krt]
            blocks = _token_blocks(True)
            xsrc = x_src.rearrange("(kc p) t -> p kc t", p=128)
            ones_b = self.cstb[:, 0, :]
            blk64 = self.cstb[:, 1, :]
            blkm = self.cstb[:, 5, :]
            Rmm = self.cstb[:, 6, :]

            def load(bi):
                t0, n = blocks[bi]
                i = bi % 2
                S.dma("sp", xin[i][:, :, :n], xsrc[:, :, t0:t0 + n], writes=[xinb[i]])
                S.dma("sp", cs[i][:, :, :n], self.rope_mla.rearrange("c p t -> p c t")[:, :, t0:t0 + n], writes=[csb[i]])
                S.dma("sp", self.krt[i][:, :, :n], self.rope_mla.rearrange("c p t -> p c t")[64:96, :, t0:t0 + n], writes=[self.krtb[i]])

            def rsq(pm, pmb, n, P, scale):
                r = self.nxt("rs2", 2)
                S.op("act", lambda e: e.activation(out=rs2[r][:P, :n], in_=pm[:P, :n], func=AF.Ln,
                                                   bias=self.eps_col[:P, 0:1], scale=scale),
                     reads=[pmb, self.eps_buf], writes=[rs2b[r]])
                S.op("act", lambda e: e.activation(out=rs2[r][:P, :n], in_=rs2[r][:P, :n], func=AF.Exp, scale=-0.5), reads=[rs2b[r]], writes=[rs2b[r]])
                return r

            load(0)
            for bi, (t0, n) in enumerate(blocks):
                if bi + 1 < len(blocks):
                    load(bi + 1)
                i = bi % 2
                tq = 0 if t0 < SEQ else 1
                self.norm_mod(L, 0, xin[i], xinb[i], n, tq, rs, rsb, sq, sqb, tmp, tmpb, [(hT[i], hTb[i], "act")], 7)
                for c in range(6):
                    M = 128 if c < 5 else 32
                    pb = self.nxt("p1q", 2)
                    pq, pqb = self.ps[pb], self.psb[pb]
                    for kc in range(8):
                        S.op("pe", lambda e, kc=kc, c=c, pq=pq, M=M: e.matmul(
                            pq[:M, :n], lhsT=W[:, kc, c * 128:c * 128 + M], rhs=hT[i][:, kc, :n], start=(kc == 0), stop=(kc == 7)),
                            reads=[Wb[kc], hTb[i]], writes=[pqb])
                    S.op("act", lambda e, c=c, pq=pq, M=M: e.activation(out=cf[:M, c, :n], in_=pq[:M, :n], func=AF.Copy),
                         reads=[pqb], writes=[cfb[c]])
                for (c0, c1, dim) in ((0, 3, 384), (3, 5, 256)):
                    pmi = 2 + self.nxt("p1m", 2)
                    pm, pmb = self.ps[pmi], self.psb[pmi]
                    for c in range(c0, c1):
                        j = self.nxt("sq", 2)
                        S.op("act", lambda e, j=j, c=c: e.activation(out=sq[j][:, :n], in_=cf[:, c, :n], func=AF.Square),
                             reads=[cfb[c]], writes=[sqb[j]])
                        S.op("pe", lambda e, j=j, c=c, pm=pm: e.matmul(pm[:, :n], lhsT=ones_b, rhs=sq[j][:, :n],
                                                                      start=(c == c0), stop=(c == c1 - 1)),
                             reads=[sqb[j], self.cstb_buf], writes=[pmb])
                    r = rsq(pm, pmb, n, 128, 1.0 / dim)
                    for c in range(c0, c1):
                        S.op("dve", lambda e, c=c, r=r: e.scalar_tensor_tensor(
                            out=cn[:, c, :n], in0=cf[:, c, :n], scalar=mg[:, c:c + 1], in1=rs2[r][:, :n], op0=ALU.mult, op1=ALU.mult),
                            reads=[cfb[c], rs2b[r], mgb], writes=[cnb[c]])
                j = self.nxt("sq", 2)
                S.op("act", lambda e, j=j: e.activation(out=sq[j][:32, :n], in_=cf[:32, 5, :n], func=AF.Square),
                     reads=[cfb[5]], writes=[sqb[j]])
                pmi = 2 + self.nxt("p1m", 2)
                pm, pmb = self.ps[pmi], self.psb[pmi]
                S.op("pe", lambda e, j=j, pm=pm: e.matmul(pm[:32, :n], lhsT=self.cstb[:32, 0, 0:32], rhs=sq[j][:32, :n], start=True, stop=True),
                     reads=[sqb[j], self.cstb_buf], writes=[pmb])
                r = rsq(pm, pmb, n, 32, 1.0 / 32)
                a = self.nxt("qn", 3)
                S.op("dve", lambda e, a=a, r=r: e.scalar_tensor_tensor(
                    out=qn[a][:32, :n], in0=cf[:32, 5, :n], scalar=mg[:32, 7:8], in1=rs2[r][:32, :n], op0=ALU.mult, op1=ALU.mult),
                    reads=[cfb[5], rs2b[r], mgb], writes=[qnb[a]])
                pri = 4 + self.nxt("p1r", 2)
                pr, prb = self.ps[pri], self.psb[pri]
                S.op("pe", lambda e, a=a, pr=pr: e.matmul(pr[:32, :n], lhsT=self.cstb[:32, 7, 0:32], rhs=qn[a][:32, :n], start=True, stop=True),
                     reads=[qnb[a], self.cstb_buf], writes=[prb])
                u = self.nxt("t1", 2)
                self._mla_krope(L, S, a, u, pr, prb, qn, qnb, t1, t1b, t2, t2b, qo, qob, n, t0, i)
                p1q_banks = [0, 1, 6]
                hst = {}

                def qP(h):
                    pb = p1q_banks[self.nxt("p1q3", 3)]
                    pq, pqb = self.ps[pb], self.psb[pb]
                    for kc in range(3):
                        S.op("pe", lambda e, kc=kc: e.matmul(
                            pq[:96, :n], lhsT=Wuq[:, kc, h * 96:(h + 1) * 96], rhs=cn[:, kc, :n], start=(kc == 0), stop=(kc == 2)),
                            reads=[Wuqb, cnb[kc]], writes=[pqb])
                    hst[h] = dict(pq=pq, pqb=pqb)

                def qN(h):
                    pq, pqb = hst[h]["pq"], hst[h]["pqb"]
                    j = self.nxt("sq", 2)
                    S.op("act", lambda e: e.activation(out=sq[j][:96, :n], in_=pq[:96, :n], func=AF.Square),
                         reads=[pqb], writes=[sqb[j]])
                    pmi = 2 + self.nxt("p1m", 2)
                    pm, pmb = self.ps[pmi], self.psb[pmi]
                    S.op("pe", lambda e: e.matmul(pm[:96, :n], lhsT=blkm[:96, 0:96], rhs=sq[j][:96, :n], start=True, stop=True),
                         reads=[sqb[j], self.cstb_buf], writes=[pmb])
                    r = rsq(pm, pmb, n, 96, 1.0)
                    a = self.nxt("qn", 3)
                    S.op("dve", lambda e: e.scalar_tensor_tensor(
                        out=qn[a][:96, :n], in0=pq[:96, :n], scalar=mg[:96, 5:6], in1=rs2[r][:96, :n], op0=ALU.mult, op1=ALU.mult),
                        reads=[pqb, rs2b[r], mgb], writes=[qnb[a]])
                    hst[h]["a"] = a

                def qR(h):
                    a = hst[h]["a"]
                    pri = 4 + self.nxt("p1r", 2)
                    pr, prb = self.ps[pri], self.psb[pri]
                    S.op("pe", lambda e: e.matmul(pr[:96, :n], lhsT=Rmm[:96, 0:96], rhs=qn[a][:96, :n], start=True, stop=True),
                         reads=[qnb[a], self.cstb_buf], writes=[prb])
                    u = self.nxt("t1", 2)
                    S.op("dve", lambda e: e.tensor_tensor(out=t1[u][:96, :n], in0=qn[a][:96, :n], in1=cs[i][:96, 0, :n], op=ALU.mult),
                         reads=[qnb[a], csb[i]], writes=[t1b[u]])
                    S.op("dve", lambda e: e.tensor_tensor(out=t2[u][:96, :n], in0=pr[:96, :n], in1=cs[i][:96, 1, :n], op=ALU.mult),
                         reads=[prb, csb[i]], writes=[t2b[u]])
                    o = self.nxt("qo", 3)
                    S.op("pool", lambda e: e.tensor_tensor(out=qo[o][:96, :n], in0=t1[u][:96, :n], in1=t2[u][:96, :n], op=ALU.add),
                         reads=[t1b[u], t2b[u]], writes=[qob[o]])
                    S.dma("sp", self.q_s[h, 0:96, t0:t0 + n], qo[o][:96, :n], reads=[qob[o]])

                for it_ in range(16 + 2):
                    if it_ < 16:
                        qP(it_)
                    if 0 <= it_ - 1 < 16:
                        qN(it_ - 1)
                    if 0 <= it_ - 2 < 16:
                        qR(it_ - 2)

                kst = {}

                def kP(c):
                    pb = p1q_banks[self.nxt("p1q3", 3)]
                    pq, pqb = self.ps[pb], self.psb[pb]
                    for kc in range(2):
                        S.op("pe", lambda e, kc=kc: e.matmul(
                            pq[:, :n], lhsT=Wuk[:, kc, 2 * c:2 * c + 2, :].rearrange("p h d -> p (h d)"), rhs=cn[:, 3 + kc, :n],
                            start=(kc == 0), stop=(kc == 1)),
                            reads=[Wukb, cnb[3 + kc]], writes=[pqb])
                    kst[c] = (pq, pqb)

                def kN(c):
                    pq, pqb = kst[c]
                    j = self.nxt("sq", 2)
                    S.op("act", lambda e: e.activation(out=sq[j][:, :n], in_=pq[:, :n], func=AF.Square),
                         reads=[pqb], writes=[sqb[j]])
                    pmi = 2 + self.nxt("p1m", 2)
                    pm, pmb = self.ps[pmi], self.psb[pmi]
                    S.op("pe", lambda e: e.matmul(pm[:, :n], lhsT=blk64, rhs=sq[j][:, :n], start=True, stop=True),
                         reads=[sqb[j], self.cstb_buf], writes=[pmb])
                    kst[c] = (pq, pqb, pm, pmb)

                def kR(c):
                    pq, pqb, pm, pmb = kst[c]
                    r = rsq(pm, pmb, n, 128, 1.0)
                    o = self.nxt("qo", 3)
                    S.op("dve", lambda e: e.scalar_tensor_tensor(
                        out=qo[o][:, :n], in0=pq[:, :n], scalar=mg[:, 6:7], in1=rs2[r][:, :n], op0=ALU.mult, op1=ALU.mult),
                        reads=[pqb, rs2b[r], mgb], writes=[qob[o]])
                    for h2 in range(2):
                        S.dma("sp", self.k_s[2 * c + h2, 0:64, t0:t0 + n], qo[o][h2 * 64:(h2 + 1) * 64, :n], reads=[qob[o]])

                for it_ in range(8 + 2):
                    if it_ < 8:
                        kP(it_)
                    if 0 <= it_ - 1 < 8:
                        kN(it_ - 1)
                    if 0 <= it_ - 2 < 8:
                        kR(it_ - 2)
                for tt in range(n // 128):
                    kt = (t0 // 128) + tt
                    for cbk in range(2):
                        pvi = 4 + self.nxt("p1r", 2)
                        pv, pvb = self.ps[pvi], self.psb[pvi]
                        for kc in range(2):
                            S.op("pe", lambda e, kc=kc, pv=pv, tt=tt, cbk=cbk: e.matmul(
                                pv[:, :512], lhsT=cn[:, 3 + kc, tt * 128:(tt + 1) * 128],
                                rhs=Wuv[:, kc, cbk * 8:(cbk + 1) * 8, :].rearrange("p h d -> p (h d)"), start=(kc == 0), stop=(kc == 1)),
                                reads=[Wuvb, cnb[3 + kc]], writes=[pvb])
                        vi = self.nxt("vt", 3)
                        S.op("act", lambda e, vi=vi, pv=pv: e.activation(out=vt[vi][:, :512], in_=pv[:, :512], func=AF.Copy),
                             reads=[pvb], writes=[vtb[vi]])
                        dst = self.v_s[cbk * 8:(cbk + 1) * 8, :, kt, :].rearrange("h p d -> p h d")
                        S.dma("sp", dst, vt[vi][:, :512].rearrange("p (h d) -> p h d", d=64), reads=[vtb[vi]])

    def _mla_krope(self, L, S, a, u, pr, prb, qn, qnb, t1, t1b, t2, t2b, qo, qob, n, t0, i):
        kt_ = self.krt[i]
        kb_ = self.krtb[i]
        S.op("pool", lambda e: e.tensor_tensor(out=t1[u][:32, :n], in0=qn[a][:32, :n], in1=kt_[:32, 0, :n], op=ALU.mult),
             reads=[qnb[a], kb_], writes=[t1b[u]])
        S.op("dve", lambda e: e.tensor_tensor(out=t2[u][:32, :n], in0=pr[:32, :n], in1=kt_[:32, 1, :n], op=ALU.mult),
             reads=[prb, kb_], writes=[t2b[u]])
        o = self.nxt("qo", 3)
        S.op("pool", lambda e: e.tensor_tensor(out=qo[o][:32, :n], in0=t1[u][:32, :n], in1=t2[u][:32, :n], op=ALU.add),
             reads=[t1b[u], t2b[u]], writes=[qob[o]])
        S.dma("sp", self.kr_s[:, t0:t0 + n], qo[o][:32, :n], reads=[qob[o]])

    def phase2(self, L, need_ctx):
        nc, S = self.nc, self.S
        kind = L % 4
        dq = 96 if kind == 2 else 64
        dv = 128 if kind == 3 else 64
        scale = dq ** -0.5
        ones_b = self.cstb[:, 0, :]
        with ExitStack() as es:
            nkv = 2 if kind == 3 else 1
            kT = [[self.sb(es, f"kT{i}_{j}", [128, T], BF16) for j in range(nkv)] for i in range(2)]
            kTb = [[Buf(t) for t in row] for row in kT]
            V = [self.sb(es, f"V{i}", [128, NKT, 128], BF16) for i in range(2)]
            Vb = [Buf(t) for t in V]
            qt = [self.sb(es, f"qt{i}", [128, 512], BF16) for i in range(3)]; qtb = [Buf(t) for t in qt]
            for row in kT:
                for t_ in row:
                    pass
            for i_, row in enumerate(kT):
                for j_, t_ in enumerate(row):
                    S.op("pool", lambda e, t_=t_: e.memset(t_[dq:128, :], 0.0), writes=[kTb[i_][j_]])
            for i_, t_ in enumerate(qt):
                S.op("pool", lambda e, t_=t_: e.memset(t_[dq:128, :], 0.0), writes=[qtb[i_]])
            LOOK = 4 if dv == 64 else 3
            sbanks = [0, 1, 2, 5, 6] if dv == 64 else [0, 1, 2, 7]
            NPT = LOOK + 2
            pt = [self.sb(es, f"pt{i}", [128, 512], BF16) for i in range(NPT)]; ptb = [Buf(t) for t in pt]
            rd = [self.sb(es, f"rd{i}", [128, 512], F32) for i in range(2)]; rdb = [Buf(t) for t in rd]
            ot = [self.sb(es, f"ot{i}", [128, 512], BF16) for i in range(2)]; otb = [Buf(t) for t in ot]
            if dv != 64:
                pp = [self.sb(es, f"pp{i}", [128, 512], BF16) for i in range(4)]; ppb = [Buf(t) for t in pp]
                pq4 = [self.sb(es, f"pq4{i}", [128, 512], BF16) for i in range(3)]; pq4b = [Buf(t) for t in pq4]
            if kind == 0:
                bt = [self.sb(es, f"bt{i}", [128, 6, 512], F32) for i in range(2)]; btb = [Buf(t) for t in bt]
                tb = [self.sb(es, f"tb{i}", [128, 512], F32) for i in range(3)]; tbb = [Buf(t) for t in tb]
            if kind == 1:
                mk = self.sb(es, "mk", [128, 6, 512], BF16); mkb = Buf(mk)
                S.dma("pool", mk[:, :, :], self.sw_mask, writes=[mkb])
                snk = self.sb(es, "snk", [128, 16], F32); snkb = Buf(snk)
                S.dma("sp", snk[:, :], self.sw_sink, writes=[snkb])
                S.op("act", lambda e: e.activation(out=snk[:, :], in_=snk[:, :], func=AF.Exp), reads=[snkb], writes=[snkb])
                p0 = [self.sb(es, f"p0{i}", [128, 512], BF16) for i in range(3)]; p0b = [Buf(t) for t in p0]
            if kind == 3:
                lam = self.sb(es, "lam", [128, 4, 64], F32); lamb = Buf(lam)
                S.dma("sp", lam[:, :, :], self.diff_lam, writes=[lamb])
                lt = self.sb(es, "lt", [128, 2, 64], F32); ltb = Buf(lt)
                ls = self.sb(es, "ls", [128, 4], F32); lsb = Buf(ls)
                S.op("dve", lambda e: e.tensor_tensor(out=lt[:, 0, :], in0=lam[:, 0, :], in1=lam[:, 1, :], op=ALU.mult), reads=[lamb], writes=[ltb])
                S.op("dve", lambda e: e.tensor_tensor(out=lt[:, 1, :], in0=lam[:, 2, :], in1=lam[:, 3, :], op=ALU.mult), reads=[lamb], writes=[ltb])
                S.op("dve", lambda e: e.tensor_reduce(out=ls[:, 0:2], in_=lt[:, :, :], axis=mybir.AxisListType.X, op=ALU.add), reads=[ltb], writes=[lsb])
                S.op("act", lambda e: e.activation(out=ls[:, 0:2], in_=ls[:, 0:2], func=AF.Exp), reads=[lsb], writes=[lsb])
                lam_init = 0.8 - 0.6 * math.exp(-0.3 * L)
                S.op("dve", lambda e: e.tensor_tensor(out=ls[:, 2:3], in0=ls[:, 1:2], in1=ls[:, 0:1], op=ALU.subtract), reads=[lsb], writes=[lsb])
                S.op("dve", lambda e: e.tensor_scalar(out=ls[:, 2:3], in0=ls[:, 2:3], scalar1=-lam_init, scalar2=None, op0=ALU.add), reads=[lsb], writes=[lsb])
                sg = self.sb(es, "sg", [128, 1], F32); sgb = Buf(sg)
                S.dma("sp", sg[:, :], self.diff_subln, writes=[sgb])
                S.op("dve", lambda e: e.tensor_scalar(out=sg[:, :], in0=sg[:, :], scalar1=1.0 - lam_init, scalar2=None, op0=ALU.mult), reads=[sgb], writes=[sgb])
                o1 = [self.sb(es, f"o1{i}", [128, 512], F32) for i in range(2)]; o1b = [Buf(t) for t in o1]
                od = [self.sb(es, f"od{i}", [128, 512], F32) for i in range(2)]; odb = [Buf(t) for t in od]
                sqd = [self.sb(es, f"sqd{i}", [128, 512], BF16) for i in range(2)]; sqdb = [Buf(t) for t in sqd]

            if kind in (0, 2):
                groups = [([h], h, [(h, 0)]) for h in range(16)]
            elif kind == 1:
                groups = [([g], g, [(4 * g + i, 0) for i in range(4)]) for g in range(4)]
            else:
                groups = [([2 * g, 2 * g + 1], g, [(2 * g, 0), (2 * g + 1, 1)]) for g in range(8)]

            qblocks = []
            for qi in range(8):
                tiles = []
                pat = 0
                if kind == 0:
                    r0 = 8 * qi
                    kr0 = min(max(r0 - 4, 0), 52)
                    pat = 0 if qi == 0 else (2 if qi == 7 else 1)
                    tiles = [(kr0 // 2 + j, "bias", j) for j in range(6)]
                elif kind == 1:
                    for j in range(6):
                        kt = 4 * qi - 1 + j
                        if 0 <= kt < 32:
                            tiles.append((kt, "mask", j))
                else:
                    tiles = [(kt, "plain", 0) for kt in range(32)]
                tiles += [(32, "plain", 0), (33, "plain", 0)]
                qblocks.append((qi * 512, 512, tiles, pat))
            if need_ctx:
                qblocks.append((SEQ, NCTX, [(32, "plain", 0), (33, "plain", 0)], -1))

            aug = (dv == 64)
            if aug:
                for i in range(2):
                    S.op("pool", lambda e, i=i: e.memset(V[i][:, :, 64:128], 1.0), writes=[Vb[i]])

            def load_group(gi):
                ks, vh, _ = groups[gi]
                i = gi % 2
                for j, kh in enumerate(ks):
                    S.dma("sp", kT[i][j][0:64, :], self.k_s[kh, 0:64, :], writes=[kTb[i][j]])
                    if kind == 2:
                        S.dma("sp", kT[i][j][64:96, :], self.kr_s[:, :], writes=[kTb[i][j]])
                if aug:
                    S.dma("sp", V[i][:, :, 0:64], self.v_s[vh], writes=[Vb[i]])
                else:
                    S.dma("sp", V[i][:, :, :], self.v_s2[vh], writes=[Vb[i]])

            load_group(0)
            for gi, (ks, vh, subs) in enumerate(groups):
                if gi + 1 < len(groups):
                    load_group(gi + 1)
                gb = gi % 2
                items = []
                for (q0, nq, tiles, pat) in qblocks:
                    for si, (qh, kvi) in enumerate(subs):
                        items.append(dict(q0=q0, nq=nq, tiles=tiles, pat=pat, si=si, qh=qh, kvi=kvi))
                work = [(ii, ti) for ii, it in enumerate(items) for ti in range(len(it["tiles"]))]
                st = dict(pat=None, bi=None, o1i=None)

                def prefetch(ii):
                    it = items[ii]
                    if kind == 0 and it["pat"] >= 0 and it["pat"] != st["pat"]:
                        st["pat"] = it["pat"]
                        st["bi"] = self.nxt("bt", 2)
                        S.dma("sp", bt[st["bi"]][:, :, :], self.na_bias[it["qh"], it["pat"]], writes=[btb[st["bi"]]])
                    it["bi"] = st["bi"]
                    it["qi"] = self.nxt("qt", 3)
                    S.dma("sp", qt[it["qi"]][0:dq, :it["nq"]], self.q_s[it["qh"], 0:dq, it["q0"]:it["q0"] + it["nq"]], writes=[qtb[it["qi"]]])

                def stageA(ii, ti):
                    it = items[ii]
                    nq = it["nq"]
                    if ti == 0:
                        if ii == 0:
                            prefetch(0)
                        if ii + 1 < len(items):
                            prefetch(ii + 1)
                        it["ni"] = 3 + self.nxt("p2n", 2)
                        it["di"] = 5 + self.nxt("p2d", 2)
                        it["pis"] = {}
                        it["pps"] = {}
                    kt, mode, ref = it["tiles"][ti]
                    qi_, bi_, kvi = it["qi"], it["bi"], it["kvi"]
                    si_ = sbanks[self.nxt("p2s", len(sbanks))]
                    sp_, spb = self.ps[si_], self.psb[si_]
                    S.op("pe", lambda e: e.matmul(
                        sp_[:, :nq], lhsT=kT[gb][kvi][:, kt * 128:(kt + 1) * 128], rhs=qt[qi_][:, :nq], start=True, stop=True),
                        reads=[kTb[gb][kvi], qtb[qi_]], writes=[spb])
                    pi = self.nxt("pt", NPT)
                    it["pis"][ti] = pi
                    if mode == "plain":
                        S.op("act", lambda e: e.activation(out=pt[pi][:, :nq], in_=sp_[:, :nq], func=AF.Exp, scale=scale),
                             reads=[spb], writes=[ptb[pi]])
                    elif mode == "mask":
                        zi = self.nxt("p0", 3)
                        S.op("act", lambda e: e.activation(out=p0[zi][:, :nq], in_=sp_[:, :nq], func=AF.Exp, scale=scale),
                             reads=[spb], writes=[p0b[zi]])
                        S.op("dve", lambda e: e.tensor_tensor(out=pt[pi][:, :nq], in0=p0[zi][:, :nq], in1=mk[:, ref, :nq], op=ALU.mult),
                             reads=[p0b[zi], mkb], writes=[ptb[pi]])
                    else:
                        zi = self.nxt("tb", 3)
                        S.op("dve", lambda e: e.scalar_tensor_tensor(
                            out=tb[zi][:, :nq], in0=sp_[:, :nq], scalar=scale, in1=bt[bi_][:, ref, :nq], op0=ALU.mult, op1=ALU.add),
                            reads=[spb, btb[bi_]], writes=[tbb[zi]])
                        S.op("act", lambda e: e.activation(out=pt[pi][:, :nq], in_=tb[zi][:, :nq], func=AF.Exp),
                             reads=[tbb[zi]], writes=[ptb[pi]])

                def pairsum(ii, ti):
                    it = items[ii]
                    nq = it["nq"]
                    if aug or ti % 2 == 0:
                        return
                    pa, pb_ = it["pis"][ti - 1], it["pis"][ti]
                    pj = self.nxt("pp", 4)
                    it["pps"][ti] = pj
                    S.op("dve", lambda e: e.tensor_tensor(out=pp[pj][:, :nq], in0=pt[pa][:, :nq], in1=pt[pb_][:, :nq], op=ALU.add),
                         reads=[ptb[pa], ptb[pb_]], writes=[ppb[pj]])

                def stageB(ii, ti):
                    it = items[ii]
                    nq, qh, si = it["nq"], it["qh"], it["si"]
                    q0 = it["q0"]
                    kt, mode, ref = it["tiles"][ti]
                    pi = it["pis"][ti]
                    ni, di = it["ni"], it["di"]
                    num, numb, den, denb = self.ps[ni], self.psb[ni], self.ps[di], self.psb[di]
                    st_, sp2 = (ti == 0), (ti == len(it["tiles"]) - 1)
                    S.op("pe", lambda e: e.matmul(num[:, :nq], lhsT=V[gb][:, kt, :], rhs=pt[pi][:, :nq], start=st_, stop=sp2),
                         reads=[Vb[gb], ptb[pi]], writes=[numb])
                    if (not aug) and ti % 2 == 1:
                        pj = it["pps"][ti]
                        S.op("pe", lambda e: e.matmul(den[:, :nq], lhsT=ones_b, rhs=pp[pj][:, :nq], start=(ti == 1), stop=sp2),
                             reads=[self.cstb_buf, ppb[pj]], writes=[denb])
                    if not sp2:
                        return None
                    return lambda: finish(it, num, numb, den, denb)

                def finish(it, num, numb, den, denb):
                    nq, qh, si, q0 = it["nq"], it["qh"], it["si"], it["q0"]
                    ri = self.nxt("rd", 2)
                    if aug:
                        if kind == 1:
                            S.op("act", lambda e: e.activation(out=rd[ri][0:64, :nq], in_=num[64:128, :nq], func=AF.Ln, bias=snk[64:128, qh:qh + 1], scale=1.0),
                                 reads=[numb, snkb], writes=[rdb[ri]])
                            S.op("act", lambda e: e.activation(out=rd[ri][0:64, :nq], in_=rd[ri][0:64, :nq], func=AF.Exp, scale=-1.0), reads=[rdb[ri]], writes=[rdb[ri]])
                        elif kind == 0:
                            S.op("act", lambda e: e.activation(out=rd[ri][0:64, :nq], in_=num[64:128, :nq], func=AF.Ln), reads=[numb], writes=[rdb[ri]])
                            S.op("act", lambda e: e.activation(out=rd[ri][0:64, :nq], in_=rd[ri][0:64, :nq], func=AF.Exp, scale=-1.0), reads=[rdb[ri]], writes=[rdb[ri]])
                        else:
                            S.op("dve", lambda e: e.reciprocal(out=rd[ri][0:64, :nq], in_=num[64:128, :nq]), reads=[numb], writes=[rdb[ri]])
                        oi = self.nxt("ot", 2)
                        S.op("dve", lambda e: e.tensor_tensor(out=ot[oi][0:64, :nq], in0=num[0:64, :nq], in1=rd[ri][0:64, :nq], op=ALU.mult),
                             reads=[numb, rdb[ri]], writes=[otb[oi]])
                        S.dma("sp", self.at_s[qh * 64:(qh + 1) * 64, q0:q0 + nq], ot[oi][0:64, :nq], reads=[otb[oi]])
                        return
                    S.op("act", lambda e: e.activation(out=rd[ri][:, :nq], in_=den[:, :nq], func=AF.Ln), reads=[denb], writes=[rdb[ri]])
                    S.op("act", lambda e: e.activation(out=rd[ri][:, :nq], in_=rd[ri][:, :nq], func=AF.Exp, scale=-1.0), reads=[rdb[ri]], writes=[rdb[ri]])
                    if si == 0:
                        o1i = self.nxt("o1", 2)
                        st["o1i"] = o1i
                        S.op("dve", lambda e: e.tensor_tensor(out=o1[o1i][:, :nq], in0=num[:, :nq], in1=rd[ri][:, :nq], op=ALU.mult),
                             reads=[numb, rdb[ri]], writes=[o1b[o1i]])
                    else:
                        o1i = st["o1i"]
                        odi = self.nxt("od", 2)
                        S.op("dve", lambda e: e.tensor_tensor(out=od[odi][:, :nq], in0=num[:, :nq], in1=rd[ri][:, :nq], op=ALU.mult),
                             reads=[numb, rdb[ri]], writes=[odb[odi]])
                        S.op("dve", lambda e: e.scalar_tensor_tensor(
                            out=od[odi][:, :nq], in0=od[odi][:, :nq], scalar=ls[:, 2:3], in1=o1[o1i][:, :nq], op0=ALU.mult, op1=ALU.add),
                            reads=[odb[odi], o1b[o1i], lsb], writes=[odb[odi]])
                        qd = self.nxt("sqd", 2)
                        S.op("act", lambda e: e.activation(out=sqd[qd][:, :nq], in_=od[odi][:, :nq], func=AF.Square),
                             reads=[odb[odi]], writes=[sqdb[qd]])
                        pm, pmb = self.ps[7], self.psb[7]
                        S.op("pe", lambda e: e.matmul(pm[:, :nq], lhsT=ones_b, rhs=sqd[qd][:, :nq], start=True, stop=True),
                             reads=[sqdb[qd], self.cstb_buf], writes=[pmb])
                        r2 = self.nxt("rd", 2)
                        S.op("act", lambda e: e.activation(out=rd[r2][:, :nq], in_=pm[:, :nq], func=AF.Ln, bias=self.eps_col[:, 0:1], scale=1.0 / 128),
                             reads=[pmb, self.eps_buf], writes=[rdb[r2]])
                        S.op("act", lambda e: e.activation(out=rd[r2][:, :nq], in_=rd[r2][:, :nq], func=AF.Exp, scale=-0.5), reads=[rdb[r2]], writes=[rdb[r2]])
                        oi = self.nxt("ot", 2)
                        S.op("dve", lambda e: e.scalar_tensor_tensor(
                            out=ot[oi][:, :nq], in0=od[odi][:, :nq], scalar=sg[:, 0:1], in1=rd[r2][:, :nq], op0=ALU.mult, op1=ALU.mult),
                            reads=[odb[odi], rdb[r2], sgb], writes=[otb[oi]])
                        S.dma("sp", self.at_s[vh * 128:(vh + 1) * 128, q0:q0 + nq], ot[oi][:, :nq], reads=[otb[oi]])

                pending = []
                for idx in range(len(work) + LOOK):
                    if idx < len(work):
                        stageA(*work[idx])
                        pairsum(*work[idx])
                    while pending and pending[0][0] <= idx:
                        pending.pop(0)[1]()
                    if idx - LOOK >= 0:
                        fin = stageB(*work[idx - LOOK])
                        if fin is not None:
                            pending.append((idx + 3, fin))
                for _, fin in pending:
                    fin()

    def phase3(self, L, x_src, x_dst, need_ctx, final):
        nc, S = self.nc, self.S
        kind = L % 4
        ident = self.cst_f[:, 3, :]
        with ExitStack() as es0:
            GT = self.sb(es0, "GT", [32, T], BF16); GTb = Buf(GT)
            with ExitStack() as es, nc.named_scope(f"L{L}_p3a"):
                Wo = self.sb(es, "wo", [128, 8, D], BF16); Wob = [Buf(Wo) for _ in range(8)]
                wsrc = self.wo[kind].rearrange("(kc p) n -> p kc n", p=128)
                for kc in range(8):
                    S.dma("pool", Wo[:, kc, :], wsrc[:, kc, :], writes=[Wob[kc]])
                Wr = self.sb(es, "wr", [128, 8, 36], F32); Wrb = Buf(Wr)
                S.dma("sp", Wr[:, :, :], self.wr[L].rearrange("(kc p) n -> p kc n", p=128), writes=[Wrb])
                brt = self.sb(es, "brt", [128, 36], F32); brb = Buf(brt)
                S.dma("sp", brt[:, :], self.br[L], writes=[brb])
                xin = [self.sb(es, f"xin{i}", [128, 8, 512], F32) for i in range(2)]; xinb = [Buf(t) for t in xin]
                at = [self.sb(es, f"at{i}", [128, 8, 512], BF16) for i in range(2)]; atb = [Buf(t) for t in at]
                h2f = self.sb(es, "h2f", [128, 8, 512], F32); h2fb = Buf(h2f)
                h2b = [self.sb(es, f"h2b{i}", [128, 8, 512], BF16) for i in range(2)]; h2bb = [Buf(t) for t in h2b]
                rs = self.sb(es, "rs", [128, 512], F32); rsb = Buf(rs)
                sq = [self.sb(es, f"sq{i}", [128, 512], BF16) for i in range(2)]; sqb = [Buf(t) for t in sq]
                tmp = [self.sb(es, f"tmp{i}", [128, 512], F32) for i in range(2)]; tmpb = [Buf(t) for t in tmp]
                rt = self.sb(es, "rt", [128, 4, 256], F32); rtb = Buf(rt)
                blocks = _token_blocks(need_ctx)
                xsrc = x_src.rearrange("(kc p) t -> p kc t", p=128)
                xdst = x_dst.rearrange("(kc p) t -> p kc t", p=128)
                asrc = self.at_s.rearrange("(kc p) t -> p kc t", p=128)
                hdst = self.h2_s.rearrange("(kc p) t -> p kc t", p=128)

                def load(bi):
                    t0, n = blocks[bi]
                    i = bi % 2
                    S.dma("sp", xin[i][:, :, :n], xsrc[:, :, t0:t0 + n], writes=[xinb[i]])
                    S.dma("sp", at[i][:, :, :n], asrc[:, :, t0:t0 + n], writes=[atb[i]])

                load(0)
                for bi, (t0, n) in enumerate(blocks):
                    if bi + 1 < len(blocks):
                        load(bi + 1)
                    i = bi % 2
                    tq = 0 if t0 < SEQ else 1
                    for oc in range(8):
                        pb = self.nxt("p3y", 2)
                        py, pyb = self.ps[pb], self.psb[pb]
                        for kc in range(8):
                            S.op("pe", lambda e, kc=kc, oc=oc, py=py: e.matmul(
                                py[:, :n], lhsT=Wo[:, kc, oc * 128:(oc + 1) * 128], rhs=at[i][:, kc, :n], start=(kc == 0), stop=(kc == 7)),
                                reads=[Wob[kc], atb[i]], writes=[pyb])
                        S.op("dve", lambda e, oc=oc, py=py: e.scalar_tensor_tensor(
                            out=xin[i][:, oc, :n], in0=py[:, :n], scalar=self.modcol(L, 2, oc, tq), in1=xin[i][:, oc, :n], op0=ALU.mult, op1=ALU.add),
                            reads=[pyb, xinb[i], self.mod_buf], writes=[xinb[i]])
                    S.dma("sp", xdst[:, :, t0:t0 + n], xin[i][:, :, :n], reads=[xinb[i]])
                    hb = h2b[i]
                    self.norm_mod(L, 1, xin[i], xinb[i], n, tq, rs, rsb, sq, sqb, tmp, tmpb,
                                  [(h2f, h2fb, "act"), (hb, h2bb[i], "pool")], 7)
                    S.dma("sp", hdst[:, :, t0:t0 + n], hb[:, :, :n], reads=[h2bb[i]])
                    nt = n // 128
                    pr, prb = self.ps[2], self.psb[2]
                    for tt in range(nt):
                        for kc in range(8):
                            S.op("pe", lambda e, kc=kc, tt=tt: e.matmul(
                                pr[:, tt * 36:(tt + 1) * 36], lhsT=h2f[:, kc, tt * 128:(tt + 1) * 128], rhs=Wr[:, kc, :], start=(kc == 0), stop=(kc == 7)),
                                reads=[h2fb, Wrb], writes=[prb])
                    self.topk(pr, prb, brt, brb, rt, rtb, nt)
                    pt_, ptb_ = self.ps[3], self.psb[3]
                    for tt in range(nt):
                        S.op("pe", lambda e, tt=tt: e.transpose(pt_[0:32, tt * 128:(tt + 1) * 128], rt[:, tt, 64:96], ident),
                             reads=[rtb, self.cst_buf], writes=[ptb_])
                    S.op("act", lambda e: e.activation(out=GT[:, t0:t0 + n], in_=pt_[0:32, 0:n], func=AF.Copy),
                         reads=[ptb_], writes=[GTb])
            S.barrier()
            with ExitStack() as es, nc.named_scope(f"L{L}_p3b"):
                sel = self.sb(es, "sel", [32, NE, 128], BF16); selb = Buf(sel)
                S.dma("pool", sel[:, :, :], self.sel_in, writes=[selb])
                SBW = 2048 + (NCTX if need_ctx else 0)
                h2 = self.sb(es, "h2", [128, 8, SBW], BF16); h2bf = Buf(h2)
                acc = self.sb(es, "acc", [128, 8, SBW], F32); accb = [Buf(acc) for _ in range(5)]
                NW = 3
                wgu = [self.sb(es, f"wgu{i}", [128, 8, 512], BF16) for i in range(NW)]; wgub = [Buf(t) for t in wgu]
                wdn = [self.sb(es, f"wdn{i}", [128, 2, D], BF16) for i in range(NW)]; wdnb = [Buf(t) for t in wdn]
                x1 = self.sb(es, "x1", [128, 8, 512], F32); x1b = Buf(x1)
                sgt = [self.sb(es, f"sgt{i}", [128, 512], F32) for i in range(2)]; sgtb = [Buf(t) for t in sgt]
                tt_ = [self.sb(es, f"tt{i}", [128, 512], F32) for i in range(2)]; ttb = [Buf(t) for t in tt_]
                aT = [self.sb(es, f"aT{i}", [128, 512], BF16) for i in range(4)]; aTb = [Buf(t) for t in aT]
                sbs = [(0, 2048), (2048, SBW)]
                hsrc = self.h2_s.rearrange("(kc p) t -> p kc t", p=128)
                xdst = x_dst.rearrange("(kc p) t -> p kc t", p=128)
                outd = self.out.rearrange("(kc p) t -> p kc t", p=128)

                def loadw(e):
                    i = e % NW
                    S.dma("pool", wgu[i][:, :, :], self.w_gu[L][e].rearrange("(kc p) n -> p kc n", p=128), writes=[wgub[i]])
                    S.dma("pool", wdn[i][:, :, :], self.w_dn[L][e].rearrange("(hc p) n -> p hc n", p=128), writes=[wdnb[i]])

                for (s0, sn) in sbs:
                    S.dma("sp", h2[:, :, :sn], hsrc[:, :, s0:s0 + sn], writes=[h2bf])
                    nb = (sn + 511) // 512
                    units = [(e, b) for e in range(NE) for b in range(nb)]

                    def gu_group(e, b, gidx):
                        wi = e % NW
                        b0 = b * 512
                        n = min(512, sn - b0)
                        j = (0, 2, 1, 3)[gidx]
                        pgj, pgbj = self.ps[j], self.psb[j]
                        for kc in range(8):
                            S.op("pe", lambda e_: e_.matmul(
                                pgj[:, :n], lhsT=wgu[wi][:, kc, j * 128:(j + 1) * 128], rhs=h2[:, kc, b0:b0 + n],
                                start=(kc == 0), stop=(kc == 7)), reads=[wgub[wi], h2bf], writes=[pgbj])

                    def chain(e, b):
                        b0 = b * 512
                        n = min(512, sn - b0)
                        pg = [self.ps[j] for j in range(4)]
                        pgb = [self.psb[j] for j in range(4)]
                        pgate, pgateb = self.ps[4], self.psb[4]
                        S.op("pe", lambda e_: e_.matmul(pgate[:, :n], lhsT=sel[:, e, :], rhs=GT[:, s0 + b0:s0 + b0 + n], start=True, stop=True),
                             reads=[selb, GTb], writes=[pgateb])
                        ai = []
                        for hc in range(2):
                            s_ = self.nxt("sgt", 2)
                            S.op("act", lambda e_: e_.activation(out=sgt[s_][:, :n], in_=pg[hc][:, :n], func=AF.Silu),
                                 reads=[pgb[hc]], writes=[sgtb[s_]])
                            t_ = self.nxt("tt", 2)
                            S.op("dve", lambda e_: e_.tensor_tensor(out=tt_[t_][:, :n], in0=pg[2 + hc][:, :n], in1=sgt[s_][:, :n], op=ALU.mult),
                                 reads=[pgb[2 + hc], sgtb[s_]], writes=[ttb[t_]])
                            a_ = self.nxt("aT", 4)
                            S.op("dve", lambda e_: e_.tensor_tensor(out=aT[a_][:, :n], in0=pgate[:, :n], in1=tt_[t_][:, :n], op=ALU.mult),
                                 reads=[pgateb, ttb[t_]], writes=[aTb[a_]])
                            ai.append(a_)
                        return ai

                    def down_part(e, b, ai, ocs):
                        wi = e % NW
                        b0 = b * 512
                        n = min(512, sn - b0)
                        for oc in ocs:
                            pdi = 5 + self.nxt("p3d", 3)
                            pd, pdb = self.ps[pdi], self.psb[pdi]
                            for hc in range(2):
                                S.op("pe", lambda e_: e_.matmul(
                                    pd[:, :n], lhsT=wdn[wi][:, hc, oc * 128:(oc + 1) * 128], rhs=aT[ai[hc]][:, :n],
                                    start=(hc == 0), stop=(hc == 1)), reads=[wdnb[wi], aTb[ai[hc]]], writes=[pdb])
                            if e == 0:
                                S.op("dve", lambda e_: e_.tensor_copy(out=acc[:, oc, b0:b0 + n], in_=pd[:, :n]),
                                     reads=[pdb], writes=[accb[b]])
                            else:
                                S.op("dve", lambda e_: e_.tensor_tensor(out=acc[:, oc, b0:b0 + n], in0=pd[:, :n], in1=acc[:, oc, b0:b0 + n], op=ALU.add),
                                     reads=[pdb, accb[b]], writes=[accb[b]])

                    loadw(0)
                    loadw(1)
                    loadw(2)
                    prev = None
                    for (e, b) in units:
                        for gidx in range(4):
                            gu_group(e, b, gidx)
                            if prev is not None:
                                down_part(prev[0], prev[1], prev[2], (2 * gidx, 2 * gidx + 1))
                        if prev is not None and prev[0] != e and prev[0] + NW < NE:
                            loadw(prev[0] + NW)
                        ai = chain(e, b)
                        prev = (e, b, ai)
                    down_part(prev[0], prev[1], prev[2], range(8))
                    for b in range(nb):
                        b0 = b * 512
                        n = min(512, sn - b0)
                        tq = 0 if s0 + b0 < SEQ else 1
                        S.dma("sp", x1[:, :, :n], xdst[:, :, s0 + b0:s0 + b0 + n], writes=[x1b])
                        for oc in range(8):
                            S.op("dve", lambda e_, oc=oc: e_.scalar_tensor_tensor(
                                out=x1[:, oc, :n], in0=acc[:, oc, b0:b0 + n], scalar=self.modcol(L, 5, oc, tq), in1=x1[:, oc, :n], op0=ALU.mult, op1=ALU.add),
                                reads=[accb[b], x1b, self.mod_buf], writes=[x1b])
                        if final:
                            if self.last:
                                if s0 + b0 < SEQ:
                                    S.dma("sp", outd[:, :, s0 + b0:s0 + b0 + n], x1[:, :, :n], reads=[x1b])
                            else:
                                S.dma("sp", outd[:, :, s0 + b0:s0 + b0 + n], x1[:, :, :n], reads=[x1b])
                        else:
                            S.dma("sp", xdst[:, :, s0 + b0:s0 + b0 + n], x1[:, :, :n], reads=[x1b])

    def topk(self, pr, prb, brt, brb, rt, rtb, nt):
        S = self.S
        X = mybir.AxisListType.X

        def op(fn, extra=()):
            S.op("dve", fn, reads=[rtb] + list(extra), writes=[rtb])

        def bc(ap, w):
            return ap.unsqueeze(2).to_broadcast([128, nt, w])

        Lg = rt[:, 0:nt, 0:36]
        S.op("dve", lambda e: e.tensor_tensor(out=Lg, in0=pr[:, 0:nt * 36].rearrange("p (t c) -> p t c", c=36),
                                              in1=brt[:, :].unsqueeze(1).to_broadcast([128, nt, 36]), op=ALU.add),
             reads=[prb, brb], writes=[rtb])
        lg, le = rt[:, 0:nt, 0:4], rt[:, 0:nt, 4:36]
        gmax, gsum, gw = rt[:, 0:nt, 40], rt[:, 0:nt, 42], rt[:, 0:nt, 43]
        m1, m2, dd, g1, g2 = rt[:, 0:nt, 52], rt[:, 0:nt, 53], rt[:, 0:nt, 54], rt[:, 0:nt, 55], rt[:, 0:nt, 56]
        gexp, pen = rt[:, 0:nt, 44:48], rt[:, 0:nt, 48:52]
        lem, mask1, mask2, lem2 = rt[:, 0:nt, 100:132], rt[:, 0:nt, 132:164], rt[:, 0:nt, 164:196], rt[:, 0:nt, 196:228]
        G = rt[:, 0:nt, 64:96]
        op(lambda e: e.tensor_reduce(out=gmax, in_=lg, axis=X, op=ALU.max))
        op(lambda e: e.tensor_tensor(out=gexp, in0=lg, in1=bc(gmax, 4), op=ALU.subtract))
        S.op("act", lambda e: e.activation(out=gexp, in_=gexp, func=AF.Exp), reads=[rtb], writes=[rtb])
        op(lambda e: e.tensor_reduce(out=gsum, in_=gexp, axis=X, op=ALU.add))
        op(lambda e: e.reciprocal(out=gw, in_=gsum))
        op(lambda e: e.tensor_tensor(out=pen, in0=lg, in1=bc(gmax, 4), op=ALU.is_equal))
        op(lambda e: e.tensor_scalar(out=pen, in0=pen, scalar1=-1.0, scalar2=30000.0, op0=ALU.add, op1=ALU.mult))
        op(lambda e: e.tensor_tensor(out=lem.rearrange("p t (g x) -> p t g x", g=4), in0=le.rearrange("p t (g x) -> p t g x", g=4),
                                     in1=pen.unsqueeze(3).to_broadcast([128, nt, 4, 8]), op=ALU.add))
        op(lambda e: e.tensor_reduce(out=m1, in_=lem, axis=X, op=ALU.max))
        op(lambda e: e.tensor_tensor(out=mask1, in0=lem, in1=bc(m1, 32), op=ALU.is_equal))
        op(lambda e: e.scalar_tensor_tensor(out=lem2, in0=mask1, scalar=-30000.0, in1=lem, op0=ALU.mult, op1=ALU.add))
        op(lambda e: e.tensor_reduce(out=m2, in_=lem2, axis=X, op=ALU.max))
        op(lambda e: e.tensor_tensor(out=mask2, in0=lem2, in1=bc(m2, 32), op=ALU.is_equal))
        op(lambda e: e.tensor_tensor(out=dd, in0=m2, in1=m1, op=ALU.subtract))
        S.op("act", lambda e: e.activation(out=dd, in_=dd, func=AF.Exp), reads=[rtb], writes=[rtb])
        op(lambda e: e.tensor_scalar(out=dd, in0=dd, scalar1=1.0, scalar2=None, op0=ALU.add))
        op(lambda e: e.reciprocal(out=dd, in_=dd))
        op(lambda e: e.tensor_tensor(out=g1, in0=dd, in1=gw, op=ALU.mult))
        op(lambda e: e.tensor_tensor(out=g2, in0=gw, in1=g1, op=ALU.subtract))
        op(lambda e: e.tensor_tensor(out=G, in0=mask1, in1=bc(g1, 32), op=ALU.mult))
        op(lambda e: e.tensor_tensor(out=mask2, in0=mask2, in1=bc(g2, 32), op=ALU.mult))
        op(lambda e: e.tensor_tensor(out=G, in0=G, in1=mask2, op=ALU.add))


def _rope_tables():
    t = np.arange(SEQ)
    rows = (t // 64).astype(np.float32)
    cols = (t % 64).astype(np.float32)

    def ang(rot_dim):
        nf = rot_dim // 4
        inv = (np.float32(10000.0) ** (-np.arange(nf, dtype=np.float32) / np.float32(nf))).astype(np.float32)
        return np.concatenate([rows[:, None] * inv, cols[:, None] * inv], axis=-1).astype(np.float32)

    a64 = ang(64)
    r64 = np.zeros((2, 128, T), np.float32)
    r64[0, :, SEQ:] = 1.0
    idx = (np.arange(128) % 64) % 32
    r64[0, :, :SEQ] = np.cos(a64).T[idx]
    r64[1, :, :SEQ] = np.sin(a64).T[idx]
    a32 = ang(32)
    rm = np.zeros((2, 96, T), np.float32)
    rm[0] = 1.0
    idx = np.arange(32) % 16
    rm[0, 64:96, :SEQ] = np.cos(a32).T[idx]
    rm[1, 64:96, :SEQ] = np.sin(a32).T[idx]
    return r64, rm


def _consts():
    c = np.zeros((128, 8, 128), np.float32)
    c[:, 0, :] = 1.0
    for b in (0, 64):
        c[b:b + 64, 1, b:b + 64] = 1.0 / 64
        for m in range(64):
            if m < 32:
                c[b + m + 32, 2, b + m] = -1.0
            else:
                c[b + m - 32, 2, b + m] = 1.0
    c[:, 3, :] = np.eye(128, dtype=np.float32)
    c[:, 4, :] = 1.0 / 128
    c[0:64, 5, 0:64] = 1.0 / 64
    c[64:96, 5, 64:96] = 1.0 / 32
    for m in range(32):
        if m < 16:
            c[64 + m + 16, 6, 64 + m] = -1.0
            c[m + 16, 7, m] = -1.0
        else:
            c[64 + m - 16, 6, 64 + m] = 1.0
            c[m - 16, 7, m] = 1.0
    return c


def _na_bias(rpb):
    out = np.full((16, 3, 128, 6, 512), NEG, np.float32)
    rl = np.arange(8)[:, None].repeat(64, 1).reshape(-1)
    c = np.arange(64)[None, :].repeat(8, 0).reshape(-1)
    krl = np.arange(2)[:, None].repeat(64, 1).reshape(-1)
    kc = np.arange(64)[None, :].repeat(2, 0).reshape(-1)
    for pi, r0 in enumerate((0, 8, 56)):
        kr0 = min(max(r0 - 4, 0), 52)
        r = r0 + rl
        row0 = np.clip(r - 4, 0, 56)
        col0 = np.clip(c - 8, 0, 48)
        for j in range(6):
            kr = kr0 + 2 * j + krl
            vr = (kr[:, None] >= row0[None, :]) & (kr[:, None] < row0[None, :] + 8)
            vc = (kc[:, None] >= col0[None, :]) & (kc[:, None] < col0[None, :] + 16)
            valid = vr & vc
            idx = (kr[:, None] - r[None, :] + 7) * 31 + (kc[:, None] - c[None, :] + 15)
            idx = np.where(valid, idx, 0)
            g = rpb[:, idx]
            out[:, pi, :, j, :] = np.where(valid[None], g, np.float32(NEG))
    return out


def _sw_mask():
    kl = np.arange(128)[:, None, None]
    j = np.arange(6)[None, :, None]
    ql = np.arange(512)[None, None, :]
    return (np.abs(ql - (kl + 128 * (j - 1))) <= 128).astype(np.float32)


def _col(v):
    return np.ascontiguousarray(v.reshape(-1, 128).T)


def _prep_shared(inp, layers):
    f = lambda a: np.ascontiguousarray(np.asarray(a, dtype=np.float32))
    kinds = set(l % 4 for l in layers)
    d = {}
    for l in layers:
        d[f"ada_w{l}"] = f(inp["ada_w"])[l]
        d[f"w_gu{l}"] = f(inp["moe_w_gate_up"])[l]
        d[f"w_dn{l}"] = f(inp["moe_w_down"])[l]
    ab = np.stack([_col(f(inp["ada_b"])[l]) for l in range(DEPTH)])
    d["ada_b"] = np.ascontiguousarray(np.repeat(ab[..., None], 2, axis=-1))
    ng = np.stack([np.stack([_col(f(inp["norm_g"])[l, i]) for i in range(2)]) for l in range(DEPTH)])
    d["norm_g"] = np.ascontiguousarray(np.repeat(ng[..., None], 2, axis=-1))
    d["wr"] = np.ascontiguousarray(np.concatenate([f(inp["moe_w_router_group"]), f(inp["moe_w_router_expert"])], axis=-1))
    br = np.concatenate([f(inp["moe_b_router_group"]), f(inp["moe_b_router_expert"])], axis=-1)
    d["br"] = np.ascontiguousarray(np.broadcast_to(br[:, None, :], (DEPTH, 128, 36)))
    d["consts"] = _consts()
    sel = np.zeros((32, NE, 128), np.float32)
    for e in range(NE):
        sel[e, e, :] = 1.0
    d["sel"] = sel
    r64, rm = _rope_tables()
    if 0 in kinds:
        d["na_w_qkv"] = f(inp["na_w_qkv"])[0]
        d["na_w_o"] = f(inp["na_w_o"])[0]
        d["na_qk_g"] = np.ascontiguousarray(np.stack([np.tile(f(inp["na_q_norm"])[0], 2), np.tile(f(inp["na_k_norm"])[0], 2)], axis=1))
        d["na_bias"] = _na_bias(f(inp["na_rpb"])[0])
    if 1 in kinds:
        d["sw_w_qkv"] = f(inp["sw_w_qkv"])[0]
        d["sw_w_o"] = f(inp["sw_w_o"])[0]
        d["sw_qk_g"] = np.ascontiguousarray(np.stack([np.tile(f(inp["sw_q_norm"])[0], 2), np.tile(f(inp["sw_k_norm"])[0], 2)], axis=1))
        d["sw_sink"] = np.ascontiguousarray(np.broadcast_to(f(inp["sw_sink"])[0][None, :], (128, 16)))
        d["sw_mask"] = _sw_mask()
    if 1 in kinds or 3 in kinds:
        d["rope64"] = r64
    if 2 in kinds:
        d["mla_w_dqkv"] = f(inp["mla_w_dqkv"])[0]
        d["mla_w_uq"] = f(inp["mla_w_uq"])[0]
        d["mla_w_ukv"] = f(inp["mla_w_ukv"])[0]
        d["mla_w_o"] = f(inp["mla_w_o"])[0]
        mg = np.zeros((128, 8), np.float32)
        mg[:, 0:3] = _col(f(inp["mla_q_a_norm"])[0])
        mg[:, 3:5] = _col(f(inp["mla_kv_a_norm"])[0])
        mg[:96, 5] = f(inp["mla_q_norm"])[0]
        mg[:, 6] = np.tile(f(inp["mla_k_norm"])[0][:64], 2)
        mg[:32, 7] = f(inp["mla_k_norm"])[0][64:96]
        d["mla_g"] = mg
        d["rope_mla"] = rm
    if 3 in kinds:
        d["diff_w_qkv"] = f(inp["diff_w_qkv"])[0]
        d["diff_w_o"] = f(inp["diff_w_o"])[0]
        d["diff_qk_g"] = np.ascontiguousarray(np.stack([np.tile(f(inp["diff_q_norm"])[0], 2), np.tile(f(inp["diff_k_norm"])[0], 2)], axis=1))
        d["diff_lam"] = np.ascontiguousarray(np.broadcast_to(f(inp["diff_lambda"])[0][None], (128, 4, 64)))
        d["diff_subln"] = np.ascontiguousarray(f(inp["diff_subln"])[0][:, None])
    return d


def _cc(inp, b):
    c = np.asarray(inp["c"], np.float32)[b]
    cx = np.asarray(inp["c_ctx"], np.float32)
    return np.ascontiguousarray(np.stack([_col(c), _col(cx)], axis=-1))


_CACHE = {}


def _get_builder(layers, last):
    key = (tuple(layers), last)
    if key not in _CACHE:
        _CACHE[key] = Builder(list(layers), layers[0] == 0, last)
    return _CACHE[key]


def run_layers(inp, layers, xT_list, cores):
    last = layers[-1] == DEPTH - 1
    B = _get_builder(layers, last)
    shared = _prep_shared(inp, layers)
    in_maps = []
    for ci, b in enumerate(cores):
        m = {"xT_in": np.ascontiguousarray(xT_list[ci]), "cc": _cc(inp, b)}
        for k in B.in_names:
            if k not in m:
                m[k] = shared[k]
        in_maps.append(m)
    res = run_bass_kernel_spmd(B.nc, in_maps, core_ids=list(range(len(cores))))
    name = "outT" if last else "xT_out"
    return [np.asarray(r[name]) for r in res.results]


FUSED = True


def kernel(**inp):
    x = np.asarray(inp["x"], np.float32)
    ctx = np.asarray(inp["ctx"], np.float32)
    nb = x.shape[0]
    xT = [np.ascontiguousarray(np.concatenate([x[b].T, ctx[b].T], axis=1)) for b in range(nb)]
    cores = list(range(nb))
    if FUSED:
        outs = run_layers(inp, [0, 1, 2, 3], xT, cores)
    else:
        cur = xT
        for L in range(DEPTH):
            cur = run_layers(inp, [L], cur, cores)
        outs = cur
    return np.ascontiguousarray(np.stack([o.T for o in outs], axis=0)).astype(np.float32)
```

```python
import math
from contextlib import ExitStack
import numpy as np
import concourse.bass as bass
import concourse.mybir as mybir
from concourse.bass_utils import run_bass_kernel_spmd

F32 = mybir.dt.float32
BF16 = mybir.dt.bfloat16
AF = mybir.ActivationFunctionType
ALU = mybir.AluOpType

D = 1024
SEQ = 4096
NCTX = 256
T = SEQ + NCTX
NKT = T // 128
DEPTH = 4
EPS = 1e-6
NEG = -30000.0
NE = 32


class Buf:
    __slots__ = ("ap", "w", "r")

    def __init__(self, ap):
        self.ap = ap
        self.w = {}
        self.r = {}


class Sched:
    NR = 8
    EPOCH = 1000000

    def __init__(self, nc):
        self.nc = nc
        self.E = {"pe": nc.tensor, "act": nc.scalar, "dve": nc.vector, "pool": nc.gpsimd, "sp": nc.sync}
        self.sems = {}
        self.ckey = {}
        self.ccnt = {}
        self.cep = {}
        for e in ("pe", "act", "dve", "pool"):
            self.cep[e] = 0
            self._new_epoch(e)
        self.seen = {e: {} for e in self.E}
        self.dkeys = {}
        self.dcnt = {}
        for q in ("sp", "pool", "act"):
            ks = []
            for i in range(self.NR):
                k = f"d_{q}_{i}"
                self.sems[k] = nc.alloc_semaphore(k)
                ks.append(k)
            self.dkeys[q] = ks
            self.dcnt[q] = 0
        self.dlast = {}
        self.n_inst = 0

    def _new_epoch(self, e):
        k = f"c_{e}_{self.cep[e]}"
        self.cep[e] += 1
        self.sems[k] = self.nc.alloc_semaphore(k)
        self.ckey[e] = k
        self.ccnt[e] = 0

    def _wait(self, e, k, v):
        if self.seen[e].get(k, 0) >= v:
            return
        self.E[e].wait_ge(self.sems[k], v)
        self.seen[e][k] = v

    def _collect(self, e, reads, writes, is_dma):
        own = f"c_{e}_"
        need = {}
        for b in reads:
            for k, v in b.w.items():
                if (not is_dma) and e == "pe" and k.startswith(own):
                    continue
                if need.get(k, 0) < v:
                    need[k] = v
        for b in writes:
            for src in (b.w, b.r):
                for k, v in src.items():
                    if (not is_dma) and k.startswith(own):
                        continue
                    if need.get(k, 0) < v:
                        need[k] = v
        for k, v in need.items():
            self._wait(e, k, v)

    def _commit(self, k, v, reads, writes):
        for b in reads:
            if b.r.get(k, 0) < v:
                b.r[k] = v
        for b in writes:
            if b.r:
                b.w = {k: v}
                b.r = {}
            else:
                b.w[k] = v

    def op(self, e, fn, reads=(), writes=()):
        self._collect(e, reads, writes, False)
        inst = fn(self.E[e])
        if self.ccnt[e] >= self.EPOCH:
            self._new_epoch(e)
        self.ccnt[e] += 1
        k, v = self.ckey[e], self.ccnt[e]
        inst.then_inc(self.sems[k], 1)
        self._commit(k, v, reads, writes)
        self.n_inst += 1

    def dma(self, q, out, in_, reads=(), writes=()):
        self._collect(q, reads, writes, True)
        i = self.dcnt[q]
        self.dcnt[q] += 1
        k = self.dkeys[q][i % self.NR]
        prev = 16 * (i // self.NR)
        if prev:
            self._wait(q, k, prev)
        inst = self.E[q].dma_start(out=out, in_=in_)
        inst.then_inc(self.sems[k], 16)
        v = prev + 16
        assert v < 60000
        self.dlast[k] = v
        self._commit(k, v, reads, writes)
        self.n_inst += 1

    def barrier(self):
        tick = {}
        for e in ("pe", "act", "dve", "pool"):
            if self.ccnt[e]:
                tick[self.ckey[e]] = self.ccnt[e]
        tick.update(self.dlast)
        for e in self.E:
            for k, v in tick.items():
                self._wait(e, k, v)


def _token_blocks(with_ctx=True):
    bl = [(i * 512, 512) for i in range(SEQ // 512)]
    if with_ctx:
        bl.append((SEQ, NCTX))
    return bl


class Builder:
    topk_eng = "dve"

    def __init__(self, layers, first, last):
        self.layers = layers
        self.first = first
        self.last = last
        nc = bass.Bass("TRN2", target_bir_lowering=False)
        self.nc = nc
        self.S = Sched(nc)
        self.rot = {}
        self._decl_dram()
        self._build()

    def din(self, name, shape, dt=F32, kinds=None):
        if kinds is not None and not (set(kinds) & set(l % 4 for l in self.layers)):
            return None
        if not hasattr(self, "in_names"):
            self.in_names = []
        self.in_names.append(name)
        return self.nc.dram_tensor(name, list(shape), dt, kind="ExternalInput").ap()

    def dscr(self, name, shape, dt):
        return self.nc.dram_tensor(name, list(shape), dt, kind="Internal").ap()

    def sb(self, es, name, shape, dt):
        self._uid = getattr(self, "_uid", 0) + 1
        t = es.enter_context(self.nc.sbuf_tensor(f"{name}_u{self._uid}", list(shape), dt))
        return t

    def nxt(self, key, n):
        i = self.rot.get(key, 0)
        self.rot[key] = i + 1
        return i % n

    def _decl_dram(self):
        nc = self.nc
        self.x_in = self.din("xT_in", [D, T])
        self.cc = self.din("cc", [128, 8, 2])
        self.ada_w = {l: self.din(f"ada_w{l}", [D, 6 * D]) for l in self.layers}
        self.ada_b = self.din("ada_b", [DEPTH, 128, 48, 2])
        self.norm_g = self.din("norm_g", [DEPTH, 2, 128, 8, 2])
        self.wr = self.din("wr", [DEPTH, D, 36])
        self.br = self.din("br", [DEPTH, 128, 36])
        self.w_gu = {l: self.din(f"w_gu{l}", [NE, D, 512]) for l in self.layers}
        self.w_dn = {l: self.din(f"w_dn{l}", [NE, 256, D]) for l in self.layers}
        self.wqkv = {0: self.din("na_w_qkv", [D, 3072], kinds=[0]), 1: self.din("sw_w_qkv", [D, 1536], kinds=[1]),
                     3: self.din("diff_w_qkv", [D, 3072], kinds=[3])}
        self.wo = {0: self.din("na_w_o", [D, D], kinds=[0]), 1: self.din("sw_w_o", [D, D], kinds=[1]),
                   2: self.din("mla_w_o", [D, D], kinds=[2]), 3: self.din("diff_w_o", [D, D], kinds=[3])}
        self.qkg = {0: self.din("na_qk_g", [128, 2], kinds=[0]), 1: self.din("sw_qk_g", [128, 2], kinds=[1]),
                    3: self.din("diff_qk_g", [128, 2], kinds=[3])}
        self.na_bias = self.din("na_bias", [16, 3, 128, 6, 512], kinds=[0])
        self.sw_sink = self.din("sw_sink", [128, 16], kinds=[1])
        self.sw_mask = self.din("sw_mask", [128, 6, 512], kinds=[1])
        self.rope64 = self.din("rope64", [2, 128, T], kinds=[1, 3])
        self.mla_w_dqkv = self.din("mla_w_dqkv", [D, 672], kinds=[2])
        self.mla_w_uq = self.din("mla_w_uq", [384, 1536], kinds=[2])
        self.mla_w_ukv = self.din("mla_w_ukv", [256, 2048], kinds=[2])
        self.mla_g = self.din("mla_g", [128, 8], kinds=[2])
        self.rope_mla = self.din("rope_mla", [2, 96, T], kinds=[2])
        self.diff_lam = self.din("diff_lam", [128, 4, 64], kinds=[3])
        self.diff_subln = self.din("diff_subln", [128, 1], kinds=[3])
        self.consts = self.din("consts", [128, 8, 128])
        self.sel_in = self.din("sel", [32, NE, 128])
        self.xs = self.dscr("xs", [D, T], F32)
        self.q_s = self.dscr("q_s", [16, 96, T], BF16)
        self.k_s = self.dscr("k_s", [16, 96, T], BF16)
        self.v_s = self.dscr("v_s", [16, 128, NKT, 64], BF16)
        self.kr_s = self.dscr("kr_s", [32, T], BF16)
        self.v_s2 = self.dscr("v_s2", [8, 128, NKT, 128], BF16)
        self.at_s = self.dscr("at_s", [D, T], BF16)
        self.h2_s = self.dscr("h2_s", [D, T], BF16)
        if self.last:
            self.out = nc.dram_tensor("outT", [D, SEQ], F32, kind="ExternalOutput").ap()
        else:
            self.out = nc.dram_tensor("xT_out", [D, T], F32, kind="ExternalOutput").ap()

    def _build(self):
        nc, S = self.nc, self.S
        with ExitStack() as es:
            self.ps = [nc.alloc_psum_tensor(f"ps{i}", [128, 512], F32) for i in range(8)]
            self.psb = [Buf(p) for p in self.ps]
            cst = self.sb(es, "cst_f", [128, 8, 128], F32)
            self.cst_f = cst
            cb = Buf(cst)
            S.dma("sp", cst[:, :, :], self.consts, writes=[cb])
            cstb = self.sb(es, "cst_b", [128, 8, 128], BF16)
            self.cstb_buf = Buf(cstb)
            self._cstb = cstb
            self.eps_col = self.sb(es, "eps_col", [128, 1], F32)
            self.eps_buf = Buf(self.eps_col)
            S.op("dve", lambda e: e.memset(self.eps_col[:, :], EPS), writes=[self.eps_buf])
            S.op("dve", lambda e: e.tensor_copy(out=cstb[:, :, :], in_=cst[:, :, :]), reads=[cb], writes=[self.cstb_buf])
            self.cst_buf = cb
            self.modT = self.sb(es, "modT", [128, DEPTH, 48, 2], F32)
            self.gsT = self.sb(es, "gsT", [128, DEPTH, 2, 8, 2], F32)
            self.mod_buf = Buf(self.modT)
            self.gs_buf = Buf(self.gsT)
            self.phase0()
            x_src = self.x_in
            for L in self.layers:
                kind = L % 4
                need_ctx = L < DEPTH - 1
                is_last_layer = (L == self.layers[-1])
                x_dst = self.xs
                S.barrier()
                with nc.named_scope(f"L{L}_p1"):
                    if kind == 2:
                        self.phase1_mla(L, x_src)
                    else:
                        self.phase1(L, x_src)
                    S.barrier()
                import os as _os
                _stop = int(_os.environ.get("K_STOP", "9"))
                with nc.named_scope(f"L{L}_p2"):
                    if _stop >= 2:
                        self.phase2(L, need_ctx)
                    S.barrier()
                if _stop >= 3:
                    self.phase3(L, x_src, x_dst, need_ctx, final=(is_last_layer))
                x_src = self.xs
            S.barrier()

    def phase0(self):
        nc, S = self.nc, self.S
        with ExitStack() as es:
            cc = self.sb(es, "cc", [128, 8, 2], F32)
            sc = self.sb(es, "scc", [128, 8, 2], F32)
            ccb, scb = Buf(cc), Buf(sc)
            S.dma("sp", cc[:, :, :], self.cc, writes=[ccb])
            S.op("act", lambda e: e.activation(out=sc[:, :, :], in_=cc[:, :, :], func=AF.Silu), reads=[ccb], writes=[scb])
            wm = [self.sb(es, f"wm{i}", [128, 8, 1024], F32) for i in range(2)]
            wmb = [Buf(w) for w in wm]
            adab = self.sb(es, "adab", [128, 48, 2], F32)
            adabb = Buf(adab)
            gn = self.sb(es, "gn", [128, 2, 8, 2], F32)
            gnb = Buf(gn)
            pm = self.ps[0]
            pmb = self.psb[0]
            for L in self.layers:
                S.dma("sp", adab[:, :, :], self.ada_b[L], writes=[adabb])
                S.dma("sp", gn[:, :, :, :], self.norm_g[L].rearrange("i p k t -> p i k t"), writes=[gnb])
                for which in range(6):
                    i = self.nxt("wm", 2)
                    src = self.ada_w[L].rearrange("(kc p) n -> p kc n", p=128)[:, :, which * 1024:(which + 1) * 1024]
                    S.dma("sp", wm[i][:, :, :], src, writes=[wmb[i]])
                    for fc in range(8):
                        j = which * 8 + fc
                        for kc in range(8):
                            S.op("pe", lambda e, i=i, fc=fc, kc=kc, j=j: e.matmul(
                                pm[:, 2 * j:2 * j + 2], lhsT=wm[i][:, kc, fc * 128:(fc + 1) * 128], rhs=sc[:, kc, :],
                                start=(kc == 0), stop=(kc == 7)), reads=[wmb[i], scb], writes=[pmb])
                mo = self.modT[:, L, :, :]
                S.op("dve", lambda e: e.tensor_tensor(out=mo, in0=pm[:, 0:96].rearrange("p (j t) -> p j t", t=2),
                                                      in1=adab[:, :, :], op=ALU.add),
                     reads=[pmb, adabb], writes=[self.mod_buf])
                for i2, sci in ((0, 1), (1, 4)):
                    g = self.gsT[:, L, i2, :, :]
                    S.op("dve", lambda e, g=g, sci=sci: e.tensor_scalar(
                        out=g, in0=self.modT[:, L, sci * 8:(sci + 1) * 8, :], scalar1=1.0, scalar2=None, op0=ALU.add),
                        reads=[self.mod_buf], writes=[self.gs_buf])
                    S.op("dve", lambda e, g=g, i2=i2: e.tensor_tensor(out=g, in0=g, in1=gn[:, i2, :, :], op=ALU.mult),
                         reads=[self.gs_buf, gnb], writes=[self.gs_buf])
            S.barrier()

    def modcol(self, L, which, fc, t):
        return self.modT[:, L, which * 8 + fc, t:t + 1]

    def gscol(self, L, i2, fc, t):
        return self.gsT[:, L, i2, fc, t:t + 1]

    def norm_mod(self, L, i2, xin, xb, n, t, rs, rsb, sq, sqb, tmp, tmpb, outs, pss):
        S = self.S
        ones_b = self.cstb[:, 0, :]
        pbank, pbuf = self.ps[pss], self.psb[pss]
        for kc in range(8):
            j = self.nxt("sq", 2)
            S.op("act", lambda e, j=j, kc=kc: e.activation(out=sq[j][:, :n], in_=xin[:, kc, :n], func=AF.Square),
                 reads=[xb], writes=[sqb[j]])
            S.op("pe", lambda e, j=j, kc=kc: e.matmul(pbank[:, :n], lhsT=ones_b, rhs=sq[j][:, :n],
                                                     start=(kc == 0), stop=(kc == 7)),
                 reads=[sqb[j], self.cstb_buf], writes=[pbuf])
        S.op("act", lambda e: e.activation(out=rs[:, :n], in_=pbank[:, :n], func=AF.Ln, bias=self.eps_col[:, 0:1], scale=1.0 / D),
             reads=[pbuf, self.eps_buf], writes=[rsb])
        S.op("act", lambda e: e.activation(out=rs[:, :n], in_=rs[:, :n], func=AF.Exp, scale=-0.5), reads=[rsb], writes=[rsb])
        sh = 0 if i2 == 0 else 3
        for kc in range(8):
            j = self.nxt("tmp", 2)
            S.op("dve", lambda e, j=j, kc=kc: e.tensor_tensor(out=tmp[j][:, :n], in0=xin[:, kc, :n], in1=rs[:, :n], op=ALU.mult),
                 reads=[xb, rsb], writes=[tmpb[j]])
            first = True
            for (ot, ob, eng) in outs:
                if first:
                    S.op("act", lambda e, j=j, kc=kc, ot=ot: e.activation(
                        out=ot[:, kc, :n], in_=tmp[j][:, :n], func=AF.Identity,
                        scale=self.gscol(L, i2, kc, t), bias=self.modcol(L, sh, kc, t)),
                        reads=[tmpb[j], self.gs_buf, self.mod_buf], writes=[ob])
                    first = False
                    prev_t, prev_b = ot, ob
                else:
                    S.op(eng, lambda e, kc=kc, ot=ot, prev_t=prev_t: e.tensor_copy(out=ot[:, kc, :n], in_=prev_t[:, kc, :n]),
                         reads=[prev_b], writes=[ob])

    @property
    def cstb(self):
        return self._cstb

    def phase1(self, L, x_src):
        nc, S = self.nc, self.S
        kind = L % 4
        ncol = {0: 3072, 1: 1536, 3: 3072}[kind]
        nq_ch = 8
        nk_ch = {0: 8, 1: 2, 3: 8}[kind]
        kcol0 = 1024
        vcol0 = {0: 2048, 1: 1280, 3: 2048}[kind]
        vw = {0: 1024, 1: 256, 3: 1024}[kind]
        dv = {0: 64, 1: 64, 3: 128}[kind]
        rope = kind in (1, 3)
        with ExitStack() as es:
            W = self.sb(es, "w1", [128, 8, ncol], BF16)
            Wb = [Buf(W) for _ in range(8)]
            wsrc = self.wqkv[kind].rearrange("(kc p) n -> p kc n", p=128)
            for kc in range(8):
                S.dma("pool", W[:, kc, :], wsrc[:, kc, :], writes=[Wb[kc]])
            qkg = self.sb(es, "qkg", [128, 2], F32)
            qkgb = Buf(qkg)
            S.dma("sp", qkg[:, :], self.qkg[kind], writes=[qkgb])
            xin = [self.sb(es, f"xin{i}", [128, 8, 512], F32) for i in range(2)]
            xinb = [Buf(t) for t in xin]
            hT = [self.sb(es, f"hT{i}", [128, 8, 512], BF16) for i in range(2)]
            hTb = [Buf(t) for t in hT]
            rs = self.sb(es, "rs", [128, 512], F32); rsb = Buf(rs)
            sq = [self.sb(es, f"sq{i}", [128, 512], BF16) for i in range(2)]; sqb = [Buf(t) for t in sq]
            tmp = [self.sb(es, f"tmp{i}", [128, 512], F32) for i in range(2)]; tmpb = [Buf(t) for t in tmp]
            rs2 = [self.sb(es, f"rs2{i}", [128, 512], F32) for i in range(2)]; rs2b = [Buf(t) for t in rs2]
            qn = [self.sb(es, f"qn{i}", [128, 512], BF16) for i in range(3)]; qnb = [Buf(t) for t in qn]
            qo = [self.sb(es, f"qo{i}", [128, 512], BF16) for i in range(3)]; qob = [Buf(t) for t in qo]
            t1 = [self.sb(es, f"t1{i}", [128, 512], F32) for i in range(2)]; t1b = [Buf(t) for t in t1]
            t2 = [self.sb(es, f"t2{i}", [128, 512], F32) for i in range(2)]; t2b = [Buf(t) for t in t2]
            vt = [self.sb(es, f"vt{i}", [128, 512], BF16) for i in range(3)]; vtb = [Buf(t) for t in vt]
            if rope:
                cs = [self.sb(es, f"cs{i}", [128, 2, 512], F32) for i in range(2)]
                csb = [Buf(t) for t in cs]
            blocks = _token_blocks(True)
            xsrc = x_src.rearrange("(kc p) t -> p kc t", p=128)

            def load(bi):
                t0, n = blocks[bi]
                i = bi % 2
                S.dma("sp", xin[i][:, :, :n], xsrc[:, :, t0:t0 + n], writes=[xinb[i]])
                if rope:
                    S.dma("sp", cs[i][:, :, :n], self.rope64.rearrange("c p t -> p c t")[:, :, t0:t0 + n], writes=[csb[i]])

            load(0)
            blk64 = self.cstb[:, 1, :]
            Rm = self.cstb[:, 2, :]
            for bi, (t0, n) in enumerate(blocks):
                if bi + 1 < len(blocks):
                    load(bi + 1)
                i = bi % 2
                tq = 0 if t0 < SEQ else 1
                self.norm_mod(L, 0, xin[i], xinb[i], n, tq, rs, rsb, sq, sqb, tmp, tmpb, [(hT[i], hTb[i], "act")], 7)
                nch = nq_ch + nk_ch
                cst = {}
                p1q_banks = [0, 1, 6]

                def stP(c):
                    isq = c < nq_ch
                    col0 = c * 128 if isq else kcol0 + (c - nq_ch) * 128
                    pb = p1q_banks[self.nxt("p1q3", 3)]
                    pq, pqb = self.ps[pb], self.psb[pb]
                    for kc in range(8):
                        S.op("pe", lambda e, kc=kc: e.matmul(
                            pq[:, :n], lhsT=W[:, kc, col0:col0 + 128], rhs=hT[i][:, kc, :n], start=(kc == 0), stop=(kc == 7)),
                            reads=[Wb[kc], hTb[i]], writes=[pqb])
                    cst[c] = dict(pq=pq, pqb=pqb, isq=isq)

                def stN(c):
                    d_ = cst[c]
                    pq, pqb, isq = d_["pq"], d_["pqb"], d_["isq"]
                    j = self.nxt("sq", 2)
                    S.op("act", lambda e: e.activation(out=sq[j][:, :n], in_=pq[:, :n], func=AF.Square),
                         reads=[pqb], writes=[sqb[j]])
                    pmi = 2 + self.nxt("p1m", 2)
                    pm, pmb = self.ps[pmi], self.psb[pmi]
                    S.op("pe", lambda e: e.matmul(pm[:, :n], lhsT=blk64, rhs=sq[j][:, :n], start=True, stop=True),
                         reads=[sqb[j], self.cstb_buf], writes=[pmb])
                    r = self.nxt("rs2", 2)
                    S.op("act", lambda e: e.activation(out=rs2[r][:, :n], in_=pm[:, :n], func=AF.Ln,
                                                       bias=self.eps_col[:, 0:1], scale=1.0),
                         reads=[pmb, self.eps_buf], writes=[rs2b[r]])
                    S.op("act", lambda e: e.activation(out=rs2[r][:, :n], in_=rs2[r][:, :n], func=AF.Exp, scale=-0.5), reads=[rs2b[r]], writes=[rs2b[r]])
                    gcol = qkg[:, 0:1] if isq else qkg[:, 1:2]
                    if rope and tq == 0:
                        a = self.nxt("qn", 3)
                        S.op("dve", lambda e: e.scalar_tensor_tensor(
                            out=qn[a][:, :n], in0=pq[:, :n], scalar=gcol, in1=rs2[r][:, :n], op0=ALU.mult, op1=ALU.mult),
                            reads=[pqb, rs2b[r], qkgb], writes=[qnb[a]])
                        d_["a"] = a
                    else:
                        o = self.nxt("qo", 3)
                        S.op("dve", lambda e: e.scalar_tensor_tensor(
                            out=qo[o][:, :n], in0=pq[:, :n], scalar=gcol, in1=rs2[r][:, :n], op0=ALU.mult, op1=ALU.mult),
                            reads=[pqb, rs2b[r], qkgb], writes=[qob[o]])
                        d_["o"] = o

                def stR(c):
                    d_ = cst[c]
                    isq = d_["isq"]
                    dst_s = self.q_s if isq else self.k_s
                    hh = (c if isq else c - nq_ch) * 2
                    if "a" in d_:
                        a = d_["a"]
                        pri = 4 + self.nxt("p1r", 2)
                        pr, prb = self.ps[pri], self.psb[pri]
                        S.op("pe", lambda e: e.matmul(pr[:, :n], lhsT=Rm, rhs=qn[a][:, :n], start=True, stop=True),
                             reads=[qnb[a], self.cstb_buf], writes=[prb])
                        u = self.nxt("t1", 2)
                        S.op("dve", lambda e: e.tensor_tensor(out=t1[u][:, :n], in0=qn[a][:, :n], in1=cs[i][:, 0, :n], op=ALU.mult),
                             reads=[qnb[a], csb[i]], writes=[t1b[u]])
                        S.op("dve", lambda e: e.tensor_tensor(out=t2[u][:, :n], in0=pr[:, :n], in1=cs[i][:, 1, :n], op=ALU.mult),
                             reads=[prb, csb[i]], writes=[t2b[u]])
                        o = self.nxt("qo", 3)
                        S.op("pool", lambda e: e.tensor_tensor(out=qo[o][:, :n], in0=t1[u][:, :n], in1=t2[u][:, :n], op=ALU.add),
                             reads=[t1b[u], t2b[u]], writes=[qob[o]])
                    else:
                        o = d_["o"]
                    for h2 in range(2):
                        S.dma("sp", dst_s[hh + h2, 0:64, t0:t0 + n], qo[o][h2 * 64:(h2 + 1) * 64, :n], reads=[qob[o]])

                for it_ in range(nch + 2):
                    if it_ < nch:
                        stP(it_)
                    if 0 <= it_ - 1 < nch:
                        stN(it_ - 1)
                    if 0 <= it_ - 2 < nch:
                        stR(it_ - 2)
                for tt in range(n // 128):
                    kt = (t0 // 128) + tt
                    for cbk in range((vw + 511) // 512):
                        w = min(512, vw - cbk * 512)
                        pvi = 4 + self.nxt("p1r", 2)
                        pv, pvb = self.ps[pvi], self.psb[pvi]
                        for kc in range(8):
                            S.op("pe", lambda e, kc=kc, pv=pv, tt=tt, cbk=cbk, w=w: e.matmul(
                                pv[:, :w], lhsT=hT[i][:, kc, tt * 128:(tt + 1) * 128],
                                rhs=W[:, kc, vcol0 + cbk * 512: vcol0 + cbk * 512 + w], start=(kc == 0), stop=(kc == 7)),
                                reads=[Wb[kc], hTb[i]], writes=[pvb])
                        vi = self.nxt("vt", 3)
                        S.op("act", lambda e, vi=vi, pv=pv, w=w: e.activation(out=vt[vi][:, :w], in_=pv[:, :w], func=AF.Copy),
                             reads=[pvb], writes=[vtb[vi]])
                        nh = w // dv
                        h0 = cbk * 512 // dv
                        if dv == 64:
                            dst = self.v_s[h0:h0 + nh, :, kt, :].rearrange("h p d -> p h d")
                        else:
                            dst = self.v_s2[h0:h0 + nh, :, kt, :].rearrange("h p d -> p h d")
                        S.dma("sp", dst, vt[vi][:, :w].rearrange("p (h d) -> p h d", d=dv), reads=[vtb[vi]])

    def phase1_mla(self, L, x_src):
        nc, S = self.nc, self.S
        with ExitStack() as es:
            W = self.sb(es, "w1", [128, 8, 672], BF16)
            Wb = [Buf(W) for _ in range(8)]
            wsrc = self.mla_w_dqkv.rearrange("(kc p) n -> p kc n", p=128)
            for kc in range(8):
                S.dma("pool", W[:, kc, :], wsrc[:, kc, :], writes=[Wb[kc]])
            Wuq = self.sb(es, "wuq", [128, 3, 1536], BF16); Wuqb = Buf(Wuq)
            S.dma("pool", Wuq[:, :, :], self.mla_w_uq.rearrange("(kc p) n -> p kc n", p=128), writes=[Wuqb])
            Wuk = self.sb(es, "wuk", [128, 2, 16, 64], BF16); Wukb = Buf(Wuk)
            Wuv = self.sb(es, "wuv", [128, 2, 16, 64], BF16); Wuvb = Buf(Wuv)
            ukv = self.mla_w_ukv.rearrange("(kc p) (h two d) -> p kc h two d", p=128, two=2, d=64)
            for kc in range(2):
                S.dma("pool", Wuk[:, kc, :, :], ukv[:, kc, :, 0, :], writes=[Wukb])
                S.dma("pool", Wuv[:, kc, :, :], ukv[:, kc, :, 1, :], writes=[Wuvb])
            mg = self.sb(es, "mg", [128, 8], F32); mgb = Buf(mg)
            S.dma("sp", mg[:, :], self.mla_g, writes=[mgb])
            xin = [self.sb(es, f"xin{i}", [128, 8, 512], F32) for i in range(2)]
            xinb = [Buf(t) for t in xin]
            hT = [self.sb(es, f"hT{i}", [128, 8, 512], BF16) for i in range(2)]
            hTb = [Buf(t) for t in hT]
            rs = self.sb(es, "rs", [128, 512], F32); rsb = Buf(rs)
            sq = [self.sb(es, f"sq{i}", [128, 512], BF16) for i in range(2)]; sqb = [Buf(t) for t in sq]
            tmp = [self.sb(es, f"tmp{i}", [128, 512], F32) for i in range(2)]; tmpb = [Buf(t) for t in tmp]
            rs2 = [self.sb(es, f"rs2{i}", [128, 512], F32) for i in range(2)]; rs2b = [Buf(t) for t in rs2]
            cf = self.sb(es, "cf", [128, 6, 512], F32); cfb = [Buf(cf) for _ in range(6)]
            cn = self.sb(es, "cn", [128, 5, 512], BF16); cnb = [Buf(cn) for _ in range(5)]
            qn = [self.sb(es, f"qn{i}", [128, 512], BF16) for i in range(3)]; qnb = [Buf(t) for t in qn]
            qo = [self.sb(es, f"qo{i}", [128, 512], BF16) for i in range(3)]; qob = [Buf(t) for t in qo]
            t1 = [self.sb(es, f"t1{i}", [128, 512], F32) for i in range(2)]; t1b = [Buf(t) for t in t1]
            t2 = [self.sb(es, f"t2{i}", [128, 512], F32) for i in range(2)]; t2b = [Buf(t) for t in t2]
            vt = [self.sb(es, f"vt{i}", [128, 512], BF16) for i in range(3)]; vtb = [Buf(t) for t in vt]
            cs = [self.sb(es, f"cs{i}", [96, 2, 512], F32) for i in range(2)]
            csb = [Buf(t) for t in cs]
            self.krt = [self.sb(es, f"krt{i}", [32, 2, 512], F32) for i in range(2)]
            self.krtb = [Buf(t) for t in self.krt]
            blocks = _token_blocks(True)
            xsrc = x_src.rearrange("(kc p) t -> p kc t", p=128)
            ones_b = self.cstb[:, 0, :]
            blk64 = self.cstb[:, 1, :]
            blkm = self.cstb[:, 5, :]
            Rmm = self.cstb[:, 6, :]

            def load(bi):
                t0, n = blocks[bi]
                i = bi % 2
                S.dma("sp", xin[i][:, :, :n], xsrc[:, :, t0:t0 + n], writes=[xinb[i]])
                S.dma("sp", cs[i][:, :, :n], self.rope_mla.rearrange("c p t -> p c t")[:, :, t0:t0 + n], writes=[csb[i]])
                S.dma("sp", self.krt[i][:, :, :n], self.rope_mla.rearrange("c p t -> p c t")[64:96, :, t0:t0 + n], writes=[self.krtb[i]])

            def rsq(pm, pmb, n, P, scale):
                r = self.nxt("rs2", 2)
                S.op("act", lambda e: e.activation(out=rs2[r][:P, :n], in_=pm[:P, :n], func=AF.Ln,
                                                   bias=self.eps_col[:P, 0:1], scale=scale),
                     reads=[pmb, self.eps_buf], writes=[rs2b[r]])
                S.op("act", lambda e: e.activation(out=rs2[r][:P, :n], in_=rs2[r][:P, :n], func=AF.Exp, scale=-0.5), reads=[rs2b[r]], writes=[rs2b[r]])
                return r

            load(0)
            for bi, (t0, n) in enumerate(blocks):
                if bi + 1 < len(blocks):
                    load(bi + 1)
                i = bi % 2
                tq = 0 if t0 < SEQ else 1
                self.norm_mod(L, 0, xin[i], xinb[i], n, tq, rs, rsb, sq, sqb, tmp, tmpb, [(hT[i], hTb[i], "act")], 7)
                for c in range(6):
                    M = 128 if c < 5 else 32
                    pb = self.nxt("p1q", 2)
                    pq, pqb = self.ps[pb], self.psb[pb]
                    for kc in range(8):
                        S.op("pe", lambda e, kc=kc, c=c, pq=pq, M=M: e.matmul(
                            pq[:M, :n], lhsT=W[:, kc, c * 128:c * 128 + M], rhs=hT[i][:, kc, :n], start=(kc == 0), stop=(kc == 7)),
                            reads=[Wb[kc], hTb[i]], writes=[pqb])
                    S.op("act", lambda e, c=c, pq=pq, M=M: e.activation(out=cf[:M, c, :n], in_=pq[:M, :n], func=AF.Copy),
                         reads=[pqb], writes=[cfb[c]])
                for (c0, c1, dim) in ((0, 3, 384), (3, 5, 256)):
                    pmi = 2 + self.nxt("p1m", 2)
                    pm, pmb = self.ps[pmi], self.psb[pmi]
                    for c in range(c0, c1):
                        j = self.nxt("sq", 2)
                        S.op("act", lambda e, j=j, c=c: e.activation(out=sq[j][:, :n], in_=cf[:, c, :n], func=AF.Square),
                             reads=[cfb[c]], writes=[sqb[j]])
                        S.op("pe", lambda e, j=j, c=c, pm=pm: e.matmul(pm[:, :n], lhsT=ones_b, rhs=sq[j][:, :n],
                                                                      start=(c == c0), stop=(c == c1 - 1)),
                             reads=[sqb[j], self.cstb_buf], writes=[pmb])
                    r = rsq(pm, pmb, n, 128, 1.0 / dim)
                    for c in range(c0, c1):
                        S.op("dve", lambda e, c=c, r=r: e.scalar_tensor_tensor(
                            out=cn[:, c, :n], in0=cf[:, c, :n], scalar=mg[:, c:c + 1], in1=rs2[r][:, :n], op0=ALU.mult, op1=ALU.mult),
                            reads=[cfb[c], rs2b[r], mgb], writes=[cnb[c]])
                j = self.nxt("sq", 2)
                S.op("act", lambda e, j=j: e.activation(out=sq[j][:32, :n], in_=cf[:32, 5, :n], func=AF.Square),
                     reads=[cfb[5]], writes=[sqb[j]])
                pmi = 2 + self.nxt("p1m", 2)
                pm, pmb = self.ps[pmi], self.psb[pmi]
                S.op("pe", lambda e, j=j, pm=pm: e.matmul(pm[:32, :n], lhsT=self.cstb[:32, 0, 0:32], rhs=sq[j][:32, :n], start=True, stop=True),
                     reads=[sqb[j], self.cstb_buf], writes=[pmb])
                r = rsq(pm, pmb, n, 32, 1.0 / 32)
                a = self.nxt("qn", 3)
                S.op("dve", lambda e, a=a, r=r: e.scalar_tensor_tensor(
                    out=qn[a][:32, :n], in0=cf[:32, 5, :n], scalar=mg[:32, 7:8], in1=rs2[r][:32, :n], op0=ALU.mult, op1=ALU.mult),
                    reads=[cfb[5], rs2b[r], mgb], writes=[qnb[a]])
                pri = 4 + self.nxt("p1r", 2)
                pr, prb = self.ps[pri], self.psb[pri]
                S.op("pe", lambda e, a=a, pr=pr: e.matmul(pr[:32, :n], lhsT=self.cstb[:32, 7, 0:32], rhs=qn[a][:32, :n], start=True, stop=True),
                     reads=[qnb[a], self.cstb_buf], writes=[prb])
                u = self.nxt("t1", 2)
                self._mla_krope(L, S, a, u, pr, prb, qn, qnb, t1, t1b, t2, t2b, qo, qob, n, t0, i)
                p1q_banks = [0, 1, 6]
                hst = {}

                def qP(h):
                    pb = p1q_banks[self.nxt("p1q3", 3)]
                    pq, pqb = self.ps[pb], self.psb[pb]
                    for kc in range(3):
                        S.op("pe", lambda e, kc=kc: e.matmul(
                            pq[:96, :n], lhsT=Wuq[:, kc, h * 96:(h + 1) * 96], rhs=cn[:, kc, :n], start=(kc == 0), stop=(kc == 2)),
                            reads=[Wuqb, cnb[kc]], writes=[pqb])
                    hst[h] = dict(pq=pq, pqb=pqb)

                def qN(h):
                    pq, pqb = hst[h]["pq"], hst[h]["pqb"]
                    j = self.nxt("sq", 2)
                    S.op("act", lambda e: e.activation(out=sq[j][:96, :n], in_=pq[:96, :n], func=AF.Square),
                         reads=[pqb], writes=[sqb[j]])
                    pmi = 2 + self.nxt("p1m", 2)
                    pm, pmb = self.ps[pmi], self.psb[pmi]
                    S.op("pe", lambda e: e.matmul(pm[:96, :n], lhsT=blkm[:96, 0:96], rhs=sq[j][:96, :n], start=True, stop=True),
                         reads=[sqb[j], self.cstb_buf], writes=[pmb])
                    r = rsq(pm, pmb, n, 96, 1.0)
                    a = self.nxt("qn", 3)
                    S.op("dve", lambda e: e.scalar_tensor_tensor(
                        out=qn[a][:96, :n], in0=pq[:96, :n], scalar=mg[:96, 5:6], in1=rs2[r][:96, :n], op0=ALU.mult, op1=ALU.mult),
                        reads=[pqb, rs2b[r], mgb], writes=[qnb[a]])
                    hst[h]["a"] = a

                def qR(h):
                    a = hst[h]["a"]
                    pri = 4 + self.nxt("p1r", 2)
                    pr, prb = self.ps[pri], self.psb[pri]
                    S.op("pe", lambda e: e.matmul(pr[:96, :n], lhsT=Rmm[:96, 0:96], rhs=qn[a][:96, :n], start=True, stop=True),
                         reads=[qnb[a], self.cstb_buf], writes=[prb])
                    u = self.nxt("t1", 2)
                    S.op("dve", lambda e: e.tensor_tensor(out=t1[u][:96, :n], in0=qn[a][:96, :n], in1=cs[i][:96, 0, :n], op=ALU.mult),
                         reads=[qnb[a], csb[i]], writes=[t1b[u]])
                    S.op("dve", lambda e: e.tensor_tensor(out=t2[u][:96, :n], in0=pr[:96, :n], in1=cs[i][:96, 1, :n], op=ALU.mult),
                         reads=[prb, csb[i]], writes=[t2b[u]])
                    o = self.nxt("qo", 3)
                    S.op("pool", lambda e: e.tensor_tensor(out=qo[o][:96, :n], in0=t1[u][:96, :n], in1=t2[u][:96, :n], op=ALU.add),
                         reads=[t1b[u], t2b[u]], writes=[qob[o]])
                    S.dma("sp", self.q_s[h, 0:96, t0:t0 + n], qo[o][:96, :n], reads=[qob[o]])

                for it_ in range(16 + 2):
                    if it_ < 16:
                        qP(it_)
                    if 0 <= it_ - 1 < 16:
                        qN(it_ - 1)
                    if 0 <= it_ - 2 < 16:
                        qR(it_ - 2)

                kst = {}

                def kP(c):
                    pb = p1q_banks[self.nxt("p1q3", 3)]
                    pq, pqb = self.ps[pb], self.psb[pb]
                    for kc in range(2):
                        S.op("pe", lambda e, kc=kc: e.matmul(
                            pq[:, :n], lhsT=Wuk[:, kc, 2 * c:2 * c + 2, :].rearrange("p h d -> p (h d)"), rhs=cn[:, 3 + kc, :n],
                            start=(kc == 0), stop=(kc == 1)),
                            reads=[Wukb, cnb[3 + kc]], writes=[pqb])
                    kst[c] = (pq, pqb)

                def kN(c):
                    pq, pqb = kst[c]
                    j = self.nxt("sq", 2)
                    S.op("act", lambda e: e.activation(out=sq[j][:, :n], in_=pq[:, :n], func=AF.Square),
                         reads=[pqb], writes=[sqb[j]])
                    pmi = 2 + self.nxt("p1m", 2)
                    pm, pmb = self.ps[pmi], self.psb[pmi]
                    S.op("pe", lambda e: e.matmul(pm[:, :n], lhsT=blk64, rhs=sq[j][:, :n], start=True, stop=True),
                         reads=[sqb[j], self.cstb_buf], writes=[pmb])
                    kst[c] = (pq, pqb, pm, pmb)

                def kR(c):
                    pq, pqb, pm, pmb = kst[c]
                    r = rsq(pm, pmb, n, 128, 1.0)
                    o = self.nxt("qo", 3)
                    S.op("dve", lambda e: e.scalar_tensor_tensor(
                        out=qo[o][:, :n], in0=pq[:, :n], scalar=mg[:, 6:7], in1=rs2[r][:, :n], op0=ALU.mult, op1=ALU.mult),
                        reads=[pqb, rs2b[r], mgb], writes=[qob[o]])
                    for h2 in range(2):
                        S.dma("sp", self.k_s[2 * c + h2, 0:64, t0:t0 + n], qo[o][h2 * 64:(h2 + 1) * 64, :n], reads=[qob[o]])

                for it_ in range(8 + 2):
                    if it_ < 8:
                        kP(it_)
                    if 0 <= it_ - 1 < 8:
                        kN(it_ - 1)
                    if 0 <= it_ - 2 < 8:
                        kR(it_ - 2)
                for tt in range(n // 128):
                    kt = (t0 // 128) + tt
                    for cbk in range(2):
                        pvi = 4 + self.nxt("p1r", 2)
                        pv, pvb = self.ps[pvi], self.psb[pvi]
                        for kc in range(2):
                            S.op("pe", lambda e, kc=kc, pv=pv, tt=tt, cbk=cbk: e.matmul(
                                pv[:, :512], lhsT=cn[:, 3 + kc, tt * 128:(tt + 1) * 128],
                                rhs=Wuv[:, kc, cbk * 8:(cbk + 1) * 8, :].rearrange("p h d -> p (h d)"), start=(kc == 0), stop=(kc == 1)),
                                reads=[Wuvb, cnb[3 + kc]], writes=[pvb])
                        vi = self.nxt("vt", 3)
                        S.op("act", lambda e, vi=vi, pv=pv: e.activation(out=vt[vi][:, :512], in_=pv[:, :512], func=AF.Copy),
                             reads=[pvb], writes=[vtb[vi]])
                        dst = self.v_s[cbk * 8:(cbk + 1) * 8, :, kt, :].rearrange("h p d -> p h d")
                        S.dma("sp", dst, vt[vi][:, :512].rearrange("p (h d) -> p h d", d=64), reads=[vtb[vi]])

    def _mla_krope(self, L, S, a, u, pr, prb, qn, qnb, t1, t1b, t2, t2b, qo, qob, n, t0, i):
        kt_ = self.krt[i]
        kb_ = self.krtb[i]
        S.op("pool", lambda e: e.tensor_tensor(out=t1[u][:32, :n], in0=qn[a][:32, :n], in1=kt_[:32, 0, :n], op=ALU.mult),
             reads=[qnb[a], kb_], writes=[t1b[u]])
        S.op("dve", lambda e: e.tensor_tensor(out=t2[u][:32, :n], in0=pr[:32, :n], in1=kt_[:32, 1, :n], op=ALU.mult),
             reads=[prb, kb_], writes=[t2b[u]])
        o = self.nxt("qo", 3)
        S.op("pool", lambda e: e.tensor_tensor(out=qo[o][:32, :n], in0=t1[u][:32, :n], in1=t2[u][:32, :n], op=ALU.add),
             reads=[t1b[u], t2b[u]], writes=[qob[o]])
        S.dma("sp", self.kr_s[:, t0:t0 + n], qo[o][:32, :n], reads=[qob[o]])

    def phase2(self, L, need_ctx):
        nc, S = self.nc, self.S
        kind = L % 4
        dq = 96 if kind == 2 else 64
        dv = 128 if kind == 3 else 64
        scale = dq ** -0.5
        ones_b = self.cstb[:, 0, :]
        with ExitStack() as es:
            nkv = 2 if kind == 3 else 1
            kT = [[self.sb(es, f"kT{i}_{j}", [128, T], BF16) for j in range(nkv)] for i in range(2)]
            kTb = [[Buf(t) for t in row] for row in kT]
            V = [self.sb(es, f"V{i}", [128, NKT, 128], BF16) for i in range(2)]
            Vb = [Buf(t) for t in V]
            qt = [self.sb(es, f"qt{i}", [128, 512], BF16) for i in range(3)]; qtb = [Buf(t) for t in qt]
            for row in kT:
                for t_ in row:
                    pass
            for i_, row in enumerate(kT):
                for j_, t_ in enumerate(row):
                    S.op("pool", lambda e, t_=t_: e.memset(t_[dq:128, :], 0.0), writes=[kTb[i_][j_]])
            for i_, t_ in enumerate(qt):
                S.op("pool", lambda e, t_=t_: e.memset(t_[dq:128, :], 0.0), writes=[qtb[i_]])
            LOOK = 4 if dv == 64 else 3
            sbanks = [0, 1, 2, 5, 6] if dv == 64 else [0, 1, 2, 7]
            NPT = LOOK + 2
            pt = [self.sb(es, f"pt{i}", [128, 512], BF16) for i in range(NPT)]; ptb = [Buf(t) for t in pt]
            rd = [self.sb(es, f"rd{i}", [128, 512], F32) for i in range(2)]; rdb = [Buf(t) for t in rd]
            ot = [self.sb(es, f"ot{i}", [128, 512], BF16) for i in range(2)]; otb = [Buf(t) for t in ot]
            if dv != 64:
                pp = [self.sb(es, f"pp{i}", [128, 512], BF16) for i in range(4)]; ppb = [Buf(t) for t in pp]
                pq4 = [self.sb(es, f"pq4{i}", [128, 512], BF16) for i in range(3)]; pq4b = [Buf(t) for t in pq4]
            if kind == 0:
                bt = [self.sb(es, f"bt{i}", [128, 6, 512], F32) for i in range(2)]; btb = [Buf(t) for t in bt]
                tb = [self.sb(es, f"tb{i}", [128, 512], F32) for i in range(3)]; tbb = [Buf(t) for t in tb]
            if kind == 1:
                mk = self.sb(es, "mk", [128, 6, 512], BF16); mkb = Buf(mk)
                S.dma("pool", mk[:, :, :], self.sw_mask, writes=[mkb])
                snk = self.sb(es, "snk", [128, 16], F32); snkb = Buf(snk)
                S.dma("sp", snk[:, :], self.sw_sink, writes=[snkb])
                S.op("act", lambda e: e.activation(out=snk[:, :], in_=snk[:, :], func=AF.Exp), reads=[snkb], writes=[snkb])
                p0 = [self.sb(es, f"p0{i}", [128, 512], BF16) for i in range(3)]; p0b = [Buf(t) for t in p0]
            if kind == 3:
                lam = self.sb(es, "lam", [128, 4, 64], F32); lamb = Buf(lam)
                S.dma("sp", lam[:, :, :], self.diff_lam, writes=[lamb])
                lt = self.sb(es, "lt", [128, 2, 64], F32); ltb = Buf(lt)
                ls = self.sb(es, "ls", [128, 4], F32); lsb = Buf(ls)
                S.op("dve", lambda e: e.tensor_tensor(out=lt[:, 0, :], in0=lam[:, 0, :], in1=lam[:, 1, :], op=ALU.mult), reads=[lamb], writes=[ltb])
                S.op("dve", lambda e: e.tensor_tensor(out=lt[:, 1, :], in0=lam[:, 2, :], in1=lam[:, 3, :], op=ALU.mult), reads=[lamb], writes=[ltb])
                S.op("dve", lambda e: e.tensor_reduce(out=ls[:, 0:2], in_=lt[:, :, :], axis=mybir.AxisListType.X, op=ALU.add), reads=[ltb], writes=[lsb])
                S.op("act", lambda e: e.activation(out=ls[:, 0:2], in_=ls[:, 0:2], func=AF.Exp), reads=[lsb], writes=[lsb])
                lam_init = 0.8 - 0.6 * math.exp(-0.3 * L)
                S.op("dve", lambda e: e.tensor_tensor(out=ls[:, 2:3], in0=ls[:, 1:2], in1=ls[:, 0:1], op=ALU.subtract), reads=[lsb], writes=[lsb])
                S.op("dve", lambda e: e.tensor_scalar(out=ls[:, 2:3], in0=ls[:, 2:3], scalar1=-lam_init, scalar2=None, op0=ALU.add), reads=[lsb], writes=[lsb])
                sg = self.sb(es, "sg", [128, 1], F32); sgb = Buf(sg)
                S.dma("sp", sg[:, :], self.diff_subln, writes=[sgb])
                S.op("dve", lambda e: e.tensor_scalar(out=sg[:, :], in0=sg[:, :], scalar1=1.0 - lam_init, scalar2=None, op0=ALU.mult), reads=[sgb], writes=[sgb])
                o1 = [self.sb(es, f"o1{i}", [128, 512], F32) for i in range(2)]; o1b = [Buf(t) for t in o1]
                od = [self.sb(es, f"od{i}", [128, 512], F32) for i in range(2)]; odb = [Buf(t) for t in od]
                sqd = [self.sb(es, f"sqd{i}", [128, 512], BF16) for i in range(2)]; sqdb = [Buf(t) for t in sqd]

            if kind in (0, 2):
                groups = [([h], h, [(h, 0)]) for h in range(16)]
            elif kind == 1:
                groups = [([g], g, [(4 * g + i, 0) for i in range(4)]) for g in range(4)]
            else:
                groups = [([2 * g, 2 * g + 1], g, [(2 * g, 0), (2 * g + 1, 1)]) for g in range(8)]

            qblocks = []
            for qi in range(8):
                tiles = []
                pat = 0
                if kind == 0:
                    r0 = 8 * qi
                    kr0 = min(max(r0 - 4, 0), 52)
                    pat = 0 if qi == 0 else (2 if qi == 7 else 1)
                    tiles = [(kr0 // 2 + j, "bias", j) for j in range(6)]
                elif kind == 1:
                    for j in range(6):
                        kt = 4 * qi - 1 + j
                        if 0 <= kt < 32:
                            tiles.append((kt, "mask", j))
                else:
                    tiles = [(kt, "plain", 0) for kt in range(32)]
                tiles += [(32, "plain", 0), (33, "plain", 0)]
                qblocks.append((qi * 512, 512, tiles, pat))
            if need_ctx:
                qblocks.append((SEQ, NCTX, [(32, "plain", 0), (33, "plain", 0)], -1))

            aug = (dv == 64)
            if aug:
                for i in range(2):
                    S.op("pool", lambda e, i=i: e.memset(V[i][:, :, 64:128], 1.0), writes=[Vb[i]])

            def load_group(gi):
                ks, vh, _ = groups[gi]
                i = gi % 2
                for j, kh in enumerate(ks):
                    S.dma("sp", kT[i][j][0:64, :], self.k_s[kh, 0:64, :], writes=[kTb[i][j]])
                    if kind == 2:
                        S.dma("sp", kT[i][j][64:96, :], self.kr_s[:, :], writes=[kTb[i][j]])
                if aug:
                    S.dma("sp", V[i][:, :, 0:64], self.v_s[vh], writes=[Vb[i]])
                else:
                    S.dma("sp", V[i][:, :, :], self.v_s2[vh], writes=[Vb[i]])

            load_group(0)
            for gi, (ks, vh, subs) in enumerate(groups):
                if gi + 1 < len(groups):
                    load_group(gi + 1)
                gb = gi % 2
                items = []
                for (q0, nq, tiles, pat) in qblocks:
                    for si, (qh, kvi) in enumerate(subs):
                        items.append(dict(q0=q0, nq=nq, tiles=tiles, pat=pat, si=si, qh=qh, kvi=kvi))
                work = [(ii, ti) for ii, it in enumerate(items) for ti in range(len(it["tiles"]))]
                st = dict(pat=None, bi=None, o1i=None)

                def prefetch(ii):
                    it = items[ii]
                    if kind == 0 and it["pat"] >= 0 and it["pat"] != st["pat"]:
                        st["pat"] = it["pat"]
                        st["bi"] = self.nxt("bt", 2)
                        S.dma("sp", bt[st["bi"]][:, :, :], self.na_bias[it["qh"], it["pat"]], writes=[btb[st["bi"]]])
                    it["bi"] = st["bi"]
                    it["qi"] = self.nxt("qt", 3)
                    S.dma("sp", qt[it["qi"]][0:dq, :it["nq"]], self.q_s[it["qh"], 0:dq, it["q0"]:it["q0"] + it["nq"]], writes=[qtb[it["qi"]]])

                def stageA(ii, ti):
                    it = items[ii]
                    nq = it["nq"]
                    if ti == 0:
                        if ii == 0:
                            prefetch(0)
                        if ii + 1 < len(items):
                            prefetch(ii + 1)
                        it["ni"] = 3 + self.nxt("p2n", 2)
                        it["di"] = 5 + self.nxt("p2d", 2)
                        it["pis"] = {}
                        it["pps"] = {}
                    kt, mode, ref = it["tiles"][ti]
                    qi_, bi_, kvi = it["qi"], it["bi"], it["kvi"]
                    si_ = sbanks[self.nxt("p2s", len(sbanks))]
                    sp_, spb = self.ps[si_], self.psb[si_]
                    S.op("pe", lambda e: e.matmul(
                        sp_[:, :nq], lhsT=kT[gb][kvi][:, kt * 128:(kt + 1) * 128], rhs=qt[qi_][:, :nq], start=True, stop=True),
                        reads=[kTb[gb][kvi], qtb[qi_]], writes=[spb])
                    pi = self.nxt("pt", NPT)
                    it["pis"][ti] = pi
                    if mode == "plain":
                        S.op("act", lambda e: e.activation(out=pt[pi][:, :nq], in_=sp_[:, :nq], func=AF.Exp, scale=scale),
                             reads=[spb], writes=[ptb[pi]])
                    elif mode == "mask":
                        zi = self.nxt("p0", 3)
                        S.op("act", lambda e: e.activation(out=p0[zi][:, :nq], in_=sp_[:, :nq], func=AF.Exp, scale=scale),
                             reads=[spb], writes=[p0b[zi]])
                        S.op("dve", lambda e: e.tensor_tensor(out=pt[pi][:, :nq], in0=p0[zi][:, :nq], in1=mk[:, ref, :nq], op=ALU.mult),
                             reads=[p0b[zi], mkb], writes=[ptb[pi]])
                    else:
                        zi = self.nxt("tb", 3)
                        S.op("dve", lambda e: e.scalar_tensor_tensor(
                            out=tb[zi][:, :nq], in0=sp_[:, :nq], scalar=scale, in1=bt[bi_][:, ref, :nq], op0=ALU.mult, op1=ALU.add),
                            reads=[spb, btb[bi_]], writes=[tbb[zi]])
                        S.op("act", lambda e: e.activation(out=pt[pi][:, :nq], in_=tb[zi][:, :nq], func=AF.Exp),
                             reads=[tbb[zi]], writes=[ptb[pi]])

                def pairsum(ii, ti):
                    it = items[ii]
                    nq = it["nq"]
                    if aug or ti % 2 == 0:
                        return
                    pa, pb_ = it["pis"][ti - 1], it["pis"][ti]
                    pj = self.nxt("pp", 4)
                    it["pps"][ti] = pj
                    S.op("dve", lambda e: e.tensor_tensor(out=pp[pj][:, :nq], in0=pt[pa][:, :nq], in1=pt[pb_][:, :nq], op=ALU.add),
                         reads=[ptb[pa], ptb[pb_]], writes=[ppb[pj]])

                def stageB(ii, ti):
                    it = items[ii]
                    nq, qh, si = it["nq"], it["qh"], it["si"]
                    q0 = it["q0"]
                    kt, mode, ref = it["tiles"][ti]
                    pi = it["pis"][ti]
                    ni, di = it["ni"], it["di"]
                    num, numb, den, denb = self.ps[ni], self.psb[ni], self.ps[di], self.psb[di]
                    st_, sp2 = (ti == 0), (ti == len(it["tiles"]) - 1)
                    S.op("pe", lambda e: e.matmul(num[:, :nq], lhsT=V[gb][:, kt, :], rhs=pt[pi][:, :nq], start=st_, stop=sp2),
                         reads=[Vb[gb], ptb[pi]], writes=[numb])
                    if (not aug) and ti % 2 == 1:
                        pj = it["pps"][ti]
                        S.op("pe", lambda e: e.matmul(den[:, :nq], lhsT=ones_b, rhs=pp[pj][:, :nq], start=(ti == 1), stop=sp2),
                             reads=[self.cstb_buf, ppb[pj]], writes=[denb])
                    if not sp2:
                        return None
                    return lambda: finish(it, num, numb, den, denb)

                def finish(it, num, numb, den, denb):
                    nq, qh, si, q0 = it["nq"], it["qh"], it["si"], it["q0"]
                    ri = self.nxt("rd", 2)
                    if aug:
                        if kind == 1:
                            S.op("act", lambda e: e.activation(out=rd[ri][0:64, :nq], in_=num[64:128, :nq], func=AF.Ln, bias=snk[64:128, qh:qh + 1], scale=1.0),
                                 reads=[numb, snkb], writes=[rdb[ri]])
                            S.op("act", lambda e: e.activation(out=rd[ri][0:64, :nq], in_=rd[ri][0:64, :nq], func=AF.Exp, scale=-1.0), reads=[rdb[ri]], writes=[rdb[ri]])
                        elif kind == 0:
                            S.op("act", lambda e: e.activation(out=rd[ri][0:64, :nq], in_=num[64:128, :nq], func=AF.Ln), reads=[numb], writes=[rdb[ri]])
                            S.op("act", lambda e: e.activation(out=rd[ri][0:64, :nq], in_=rd[ri][0:64, :nq], func=AF.Exp, scale=-1.0), reads=[rdb[ri]], writes=[rdb[ri]])
                        else:
                            S.op("dve", lambda e: e.reciprocal(out=rd[ri][0:64, :nq], in_=num[64:128, :nq]), reads=[numb], writes=[rdb[ri]])
                        oi = self.nxt("ot", 2)
                        S.op("dve", lambda e: e.tensor_tensor(out=ot[oi][0:64, :nq], in0=num[0:64, :nq], in1=rd[ri][0:64, :nq], op=ALU.mult),
                             reads=[numb, rdb[ri]], writes=[otb[oi]])
                        S.dma("sp", self.at_s[qh * 64:(qh + 1) * 64, q0:q0 + nq], ot[oi][0:64, :nq], reads=[otb[oi]])
                        return
                    S.op("act", lambda e: e.activation(out=rd[ri][:, :nq], in_=den[:, :nq], func=AF.Ln), reads=[denb], writes=[rdb[ri]])
                    S.op("act", lambda e: e.activation(out=rd[ri][:, :nq], in_=rd[ri][:, :nq], func=AF.Exp, scale=-1.0), reads=[rdb[ri]], writes=[rdb[ri]])
                    if si == 0:
                        o1i = self.nxt("o1", 2)
                        st["o1i"] = o1i
                        S.op("dve", lambda e: e.tensor_tensor(out=o1[o1i][:, :nq], in0=num[:, :nq], in1=rd[ri][:, :nq], op=ALU.mult),
                             reads=[numb, rdb[ri]], writes=[o1b[o1i]])
                    else:
                        o1i = st["o1i"]
                        odi = self.nxt("od", 2)
                        S.op("dve", lambda e: e.tensor_tensor(out=od[odi][:, :nq], in0=num[:, :nq], in1=rd[ri][:, :nq], op=ALU.mult),
                             reads=[numb, rdb[ri]], writes=[odb[odi]])
                        S.op("dve", lambda e: e.scalar_tensor_tensor(
                            out=od[odi][:, :nq], in0=od[odi][:, :nq], scalar=ls[:, 2:3], in1=o1[o1i][:, :nq], op0=ALU.mult, op1=ALU.add),
                            reads=[odb[odi], o1b[o1i], lsb], writes=[odb[odi]])
                        qd = self.nxt("sqd", 2)
                        S.op("act", lambda e: e.activation(out=sqd[qd][:, :nq], in_=od[odi][:, :nq], func=AF.Square),
                             reads=[odb[odi]], writes=[sqdb[qd]])
                        pm, pmb = self.ps[7], self.psb[7]
                        S.op("pe", lambda e: e.matmul(pm[:, :nq], lhsT=ones_b, rhs=sqd[qd][:, :nq], start=True, stop=True),
                             reads=[sqdb[qd], self.cstb_buf], writes=[pmb])
                        r2 = self.nxt("rd", 2)
                        S.op("act", lambda e: e.activation(out=rd[r2][:, :nq], in_=pm[:, :nq], func=AF.Ln, bias=self.eps_col[:, 0:1], scale=1.0 / 128),
                             reads=[pmb, self.eps_buf], writes=[rdb[r2]])
                        S.op("act", lambda e: e.activation(out=rd[r2][:, :nq], in_=rd[r2][:, :nq], func=AF.Exp, scale=-0.5), reads=[rdb[r2]], writes=[rdb[r2]])
                        oi = self.nxt("ot", 2)
                        S.op("dve", lambda e: e.scalar_tensor_tensor(
                            out=ot[oi][:, :nq], in0=od[odi][:, :nq], scalar=sg[:, 0:1], in1=rd[r2][:, :nq], op0=ALU.mult, op1=ALU.mult),
                            reads=[odb[odi], rdb[r2], sgb], writes=[otb[oi]])
                        S.dma("sp", self.at_s[vh * 128:(vh + 1) * 128, q0:q0 + nq], ot[oi][:, :nq], reads=[otb[oi]])

                pending = []
                for idx in range(len(work) + LOOK):
                    if idx < len(work):
                        stageA(*work[idx])
                        pairsum(*work[idx])
                    while pending and pending[0][0] <= idx:
                        pending.pop(0)[1]()
                    if idx - LOOK >= 0:
                        fin = stageB(*work[idx - LOOK])
                        if fin is not None:
                            pending.append((idx + 3, fin))
                for _, fin in pending:
                    fin()

    def phase3(self, L, x_src, x_dst, need_ctx, final):
        nc, S = self.nc, self.S
        kind = L % 4
        ident = self.cst_f[:, 3, :]
        with ExitStack() as es0:
            GT = self.sb(es0, "GT", [32, T], BF16); GTb = Buf(GT)
            with ExitStack() as es, nc.named_scope(f"L{L}_p3a"):
                Wo = self.sb(es, "wo", [128, 8, D], BF16); Wob = [Buf(Wo) for _ in range(8)]
                wsrc = self.wo[kind].rearrange("(kc p) n -> p kc n", p=128)
                for kc in range(8):
                    S.dma("pool", Wo[:, kc, :], wsrc[:, kc, :], writes=[Wob[kc]])
                Wr = self.sb(es, "wr", [128, 8, 36], F32); Wrb = Buf(Wr)
                S.dma("sp", Wr[:, :, :], self.wr[L].rearrange("(kc p) n -> p kc n", p=128), writes=[Wrb])
                brt = self.sb(es, "brt", [128, 36], F32); brb = Buf(brt)
                S.dma("sp", brt[:, :], self.br[L], writes=[brb])
                xin = [self.sb(es, f"xin{i}", [128, 8, 512], F32) for i in range(2)]; xinb = [Buf(t) for t in xin]
                at = [self.sb(es, f"at{i}", [128, 8, 512], BF16) for i in range(2)]; atb = [Buf(t) for t in at]
                h2f = self.sb(es, "h2f", [128, 8, 512], F32); h2fb = Buf(h2f)
                h2b = [self.sb(es, f"h2b{i}", [128, 8, 512], BF16) for i in range(2)]; h2bb = [Buf(t) for t in h2b]
                rs = self.sb(es, "rs", [128, 512], F32); rsb = Buf(rs)
                sq = [self.sb(es, f"sq{i}", [128, 512], BF16) for i in range(2)]; sqb = [Buf(t) for t in sq]
                tmp = [self.sb(es, f"tmp{i}", [128, 512], F32) for i in range(2)]; tmpb = [Buf(t) for t in tmp]
                rt = self.sb(es, "rt", [128, 4, 256], F32); rtb = Buf(rt)
                blocks = _token_blocks(need_ctx)
                xsrc = x_src.rearrange("(kc p) t -> p kc t", p=128)
                xdst = x_dst.rearrange("(kc p) t -> p kc t", p=128)
                asrc = self.at_s.rearrange("(kc p) t -> p kc t", p=128)
                hdst = self.h2_s.rearrange("(kc p) t -> p kc t", p=128)

                def load(bi):
                    t0, n = blocks[bi]
                    i = bi % 2
                    S.dma("sp", xin[i][:, :, :n], xsrc[:, :, t0:t0 + n], writes=[xinb[i]])
                    S.dma("sp", at[i][:, :, :n], asrc[:, :, t0:t0 + n], writes=[atb[i]])

                load(0)
                for bi, (t0, n) in enumerate(blocks):
                    if bi + 1 < len(blocks):
                        load(bi + 1)
                    i = bi % 2
                    tq = 0 if t0 < SEQ else 1
                    for oc in range(8):
                        pb = self.nxt("p3y", 2)
                        py, pyb = self.ps[pb], self.psb[pb]
                        for kc in range(8):
                            S.op("pe", lambda e, kc=kc, oc=oc, py=py: e.matmul(
                                py[:, :n], lhsT=Wo[:, kc, oc * 128:(oc + 1) * 128], rhs=at[i][:, kc, :n], start=(kc == 0), stop=(kc == 7)),
                                reads=[Wob[kc], atb[i]], writes=[pyb])
                        S.op("dve", lambda e, oc=oc, py=py: e.scalar_tensor_tensor(
                            out=xin[i][:, oc, :n], in0=py[:, :n], scalar=self.modcol(L, 2, oc, tq), in1=xin[i][:, oc, :n], op0=ALU.mult, op1=ALU.add),
                            reads=[pyb, xinb[i], self.mod_buf], writes=[xinb[i]])
                    S.dma("sp", xdst[:, :, t0:t0 + n], xin[i][:, :, :n], reads=[xinb[i]])
                    hb = h2b[i]
                    self.norm_mod(L, 1, xin[i], xinb[i], n, tq, rs, rsb, sq, sqb, tmp, tmpb,
                                  [(h2f, h2fb, "act"), (hb, h2bb[i], "pool")], 7)
                    S.dma("sp", hdst[:, :, t0:t0 + n], hb[:, :, :n], reads=[h2bb[i]])
                    nt = n // 128
                    pr, prb = self.ps[2], self.psb[2]
                    for tt in range(nt):
                        for kc in range(8):
                            S.op("pe", lambda e, kc=kc, tt=tt: e.matmul(
                                pr[:, tt * 36:(tt + 1) * 36], lhsT=h2f[:, kc, tt * 128:(tt + 1) * 128], rhs=Wr[:, kc, :], start=(kc == 0), stop=(kc == 7)),
                                reads=[h2fb, Wrb], writes=[prb])
                    self.topk(pr, prb, brt, brb, rt, rtb, nt)
                    pt_, ptb_ = self.ps[3], self.psb[3]
                    for tt in range(nt):
                        S.op("pe", lambda e, tt=tt: e.transpose(pt_[0:32, tt * 128:(tt + 1) * 128], rt[:, tt, 64:96], ident),
                             reads=[rtb, self.cst_buf], writes=[ptb_])
                    S.op("act", lambda e: e.activation(out=GT[:, t0:t0 + n], in_=pt_[0:32, 0:n], func=AF.Copy),
                         reads=[ptb_], writes=[GTb])
            S.barrier()
            with ExitStack() as es, nc.named_scope(f"L{L}_p3b"):
                sel = self.sb(es, "sel", [32, NE, 128], BF16); selb = Buf(sel)
                S.dma("pool", sel[:, :, :], self.sel_in, writes=[selb])
                SBW = 2048 + (NCTX if need_ctx else 0)
                h2 = self.sb(es, "h2", [128, 8, SBW], BF16); h2bf = Buf(h2)
                acc = self.sb(es, "acc", [128, 8, SBW], F32); accb = [Buf(acc) for _ in range(5)]
                NW = 3
                wgu = [self.sb(es, f"wgu{i}", [128, 8, 512], BF16) for i in range(NW)]; wgub = [Buf(t) for t in wgu]
                wdn = [self.sb(es, f"wdn{i}", [128, 2, D], BF16) for i in range(NW)]; wdnb = [Buf(t) for t in wdn]
                x1 = self.sb(es, "x1", [128, 8, 512], F32); x1b = Buf(x1)
                sgt = [self.sb(es, f"sgt{i}", [128, 512], F32) for i in range(2)]; sgtb = [Buf(t) for t in sgt]
                tt_ = [self.sb(es, f"tt{i}", [128, 512], F32) for i in range(2)]; ttb = [Buf(t) for t in tt_]
                aT = [self.sb(es, f"aT{i}", [128, 512], BF16) for i in range(4)]; aTb = [Buf(t) for t in aT]
                sbs = [(0, 2048), (2048, SBW)]
                hsrc = self.h2_s.rearrange("(kc p) t -> p kc t", p=128)
                xdst = x_dst.rearrange("(kc p) t -> p kc t", p=128)
                outd = self.out.rearrange("(kc p) t -> p kc t", p=128)

                def loadw(e):
                    i = e % NW
                    S.dma("pool", wgu[i][:, :, :], self.w_gu[L][e].rearrange("(kc p) n -> p kc n", p=128), writes=[wgub[i]])
                    S.dma("pool", wdn[i][:, :, :], self.w_dn[L][e].rearrange("(hc p) n -> p hc n", p=128), writes=[wdnb[i]])

                for (s0, sn) in sbs:
                    S.dma("sp", h2[:, :, :sn], hsrc[:, :, s0:s0 + sn], writes=[h2bf])
                    nb = (sn + 511) // 512
                    units = [(e, b) for e in range(NE) for b in range(nb)]

                    def gu_group(e, b, gidx):
                        wi = e % NW
                        b0 = b * 512
                        n = min(512, sn - b0)
                        j = (0, 2, 1, 3)[gidx]
                        pgj, pgbj = self.ps[j], self.psb[j]
                        for kc in range(8):
                            S.op("pe", lambda e_: e_.matmul(
                                pgj[:, :n], lhsT=wgu[wi][:, kc, j * 128:(j + 1) * 128], rhs=h2[:, kc, b0:b0 + n],
                                start=(kc == 0), stop=(kc == 7)), reads=[wgub[wi], h2bf], writes=[pgbj])

                    def chain(e, b):
                        b0 = b * 512
                        n = min(512, sn - b0)
                        pg = [self.ps[j] for j in range(4)]
                        pgb = [self.psb[j] for j in range(4)]
                        pgate, pgateb = self.ps[4], self.psb[4]
                        S.op("pe", lambda e_: e_.matmul(pgate[:, :n], lhsT=sel[:, e, :], rhs=GT[:, s0 + b0:s0 + b0 + n], start=True, stop=True),
                             reads=[selb, GTb], writes=[pgateb])
                        ai = []
                        for hc in range(2):
                            s_ = self.nxt("sgt", 2)
                            S.op("act", lambda e_: e_.activation(out=sgt[s_][:, :n], in_=pg[hc][:, :n], func=AF.Silu),
                                 reads=[pgb[hc]], writes=[sgtb[s_]])
                            t_ = self.nxt("tt", 2)
                            S.op("dve", lambda e_: e_.tensor_tensor(out=tt_[t_][:, :n], in0=pg[2 + hc][:, :n], in1=sgt[s_][:, :n], op=ALU.mult),
                                 reads=[pgb[2 + hc], sgtb[s_]], writes=[ttb[t_]])
                            a_ = self.nxt("aT", 4)
                            S.op("dve", lambda e_: e_.tensor_tensor(out=aT[a_][:, :n], in0=pgate[:, :n], in1=tt_[t_][:, :n], op=ALU.mult),
                                 reads=[pgateb, ttb[t_]], writes=[aTb[a_]])
                            ai.append(a_)
                        return ai

                    def down_part(e, b, ai, ocs):
                        wi = e % NW
                        b0 = b * 512
                        n = min(512, sn - b0)
                        for oc in ocs:
                            pdi = 5 + self.nxt("p3d", 3)
                            pd, pdb = self.ps[pdi], self.psb[pdi]
                            for hc in range(2):
                                S.op("pe", lambda e_: e_.matmul(
                                    pd[:, :n], lhsT=wdn[wi][:, hc, oc * 128:(oc + 1) * 128], rhs=aT[ai[hc]][:, :n],
                                    start=(hc == 0), stop=(hc == 1)), reads=[wdnb[wi], aTb[ai[hc]]], writes=[pdb])
                            if e == 0:
                                S.op("dve", lambda e_: e_.tensor_copy(out=acc[:, oc, b0:b0 + n], in_=pd[:, :n]),
                                     reads=[pdb], writes=[accb[b]])
                            else:
                                S.op("dve", lambda e_: e_.tensor_tensor(out=acc[:, oc, b0:b0 + n], in0=pd[:, :n], in1=acc[:, oc, b0:b0 + n], op=ALU.add),
                                     reads=[pdb, accb[b]], writes=[accb[b]])

                    loadw(0)
                    loadw(1)
                    loadw(2)
                    prev = None
                    for (e, b) in units:
                        for gidx in range(4):
                            gu_group(e, b, gidx)
                            if prev is not None and gidx >= 1:
                                down_part(prev[0], prev[1], prev[2], (2 * gidx - 2, 2 * gidx - 1))
                        ai = chain(e, b)
                        if prev is not None:
                            down_part(prev[0], prev[1], prev[2], (6, 7))
                            if prev[0] != e and prev[0] + NW < NE:
                                loadw(prev[0] + NW)
                        prev = (e, b, ai)
                    down_part(prev[0], prev[1], prev[2], range(8))
                    for b in range(nb):
                        b0 = b * 512
                        n = min(512, sn - b0)
                        tq = 0 if s0 + b0 < SEQ else 1
                        S.dma("sp", x1[:, :, :n], xdst[:, :, s0 + b0:s0 + b0 + n], writes=[x1b])
                        for oc in range(8):
                            S.op("dve", lambda e_, oc=oc: e_.scalar_tensor_tensor(
                                out=x1[:, oc, :n], in0=acc[:, oc, b0:b0 + n], scalar=self.modcol(L, 5, oc, tq), in1=x1[:, oc, :n], op0=ALU.mult, op1=ALU.add),
                                reads=[accb[b], x1b, self.mod_buf], writes=[x1b])
                        if final:
                            if self.last:
                                if s0 + b0 < SEQ:
                                    S.dma("sp", outd[:, :, s0 + b0:s0 + b0 + n], x1[:, :, :n], reads=[x1b])
                            else:
                                S.dma("sp", outd[:, :, s0 + b0:s0 + b0 + n], x1[:, :, :n], reads=[x1b])
                        else:
                            S.dma("sp", xdst[:, :, s0 + b0:s0 + b0 + n], x1[:, :, :n], reads=[x1b])

    def topk(self, pr, prb, brt, brb, rt, rtb, nt):
        S = self.S
        X = mybir.AxisListType.X

        def op(fn, extra=()):
            S.op("dve", fn, reads=[rtb] + list(extra), writes=[rtb])

        def bc(ap, w):
            return ap.unsqueeze(2).to_broadcast([128, nt, w])

        Lg = rt[:, 0:nt, 0:36]
        S.op("dve", lambda e: e.tensor_tensor(out=Lg, in0=pr[:, 0:nt * 36].rearrange("p (t c) -> p t c", c=36),
                                              in1=brt[:, :].unsqueeze(1).to_broadcast([128, nt, 36]), op=ALU.add),
             reads=[prb, brb], writes=[rtb])
        lg, le = rt[:, 0:nt, 0:4], rt[:, 0:nt, 4:36]
        gmax, gsum, gw = rt[:, 0:nt, 40], rt[:, 0:nt, 42], rt[:, 0:nt, 43]
        m1, m2, dd, g1, g2 = rt[:, 0:nt, 52], rt[:, 0:nt, 53], rt[:, 0:nt, 54], rt[:, 0:nt, 55], rt[:, 0:nt, 56]
        gexp, pen = rt[:, 0:nt, 44:48], rt[:, 0:nt, 48:52]
        lem, mask1, mask2, lem2 = rt[:, 0:nt, 100:132], rt[:, 0:nt, 132:164], rt[:, 0:nt, 164:196], rt[:, 0:nt, 196:228]
        G = rt[:, 0:nt, 64:96]
        op(lambda e: e.tensor_reduce(out=gmax, in_=lg, axis=X, op=ALU.max))
        op(lambda e: e.tensor_tensor(out=gexp, in0=lg, in1=bc(gmax, 4), op=ALU.subtract))
        S.op("act", lambda e: e.activation(out=gexp, in_=gexp, func=AF.Exp), reads=[rtb], writes=[rtb])
        op(lambda e: e.tensor_reduce(out=gsum, in_=gexp, axis=X, op=ALU.add))
        op(lambda e: e.reciprocal(out=gw, in_=gsum))
        op(lambda e: e.tensor_tensor(out=pen, in0=lg, in1=bc(gmax, 4), op=ALU.is_equal))
        op(lambda e: e.tensor_scalar(out=pen, in0=pen, scalar1=-1.0, scalar2=30000.0, op0=ALU.add, op1=ALU.mult))
        op(lambda e: e.tensor_tensor(out=lem.rearrange("p t (g x) -> p t g x", g=4), in0=le.rearrange("p t (g x) -> p t g x", g=4),
                                     in1=pen.unsqueeze(3).to_broadcast([128, nt, 4, 8]), op=ALU.add))
        op(lambda e: e.tensor_reduce(out=m1, in_=lem, axis=X, op=ALU.max))
        op(lambda e: e.tensor_tensor(out=mask1, in0=lem, in1=bc(m1, 32), op=ALU.is_equal))
        op(lambda e: e.scalar_tensor_tensor(out=lem2, in0=mask1, scalar=-30000.0, in1=lem, op0=ALU.mult, op1=ALU.add))
        op(lambda e: e.tensor_reduce(out=m2, in_=lem2, axis=X, op=ALU.max))
        op(lambda e: e.tensor_tensor(out=mask2, in0=lem2, in1=bc(m2, 32), op=ALU.is_equal))
        op(lambda e: e.tensor_tensor(out=dd, in0=m2, in1=m1, op=ALU.subtract))
        S.op("act", lambda e: e.activation(out=dd, in_=dd, func=AF.Exp), reads=[rtb], writes=[rtb])
        op(lambda e: e.tensor_scalar(out=dd, in0=dd, scalar1=1.0, scalar2=None, op0=ALU.add))
        op(lambda e: e.reciprocal(out=dd, in_=dd))
        op(lambda e: e.tensor_tensor(out=g1, in0=dd, in1=gw, op=ALU.mult))
        op(lambda e: e.tensor_tensor(out=g2, in0=gw, in1=g1, op=ALU.subtract))
        op(lambda e: e.tensor_tensor(out=G, in0=mask1, in1=bc(g1, 32), op=ALU.mult))
        op(lambda e: e.tensor_tensor(out=mask2, in0=mask2, in1=bc(g2, 32), op=ALU.mult))
        op(lambda e: e.tensor_tensor(out=G, in0=G, in1=mask2, op=ALU.add))


def _rope_tables():
    t = np.arange(SEQ)
    rows = (t // 64).astype(np.float32)
    cols = (t % 64).astype(np.float32)

    def ang(rot_dim):
        nf = rot_dim // 4
        inv = (np.float32(10000.0) ** (-np.arange(nf, dtype=np.float32) / np.float32(nf))).astype(np.float32)
        return np.concatenate([rows[:, None] * inv, cols[:, None] * inv], axis=-1).astype(np.float32)

    a64 = ang(64)
    r64 = np.zeros((2, 128, T), np.float32)
    r64[0, :, SEQ:] = 1.0
    idx = (np.arange(128) % 64) % 32
    r64[0, :, :SEQ] = np.cos(a64).T[idx]
    r64[1, :, :SEQ] = np.sin(a64).T[idx]
    a32 = ang(32)
    rm = np.zeros((2, 96, T), np.float32)
    rm[0] = 1.0
    idx = np.arange(32) % 16
    rm[0, 64:96, :SEQ] = np.cos(a32).T[idx]
    rm[1, 64:96, :SEQ] = np.sin(a32).T[idx]
    return r64, rm


def _consts():
    c = np.zeros((128, 8, 128), np.float32)
    c[:, 0, :] = 1.0
    for b in (0, 64):
        c[b:b + 64, 1, b:b + 64] = 1.0 / 64
        for m in range(64):
            if m < 32:
                c[b + m + 32, 2, b + m] = -1.0
            else:
                c[b + m - 32, 2, b + m] = 1.0
    c[:, 3, :] = np.eye(128, dtype=np.float32)
    c[:, 4, :] = 1.0 / 128
    c[0:64, 5, 0:64] = 1.0 / 64
    c[64:96, 5, 64:96] = 1.0 / 32
    for m in range(32):
        if m < 16:
            c[64 + m + 16, 6, 64 + m] = -1.0
            c[m + 16, 7, m] = -1.0
        else:
            c[64 + m - 16, 6, 64 + m] = 1.0
            c[m - 16, 7, m] = 1.0
    return c


def _na_bias(rpb):
    out = np.full((16, 3, 128, 6, 512), NEG, np.float32)
    rl = np.arange(8)[:, None].repeat(64, 1).reshape(-1)
    c = np.arange(64)[None, :].repeat(8, 0).reshape(-1)
    krl = np.arange(2)[:, None].repeat(64, 1).reshape(-1)
    kc = np.arange(64)[None, :].repeat(2, 0).reshape(-1)
    for pi, r0 in enumerate((0, 8, 56)):
        kr0 = min(max(r0 - 4, 0), 52)
        r = r0 + rl
        row0 = np.clip(r - 4, 0, 56)
        col0 = np.clip(c - 8, 0, 48)
        for j in range(6):
            kr = kr0 + 2 * j + krl
            vr = (kr[:, None] >= row0[None, :]) & (kr[:, None] < row0[None, :] + 8)
            vc = (kc[:, None] >= col0[None, :]) & (kc[:, None] < col0[None, :] + 16)
            valid = vr & vc
            idx = (kr[:, None] - r[None, :] + 7) * 31 + (kc[:, None] - c[None, :] + 15)
            idx = np.where(valid, idx, 0)
            g = rpb[:, idx]
            out[:, pi, :, j, :] = np.where(valid[None], g, np.float32(NEG))
    return out


def _sw_mask():
    kl = np.arange(128)[:, None, None]
    j = np.arange(6)[None, :, None]
    ql = np.arange(512)[None, None, :]
    return (np.abs(ql - (kl + 128 * (j - 1))) <= 128).astype(np.float32)


def _col(v):
    return np.ascontiguousarray(v.reshape(-1, 128).T)


def _prep_shared(inp, layers):
    f = lambda a: np.ascontiguousarray(np.asarray(a, dtype=np.float32))
    kinds = set(l % 4 for l in layers)
    d = {}
    for l in layers:
        d[f"ada_w{l}"] = f(inp["ada_w"])[l]
        d[f"w_gu{l}"] = f(inp["moe_w_gate_up"])[l]
        d[f"w_dn{l}"] = f(inp["moe_w_down"])[l]
    ab = np.stack([_col(f(inp["ada_b"])[l]) for l in range(DEPTH)])
    d["ada_b"] = np.ascontiguousarray(np.repeat(ab[..., None], 2, axis=-1))
    ng = np.stack([np.stack([_col(f(inp["norm_g"])[l, i]) for i in range(2)]) for l in range(DEPTH)])
    d["norm_g"] = np.ascontiguousarray(np.repeat(ng[..., None], 2, axis=-1))
    d["wr"] = np.ascontiguousarray(np.concatenate([f(inp["moe_w_router_group"]), f(inp["moe_w_router_expert"])], axis=-1))
    br = np.concatenate([f(inp["moe_b_router_group"]), f(inp["moe_b_router_expert"])], axis=-1)
    d["br"] = np.ascontiguousarray(np.broadcast_to(br[:, None, :], (DEPTH, 128, 36)))
    d["consts"] = _consts()
    sel = np.zeros((32, NE, 128), np.float32)
    for e in range(NE):
        sel[e, e, :] = 1.0
    d["sel"] = sel
    r64, rm = _rope_tables()
    if 0 in kinds:
        d["na_w_qkv"] = f(inp["na_w_qkv"])[0]
        d["na_w_o"] = f(inp["na_w_o"])[0]
        d["na_qk_g"] = np.ascontiguousarray(np.stack([np.tile(f(inp["na_q_norm"])[0], 2), np.tile(f(inp["na_k_norm"])[0], 2)], axis=1))
        d["na_bias"] = _na_bias(f(inp["na_rpb"])[0])
    if 1 in kinds:
        d["sw_w_qkv"] = f(inp["sw_w_qkv"])[0]
        d["sw_w_o"] = f(inp["sw_w_o"])[0]
        d["sw_qk_g"] = np.ascontiguousarray(np.stack([np.tile(f(inp["sw_q_norm"])[0], 2), np.tile(f(inp["sw_k_norm"])[0], 2)], axis=1))
        d["sw_sink"] = np.ascontiguousarray(np.broadcast_to(f(inp["sw_sink"])[0][None, :], (128, 16)))
        d["sw_mask"] = _sw_mask()
    if 1 in kinds or 3 in kinds:
        d["rope64"] = r64
    if 2 in kinds:
        d["mla_w_dqkv"] = f(inp["mla_w_dqkv"])[0]
        d["mla_w_uq"] = f(inp["mla_w_uq"])[0]
        d["mla_w_ukv"] = f(inp["mla_w_ukv"])[0]
        d["mla_w_o"] = f(inp["mla_w_o"])[0]
        mg = np.zeros((128, 8), np.float32)
        mg[:, 0:3] = _col(f(inp["mla_q_a_norm"])[0])
        mg[:, 3:5] = _col(f(inp["mla_kv_a_norm"])[0])
        mg[:96, 5] = f(inp["mla_q_norm"])[0]
        mg[:, 6] = np.tile(f(inp["mla_k_norm"])[0][:64], 2)
        mg[:32, 7] = f(inp["mla_k_norm"])[0][64:96]
        d["mla_g"] = mg
        d["rope_mla"] = rm
    if 3 in kinds:
        d["diff_w_qkv"] = f(inp["diff_w_qkv"])[0]
        d["diff_w_o"] = f(inp["diff_w_o"])[0]
        d["diff_qk_g"] = np.ascontiguousarray(np.stack([np.tile(f(inp["diff_q_norm"])[0], 2), np.tile(f(inp["diff_k_norm"])[0], 2)], axis=1))
        d["diff_lam"] = np.ascontiguousarray(np.broadcast_to(f(inp["diff_lambda"])[0][None], (128, 4, 64)))
        d["diff_subln"] = np.ascontiguousarray(f(inp["diff_subln"])[0][:, None])
    return d


def _cc(inp, b):
    c = np.asarray(inp["c"], np.float32)[b]
    cx = np.asarray(inp["c_ctx"], np.float32)
    return np.ascontiguousarray(np.stack([_col(c), _col(cx)], axis=-1))


_CACHE = {}


def _get_builder(layers, last):
    key = (tuple(layers), last)
    if key not in _CACHE:
        _CACHE[key] = Builder(list(layers), layers[0] == 0, last)
    return _CACHE[key]


def run_layers(inp, layers, xT_list, cores):
    last = layers[-1] == DEPTH - 1
    B = _get_builder(layers, last)
    shared = _prep_shared(inp, layers)
    in_maps = []
    for ci, b in enumerate(cores):
        m = {"xT_in": np.ascontiguousarray(xT_list[ci]), "cc": _cc(inp, b)}
        for k in B.in_names:
            if k not in m:
                m[k] = shared[k]
        in_maps.append(m)
    res = run_bass_kernel_spmd(B.nc, in_maps, core_ids=list(range(len(cores))))
    name = "outT" if last else "xT_out"
    return [np.asarray(r[name]) for r in res.results]


FUSED = True


def kernel(**inp):
    x = np.asarray(inp["x"], np.float32)
    ctx = np.asarray(inp["ctx"], np.float32)
    nb = x.shape[0]
    xT = [np.ascontiguousarray(np.concatenate([x[b].T, ctx[b].T], axis=1)) for b in range(nb)]
    cores = list(range(nb))
    if FUSED:
        outs = run_layers(inp, [0, 1, 2, 3], xT, cores)
    else:
        cur = xT
        for L in range(DEPTH):
            cur = run_layers(inp, [L], cur, cores)
        outs = cur
    return np.ascontiguousarray(np.stack([o.T for o in outs], axis=0)).astype(np.float32)
```

```python
import math
from contextlib import ExitStack
import numpy as np
import concourse.bass as bass
import concourse.mybir as mybir
from concourse.bass_utils import run_bass_kernel_spmd

F32 = mybir.dt.float32
BF16 = mybir.dt.bfloat16
AF = mybir.ActivationFunctionType
ALU = mybir.AluOpType

D = 1024
SEQ = 4096
NCTX = 256
T = SEQ + NCTX
NKT = T // 128
DEPTH = 4
EPS = 1e-6
NEG = -30000.0
NE = 32


class Buf:
    __slots__ = ("ap", "w", "r")

    def __init__(self, ap):
        self.ap = ap
        self.w = {}
        self.r = {}


class Sched:
    NR = 8
    EPOCH = 1000000

    def __init__(self, nc):
        self.nc = nc
        self.E = {"pe": nc.tensor, "act": nc.scalar, "dve": nc.vector, "pool": nc.gpsimd, "sp": nc.sync}
        self.sems = {}
        self.ckey = {}
        self.ccnt = {}
        self.cep = {}
        for e in ("pe", "act", "dve", "pool"):
            self.cep[e] = 0
            self._new_epoch(e)
        self.seen = {e: {} for e in self.E}
        self.dkeys = {}
        self.dcnt = {}
        for q in ("sp", "pool", "act"):
            ks = []
            for i in range(self.NR):
                k = f"d_{q}_{i}"
                self.sems[k] = nc.alloc_semaphore(k)
                ks.append(k)
            self.dkeys[q] = ks
            self.dcnt[q] = 0
        self.dlast = {}
        self.n_inst = 0

    def _new_epoch(self, e):
        k = f"c_{e}_{self.cep[e]}"
        self.cep[e] += 1
        self.sems[k] = self.nc.alloc_semaphore(k)
        self.ckey[e] = k
        self.ccnt[e] = 0

    def _wait(self, e, k, v):
        if self.seen[e].get(k, 0) >= v:
            return
        self.E[e].wait_ge(self.sems[k], v)
        self.seen[e][k] = v

    def _collect(self, e, reads, writes, is_dma):
        own = f"c_{e}_"
        need = {}
        for b in reads:
            for k, v in b.w.items():
                if (not is_dma) and e == "pe" and k.startswith(own):
                    continue
                if need.get(k, 0) < v:
                    need[k] = v
        for b in writes:
            for src in (b.w, b.r):
                for k, v in src.items():
                    if (not is_dma) and k.startswith(own):
                        continue
                    if need.get(k, 0) < v:
                        need[k] = v
        for k, v in need.items():
            self._wait(e, k, v)

    def _commit(self, k, v, reads, writes):
        for b in reads:
            if b.r.get(k, 0) < v:
                b.r[k] = v
        for b in writes:
            if b.r:
                b.w = {k: v}
                b.r = {}
            else:
                b.w[k] = v

    def op(self, e, fn, reads=(), writes=()):
        self._collect(e, reads, writes, False)
        inst = fn(self.E[e])
        if self.ccnt[e] >= self.EPOCH:
            self._new_epoch(e)
        self.ccnt[e] += 1
        k, v = self.ckey[e], self.ccnt[e]
        inst.then_inc(self.sems[k], 1)
        self._commit(k, v, reads, writes)
        self.n_inst += 1

    def dma(self, q, out, in_, reads=(), writes=()):
        self._collect(q, reads, writes, True)
        i = self.dcnt[q]
        self.dcnt[q] += 1
        k = self.dkeys[q][i % self.NR]
        prev = 16 * (i // self.NR)
        if prev:
            self._wait(q, k, prev)
        inst = self.E[q].dma_start(out=out, in_=in_)
        inst.then_inc(self.sems[k], 16)
        v = prev + 16
        assert v < 60000
        self.dlast[k] = v
        self._commit(k, v, reads, writes)
        self.n_inst += 1

    def barrier(self):
        tick = {}
        for e in ("pe", "act", "dve", "pool"):
            if self.ccnt[e]:
                tick[self.ckey[e]] = self.ccnt[e]
        tick.update(self.dlast)
        for e in self.E:
            for k, v in tick.items():
                self._wait(e, k, v)


def _token_blocks(with_ctx=True):
    bl = [(i * 512, 512) for i in range(SEQ // 512)]
    if with_ctx:
        bl.append((SEQ, NCTX))
    return bl


class Builder:
    topk_eng = "dve"

    def __init__(self, layers, first, last):
        self.layers = layers
        self.first = first
        self.last = last
        nc = bass.Bass("TRN2", target_bir_lowering=False)
        self.nc = nc
        self.S = Sched(nc)
        self.rot = {}
        self._decl_dram()
        self._build()

    def din(self, name, shape, dt=F32, kinds=None):
        if kinds is not None and not (set(kinds) & set(l % 4 for l in self.layers)):
            return None
        if not hasattr(self, "in_names"):
            self.in_names = []
        self.in_names.append(name)
        return self.nc.dram_tensor(name, list(shape), dt, kind="ExternalInput").ap()

    def dscr(self, name, shape, dt):
        return self.nc.dram_tensor(name, list(shape), dt, kind="Internal").ap()

    def sb(self, es, name, shape, dt):
        self._uid = getattr(self, "_uid", 0) + 1
        t = es.enter_context(self.nc.sbuf_tensor(f"{name}_u{self._uid}", list(shape), dt))
        return t

    def nxt(self, key, n):
        i = self.rot.get(key, 0)
        self.rot[key] = i + 1
        return i % n

    def _decl_dram(self):
        nc = self.nc
        self.x_in = self.din("xT_in", [D, T])
        self.cc = self.din("cc", [128, 8, 2])
        self.ada_w = {l: self.din(f"ada_w{l}", [D, 6 * D]) for l in self.layers}
        self.ada_b = self.din("ada_b", [DEPTH, 128, 48, 2])
        self.norm_g = self.din("norm_g", [DEPTH, 2, 128, 8, 2])
        self.wr = self.din("wr", [DEPTH, D, 36])
        self.br = self.din("br", [DEPTH, 128, 36])
        self.w_gu = {l: self.din(f"w_gu{l}", [NE, D, 512]) for l in self.layers}
        self.w_dn = {l: self.din(f"w_dn{l}", [NE, 256, D]) for l in self.layers}
        self.wqkv = {0: self.din("na_w_qkv", [D, 3072], kinds=[0]), 1: self.din("sw_w_qkv", [D, 1536], kinds=[1]),
                     3: self.din("diff_w_qkv", [D, 3072], kinds=[3])}
        self.wo = {0: self.din("na_w_o", [D, D], kinds=[0]), 1: self.din("sw_w_o", [D, D], kinds=[1]),
                   2: self.din("mla_w_o", [D, D], kinds=[2]), 3: self.din("diff_w_o", [D, D], kinds=[3])}
        self.qkg = {0: self.din("na_qk_g", [128, 2], kinds=[0]), 1: self.din("sw_qk_g", [128, 2], kinds=[1]),
                    3: self.din("diff_qk_g", [128, 2], kinds=[3])}
        self.na_bias = self.din("na_bias", [16, 3, 128, 6, 512], kinds=[0])
        self.sw_sink = self.din("sw_sink", [128, 16], kinds=[1])
        self.sw_mask = self.din("sw_mask", [128, 6, 512], kinds=[1])
        self.rope64 = self.din("rope64", [2, 128, T], kinds=[1, 3])
        self.mla_w_dqkv = self.din("mla_w_dqkv", [D, 672], kinds=[2])
        self.mla_w_uq = self.din("mla_w_uq", [384, 1536], kinds=[2])
        self.mla_w_ukv = self.din("mla_w_ukv", [256, 2048], kinds=[2])
        self.mla_g = self.din("mla_g", [128, 8], kinds=[2])
        self.rope_mla = self.din("rope_mla", [2, 96, T], kinds=[2])
        self.diff_lam = self.din("diff_lam", [128, 4, 64], kinds=[3])
        self.diff_subln = self.din("diff_subln", [128, 1], kinds=[3])
        self.consts = self.din("consts", [128, 8, 128])
        self.sel_in = self.din("sel", [32, NE, 128])
        self.xs = self.dscr("xs", [D, T], F32)
        self.q_s = self.dscr("q_s", [16, 96, T], BF16)
        self.k_s = self.dscr("k_s", [16, 96, T], BF16)
        self.v_s = self.dscr("v_s", [16, 128, NKT, 64], BF16)
        self.kr_s = self.dscr("kr_s", [32, T], BF16)
        self.v_s2 = self.dscr("v_s2", [8, 128, NKT, 128], BF16)
        self.at_s = self.dscr("at_s", [D, T], BF16)
        self.h2_s = self.dscr("h2_s", [D, T], BF16)
        if self.last:
            self.out = nc.dram_tensor("outT", [D, SEQ], F32, kind="ExternalOutput").ap()
        else:
            self.out = nc.dram_tensor("xT_out", [D, T], F32, kind="ExternalOutput").ap()

    def _build(self):
        nc, S = self.nc, self.S
        with ExitStack() as es:
            self.ps = [nc.alloc_psum_tensor(f"ps{i}", [128, 512], F32) for i in range(8)]
            self.psb = [Buf(p) for p in self.ps]
            cst = self.sb(es, "cst_f", [128, 8, 128], F32)
            self.cst_f = cst
            cb = Buf(cst)
            S.dma("sp", cst[:, :, :], self.consts, writes=[cb])
            cstb = self.sb(es, "cst_b", [128, 8, 128], BF16)
            self.cstb_buf = Buf(cstb)
            self._cstb = cstb
            self.eps_col = self.sb(es, "eps_col", [128, 1], F32)
            self.eps_buf = Buf(self.eps_col)
            S.op("dve", lambda e: e.memset(self.eps_col[:, :], EPS), writes=[self.eps_buf])
            S.op("dve", lambda e: e.tensor_copy(out=cstb[:, :, :], in_=cst[:, :, :]), reads=[cb], writes=[self.cstb_buf])
            self.cst_buf = cb
            self.modT = self.sb(es, "modT", [128, DEPTH, 48, 2], F32)
            self.gsT = self.sb(es, "gsT", [128, DEPTH, 2, 8, 2], F32)
            self.mod_buf = Buf(self.modT)
            self.gs_buf = Buf(self.gsT)
            self.phase0()
            x_src = self.x_in
            for L in self.layers:
                kind = L % 4
                need_ctx = L < DEPTH - 1
                is_last_layer = (L == self.layers[-1])
                x_dst = self.xs
                S.barrier()
                with nc.named_scope(f"L{L}_p1"):
                    if kind == 2:
                        self.phase1_mla(L, x_src)
                    else:
                        self.phase1(L, x_src)
                    S.barrier()
                import os as _os
                _stop = int(_os.environ.get("K_STOP", "9"))
                with nc.named_scope(f"L{L}_p2"):
                    if _stop >= 2:
                        self.phase2(L, need_ctx)
                    S.barrier()
                if _stop >= 3:
                    self.phase3(L, x_src, x_dst, need_ctx, final=(is_last_layer))
                x_src = self.xs
            S.barrier()

    def phase0(self):
        nc, S = self.nc, self.S
        with ExitStack() as es:
            cc = self.sb(es, "cc", [128, 8, 2], F32)
            sc = self.sb(es, "scc", [128, 8, 2], F32)
            ccb, scb = Buf(cc), Buf(sc)
            S.dma("sp", cc[:, :, :], self.cc, writes=[ccb])
            S.op("act", lambda e: e.activation(out=sc[:, :, :], in_=cc[:, :, :], func=AF.Silu), reads=[ccb], writes=[scb])
            wm = [self.sb(es, f"wm{i}", [128, 8, 1024], F32) for i in range(2)]
            wmb = [Buf(w) for w in wm]
            adab = self.sb(es, "adab", [128, 48, 2], F32)
            adabb = Buf(adab)
            gn = self.sb(es, "gn", [128, 2, 8, 2], F32)
            gnb = Buf(gn)
            pm = self.ps[0]
            pmb = self.psb[0]
            for L in self.layers:
                S.dma("sp", adab[:, :, :], self.ada_b[L], writes=[adabb])
                S.dma("sp", gn[:, :, :, :], self.norm_g[L].rearrange("i p k t -> p i k t"), writes=[gnb])
                for which in range(6):
                    i = self.nxt("wm", 2)
                    src = self.ada_w[L].rearrange("(kc p) n -> p kc n", p=128)[:, :, which * 1024:(which + 1) * 1024]
                    S.dma("sp", wm[i][:, :, :], src, writes=[wmb[i]])
                    for fc in range(8):
                        j = which * 8 + fc
                        for kc in range(8):
                            S.op("pe", lambda e, i=i, fc=fc, kc=kc, j=j: e.matmul(
                                pm[:, 2 * j:2 * j + 2], lhsT=wm[i][:, kc, fc * 128:(fc + 1) * 128], rhs=sc[:, kc, :],
                                start=(kc == 0), stop=(kc == 7)), reads=[wmb[i], scb], writes=[pmb])
                mo = self.modT[:, L, :, :]
                S.op("dve", lambda e: e.tensor_tensor(out=mo, in0=pm[:, 0:96].rearrange("p (j t) -> p j t", t=2),
                                                      in1=adab[:, :, :], op=ALU.add),
                     reads=[pmb, adabb], writes=[self.mod_buf])
                for i2, sci in ((0, 1), (1, 4)):
                    g = self.gsT[:, L, i2, :, :]
                    S.op("dve", lambda e, g=g, sci=sci: e.tensor_scalar(
                        out=g, in0=self.modT[:, L, sci * 8:(sci + 1) * 8, :], scalar1=1.0, scalar2=None, op0=ALU.add),
                        reads=[self.mod_buf], writes=[self.gs_buf])
                    S.op("dve", lambda e, g=g, i2=i2: e.tensor_tensor(out=g, in0=g, in1=gn[:, i2, :, :], op=ALU.mult),
                         reads=[self.gs_buf, gnb], writes=[self.gs_buf])
            S.barrier()

    def modcol(self, L, which, fc, t):
        return self.modT[:, L, which * 8 + fc, t:t + 1]

    def gscol(self, L, i2, fc, t):
        return self.gsT[:, L, i2, fc, t:t + 1]

    def norm_mod(self, L, i2, xin, xb, n, t, rs, rsb, sq, sqb, tmp, tmpb, outs, pss):
        S = self.S
        ones_b = self.cstb[:, 0, :]
        pbank, pbuf = self.ps[pss], self.psb[pss]
        for kc in range(8):
            j = self.nxt("sq", 2)
            S.op("act", lambda e, j=j, kc=kc: e.activation(out=sq[j][:, :n], in_=xin[:, kc, :n], func=AF.Square),
                 reads=[xb], writes=[sqb[j]])
            S.op("pe", lambda e, j=j, kc=kc: e.matmul(pbank[:, :n], lhsT=ones_b, rhs=sq[j][:, :n],
                                                     start=(kc == 0), stop=(kc == 7)),
                 reads=[sqb[j], self.cstb_buf], writes=[pbuf])
        S.op("act", lambda e: e.activation(out=rs[:, :n], in_=pbank[:, :n], func=AF.Ln, bias=self.eps_col[:, 0:1], scale=1.0 / D),
             reads=[pbuf, self.eps_buf], writes=[rsb])
        S.op("act", lambda e: e.activation(out=rs[:, :n], in_=rs[:, :n], func=AF.Exp, scale=-0.5), reads=[rsb], writes=[rsb])
        sh = 0 if i2 == 0 else 3
        for kc in range(8):
            j = self.nxt("tmp", 2)
            S.op("dve", lambda e, j=j, kc=kc: e.tensor_tensor(out=tmp[j][:, :n], in0=xin[:, kc, :n], in1=rs[:, :n], op=ALU.mult),
                 reads=[xb, rsb], writes=[tmpb[j]])
            first = True
            for (ot, ob, eng) in outs:
                if first:
                    S.op("act", lambda e, j=j, kc=kc, ot=ot: e.activation(
                        out=ot[:, kc, :n], in_=tmp[j][:, :n], func=AF.Identity,
                        scale=self.gscol(L, i2, kc, t), bias=self.modcol(L, sh, kc, t)),
                        reads=[tmpb[j], self.gs_buf, self.mod_buf], writes=[ob])
                    first = False
                    prev_t, prev_b = ot, ob
                else:
                    S.op(eng, lambda e, kc=kc, ot=ot, prev_t=prev_t: e.tensor_copy(out=ot[:, kc, :n], in_=prev_t[:, kc, :n]),
                         reads=[prev_b], writes=[ob])

    @property
    def cstb(self):
        return self._cstb

    def phase1(self, L, x_src):
        nc, S = self.nc, self.S
        kind = L % 4
        ncol = {0: 3072, 1: 1536, 3: 3072}[kind]
        nq_ch = 8
        nk_ch = {0: 8, 1: 2, 3: 8}[kind]
        kcol0 = 1024
        vcol0 = {0: 2048, 1: 1280, 3: 2048}[kind]
        vw = {0: 1024, 1: 256, 3: 1024}[kind]
        dv = {0: 64, 1: 64, 3: 128}[kind]
        rope = kind in (1, 3)
        with ExitStack() as es:
            W = self.sb(es, "w1", [128, 8, ncol], BF16)
            Wb = [Buf(W) for _ in range(8)]
            wsrc = self.wqkv[kind].rearrange("(kc p) n -> p kc n", p=128)
            for kc in range(8):
                S.dma("pool", W[:, kc, :], wsrc[:, kc, :], writes=[Wb[kc]])
            qkg = self.sb(es, "qkg", [128, 2], F32)
            qkgb = Buf(qkg)
            S.dma("sp", qkg[:, :], self.qkg[kind], writes=[qkgb])
            xin = [self.sb(es, f"xin{i}", [128, 8, 512], F32) for i in range(2)]
            xinb = [Buf(t) for t in xin]
            hT = [self.sb(es, f"hT{i}", [128, 8, 512], BF16) for i in range(2)]
            hTb = [Buf(t) for t in hT]
            rs = self.sb(es, "rs", [128, 512], F32); rsb = Buf(rs)
            sq = [self.sb(es, f"sq{i}", [128, 512], BF16) for i in range(2)]; sqb = [Buf(t) for t in sq]
            tmp = [self.sb(es, f"tmp{i}", [128, 512], F32) for i in range(2)]; tmpb = [Buf(t) for t in tmp]
            rs2 = [self.sb(es, f"rs2{i}", [128, 512], F32) for i in range(2)]; rs2b = [Buf(t) for t in rs2]
            qn = [self.sb(es, f"qn{i}", [128, 512], BF16) for i in range(3)]; qnb = [Buf(t) for t in qn]
            qo = [self.sb(es, f"qo{i}", [128, 512], BF16) for i in range(3)]; qob = [Buf(t) for t in qo]
            t1 = [self.sb(es, f"t1{i}", [128, 512], F32) for i in range(2)]; t1b = [Buf(t) for t in t1]
            t2 = [self.sb(es, f"t2{i}", [128, 512], F32) for i in range(2)]; t2b = [Buf(t) for t in t2]
            vt = [self.sb(es, f"vt{i}", [128, 512], BF16) for i in range(3)]; vtb = [Buf(t) for t in vt]
            if rope:
                cs = [self.sb(es, f"cs{i}", [128, 2, 512], F32) for i in range(2)]
                csb = [Buf(t) for t in cs]
            blocks = _token_blocks(True)
            xsrc = x_src.rearrange("(kc p) t -> p kc t", p=128)

            def load(bi):
                t0, n = blocks[bi]
                i = bi % 2
                S.dma("sp", xin[i][:, :, :n], xsrc[:, :, t0:t0 + n], writes=[xinb[i]])
                if rope:
                    S.dma("sp", cs[i][:, :, :n], self.rope64.rearrange("c p t -> p c t")[:, :, t0:t0 + n], writes=[csb[i]])

            load(0)
            blk64 = self.cstb[:, 1, :]
            Rm = self.cstb[:, 2, :]
            for bi, (t0, n) in enumerate(blocks):
                if bi + 1 < len(blocks):
                    load(bi + 1)
                i = bi % 2
                tq = 0 if t0 < SEQ else 1
                self.norm_mod(L, 0, xin[i], xinb[i], n, tq, rs, rsb, sq, sqb, tmp, tmpb, [(hT[i], hTb[i], "act")], 7)
                nch = nq_ch + nk_ch
                cst = {}
                p1q_banks = [0, 1, 6]

                def stP(c):
                    isq = c < nq_ch
                    col0 = c * 128 if isq else kcol0 + (c - nq_ch) * 128
                    pb = p1q_banks[self.nxt("p1q3", 3)]
                    pq, pqb = self.ps[pb], self.psb[pb]
                    for kc in range(8):
                        S.op("pe", lambda e, kc=kc: e.matmul(
                            pq[:, :n], lhsT=W[:, kc, col0:col0 + 128], rhs=hT[i][:, kc, :n], start=(kc == 0), stop=(kc == 7)),
                            reads=[Wb[kc], hTb[i]], writes=[pqb])
                    cst[c] = dict(pq=pq, pqb=pqb, isq=isq)

                def stN(c):
                    d_ = cst[c]
                    pq, pqb, isq = d_["pq"], d_["pqb"], d_["isq"]
                    j = self.nxt("sq", 2)
                    S.op("act", lambda e: e.activation(out=sq[j][:, :n], in_=pq[:, :n], func=AF.Square),
                         reads=[pqb], writes=[sqb[j]])
                    pmi = 2 + self.nxt("p1m", 2)
                    pm, pmb = self.ps[pmi], self.psb[pmi]
                    S.op("pe", lambda e: e.matmul(pm[:, :n], lhsT=blk64, rhs=sq[j][:, :n], start=True, stop=True),
                         reads=[sqb[j], self.cstb_buf], writes=[pmb])
                    r = self.nxt("rs2", 2)
                    S.op("act", lambda e: e.activation(out=rs2[r][:, :n], in_=pm[:, :n], func=AF.Ln,
                                                       bias=self.eps_col[:, 0:1], scale=1.0),
                         reads=[pmb, self.eps_buf], writes=[rs2b[r]])
                    S.op("act", lambda e: e.activation(out=rs2[r][:, :n], in_=rs2[r][:, :n], func=AF.Exp, scale=-0.5), reads=[rs2b[r]], writes=[rs2b[r]])
                    gcol = qkg[:, 0:1] if isq else qkg[:, 1:2]
                    if rope and tq == 0:
                        a = self.nxt("qn", 3)
                        S.op("dve", lambda e: e.scalar_tensor_tensor(
                            out=qn[a][:, :n], in0=pq[:, :n], scalar=gcol, in1=rs2[r][:, :n], op0=ALU.mult, op1=ALU.mult),
                            reads=[pqb, rs2b[r], qkgb], writes=[qnb[a]])
                        d_["a"] = a
                    else:
                        o = self.nxt("qo", 3)
                        S.op("dve", lambda e: e.scalar_tensor_tensor(
                            out=qo[o][:, :n], in0=pq[:, :n], scalar=gcol, in1=rs2[r][:, :n], op0=ALU.mult, op1=ALU.mult),
                            reads=[pqb, rs2b[r], qkgb], writes=[qob[o]])
                        d_["o"] = o

                def stR(c):
                    d_ = cst[c]
                    isq = d_["isq"]
                    dst_s = self.q_s if isq else self.k_s
                    hh = (c if isq else c - nq_ch) * 2
                    if "a" in d_:
                        a = d_["a"]
                        pri = 4 + self.nxt("p1r", 2)
                        pr, prb = self.ps[pri], self.psb[pri]
                        S.op("pe", lambda e: e.matmul(pr[:, :n], lhsT=Rm, rhs=qn[a][:, :n], start=True, stop=True),
                             reads=[qnb[a], self.cstb_buf], writes=[prb])
                        u = self.nxt("t1", 2)
                        S.op("dve", lambda e: e.tensor_tensor(out=t1[u][:, :n], in0=qn[a][:, :n], in1=cs[i][:, 0, :n], op=ALU.mult),
                             reads=[qnb[a], csb[i]], writes=[t1b[u]])
                        S.op("dve", lambda e: e.tensor_tensor(out=t2[u][:, :n], in0=pr[:, :n], in1=cs[i][:, 1, :n], op=ALU.mult),
                             reads=[prb, csb[i]], writes=[t2b[u]])
                        o = self.nxt("qo", 3)
                        S.op("pool", lambda e: e.tensor_tensor(out=qo[o][:, :n], in0=t1[u][:, :n], in1=t2[u][:, :n], op=ALU.add),
                             reads=[t1b[u], t2b[u]], writes=[qob[o]])
                    else:
                        o = d_["o"]
                    for h2 in range(2):
                        S.dma("sp", dst_s[hh + h2, 0:64, t0:t0 + n], qo[o][h2 * 64:(h2 + 1) * 64, :n], reads=[qob[o]])

                for it_ in range(nch + 2):
                    if it_ < nch:
                        stP(it_)
                    if 0 <= it_ - 1 < nch:
                        stN(it_ - 1)
                    if 0 <= it_ - 2 < nch:
                        stR(it_ - 2)
                for tt in range(n // 128):
                    kt = (t0 // 128) + tt
                    for cbk in range((vw + 511) // 512):
                        w = min(512, vw - cbk * 512)
                        pvi = 4 + self.nxt("p1r", 2)
                        pv, pvb = self.ps[pvi], self.psb[pvi]
                        for kc in range(8):
                            S.op("pe", lambda e, kc=kc, pv=pv, tt=tt, cbk=cbk, w=w: e.matmul(
                                pv[:, :w], lhsT=hT[i][:, kc, tt * 128:(tt + 1) * 128],
                                rhs=W[:, kc, vcol0 + cbk * 512: vcol0 + cbk * 512 + w], start=(kc == 0), stop=(kc == 7)),
                                reads=[Wb[kc], hTb[i]], writes=[pvb])
                        vi = self.nxt("vt", 3)
                        S.op("act", lambda e, vi=vi, pv=pv, w=w: e.activation(out=vt[vi][:, :w], in_=pv[:, :w], func=AF.Copy),
                             reads=[pvb], writes=[vtb[vi]])
                        nh = w // dv
                        h0 = cbk * 512 // dv
                        if dv == 64:
                            dst = self.v_s[h0:h0 + nh, :, kt, :].rearrange("h p d -> p h d")
                        else:
                            dst = self.v_s2[h0:h0 + nh, :, kt, :].rearrange("h p d -> p h d")
                        S.dma("sp", dst, vt[vi][:, :w].rearrange("p (h d) -> p h d", d=dv), reads=[vtb[vi]])

    def phase1_mla(self, L, x_src):
        nc, S = self.nc, self.S
        with ExitStack() as es:
            W = self.sb(es, "w1", [128, 8, 672], BF16)
            Wb = [Buf(W) for _ in range(8)]
            wsrc = self.mla_w_dqkv.rearrange("(kc p) n -> p kc n", p=128)
            for kc in range(8):
                S.dma("pool", W[:, kc, :], wsrc[:, kc, :], writes=[Wb[kc]])
            Wuq = self.sb(es, "wuq", [128, 3, 1536], BF16); Wuqb = Buf(Wuq)
            S.dma("pool", Wuq[:, :, :], self.mla_w_uq.rearrange("(kc p) n -> p kc n", p=128), writes=[Wuqb])
            Wuk = self.sb(es, "wuk", [128, 2, 16, 64], BF16); Wukb = Buf(Wuk)
            Wuv = self.sb(es, "wuv", [128, 2, 16, 64], BF16); Wuvb = Buf(Wuv)
            ukv = self.mla_w_ukv.rearrange("(kc p) (h two d) -> p kc h two d", p=128, two=2, d=64)
            for kc in range(2):
                S.dma("pool", Wuk[:, kc, :, :], ukv[:, kc, :, 0, :], writes=[Wukb])
                S.dma("pool", Wuv[:, kc, :, :], ukv[:, kc, :, 1, :], writes=[Wuvb])
            mg = self.sb(es, "mg", [128, 8], F32); mgb = Buf(mg)
            S.dma("sp", mg[:, :], self.mla_g, writes=[mgb])
            xin = [self.sb(es, f"xin{i}", [128, 8, 512], F32) for i in range(2)]
            xinb = [Buf(t) for t in xin]
            hT = [self.sb(es, f"hT{i}", [128, 8, 512], BF16) for i in range(2)]
            hTb = [Buf(t) for t in hT]
            rs = self.sb(es, "rs", [128, 512], F32); rsb = Buf(rs)
            sq = [self.sb(es, f"sq{i}", [128, 512], BF16) for i in range(2)]; sqb = [Buf(t) for t in sq]
            tmp = [self.sb(es, f"tmp{i}", [128, 512], F32) for i in range(2)]; tmpb = [Buf(t) for t in tmp]
            rs2 = [self.sb(es, f"rs2{i}", [128, 512], F32) for i in range(2)]; rs2b = [Buf(t) for t in rs2]
            cf = self.sb(es, "cf", [128, 6, 512], F32); cfb = [Buf(cf) for _ in range(6)]
            cn = self.sb(es, "cn", [128, 5, 512], BF16); cnb = [Buf(cn) for _ in range(5)]
            qn = [self.sb(es, f"qn{i}", [128, 512], BF16) for i in range(3)]; qnb = [Buf(t) for t in qn]
            qo = [self.sb(es, f"qo{i}", [128, 512], BF16) for i in range(3)]; qob = [Buf(t) for t in qo]
            t1 = [self.sb(es, f"t1{i}", [128, 512], F32) for i in range(2)]; t1b = [Buf(t) for t in t1]
            t2 = [self.sb(es, f"t2{i}", [128, 512], F32) for i in range(2)]; t2b = [Buf(t) for t in t2]
            vt = [self.sb(es, f"vt{i}", [128, 512], BF16) for i in range(3)]; vtb = [Buf(t) for t in vt]
            cs = [self.sb(es, f"cs{i}", [96, 2, 512], F32) for i in range(2)]
            csb = [Buf(t) for t in cs]
            self.krt = [self.sb(es, f"krt{i}", [32, 2, 512], F32) for i in range(2)]
            self.krtb = [Buf(t) for t in self.krt]
            blocks = _token_blocks(True)
            xsrc = x_src.rearrange("(kc p) t -> p kc t", p=128)
            ones_b = self.cstb[:, 0, :]
            blk64 = self.cstb[:, 1, :]
            blkm = self.cstb[:, 5, :]
            Rmm = self.cstb[:, 6, :]

            def load(bi):
                t0, n = blocks[bi]
                i = bi % 2
                S.dma("sp", xin[i][:, :, :n], xsrc[:, :, t0:t0 + n], writes=[xinb[i]])
                S.dma("sp", cs[i][:, :, :n], self.rope_mla.rearrange("c p t -> p c t")[:, :, t0:t0 + n], writes=[csb[i]])
                S.dma("sp", self.krt[i][:, :, :n], self.rope_mla.rearrange("c p t -> p c t")[64:96, :, t0:t0 + n], writes=[self.krtb[i]])

            def rsq(pm, pmb, n, P, scale):
                r = self.nxt("rs2", 2)
                S.op("act", lambda e: e.activation(out=rs2[r][:P, :n], in_=pm[:P, :n], func=AF.Ln,
                                                   bias=self.eps_col[:P, 0:1], scale=scale),
                     reads=[pmb, self.eps_buf], writes=[rs2b[r]])
                S.op("act", lambda e: e.activation(out=rs2[r][:P, :n], in_=rs2[r][:P, :n], func=AF.Exp, scale=-0.5), reads=[rs2b[r]], writes=[rs2b[r]])
                return r

            load(0)
            for bi, (t0, n) in enumerate(blocks):
                if bi + 1 < len(blocks):
                    load(bi + 1)
                i = bi % 2
                tq = 0 if t0 < SEQ else 1
                self.norm_mod(L, 0, xin[i], xinb[i], n, tq, rs, rsb, sq, sqb, tmp, tmpb, [(hT[i], hTb[i], "act")], 7)
                for c in range(6):
                    M = 128 if c < 5 else 32
                    pb = self.nxt("p1q", 2)
                    pq, pqb = self.ps[pb], self.psb[pb]
                    for kc in range(8):
                        S.op("pe", lambda e, kc=kc, c=c, pq=pq, M=M: e.matmul(
                            pq[:M, :n], lhsT=W[:, kc, c * 128:c * 128 + M], rhs=hT[i][:, kc, :n], start=(kc == 0), stop=(kc == 7)),
                            reads=[Wb[kc], hTb[i]], writes=[pqb])
                    S.op("act", lambda e, c=c, pq=pq, M=M: e.activation(out=cf[:M, c, :n], in_=pq[:M, :n], func=AF.Copy),
                         reads=[pqb], writes=[cfb[c]])
                for (c0, c1, dim) in ((0, 3, 384), (3, 5, 256)):
                    pmi = 2 + self.nxt("p1m", 2)
                    pm, pmb = self.ps[pmi], self.psb[pmi]
                    for c in range(c0, c1):
                        j = self.nxt("sq", 2)
                        S.op("act", lambda e, j=j, c=c: e.activation(out=sq[j][:, :n], in_=cf[:, c, :n], func=AF.Square),
                             reads=[cfb[c]], writes=[sqb[j]])
                        S.op("pe", lambda e, j=j, c=c, pm=pm: e.matmul(pm[:, :n], lhsT=ones_b, rhs=sq[j][:, :n],
                                                                      start=(c == c0), stop=(c == c1 - 1)),
                             reads=[sqb[j], self.cstb_buf], writes=[pmb])
                    r = rsq(pm, pmb, n, 128, 1.0 / dim)
                    for c in range(c0, c1):
                        S.op("dve", lambda e, c=c, r=r: e.scalar_tensor_tensor(
                            out=cn[:, c, :n], in0=cf[:, c, :n], scalar=mg[:, c:c + 1], in1=rs2[r][:, :n], op0=ALU.mult, op1=ALU.mult),
                            reads=[cfb[c], rs2b[r], mgb], writes=[cnb[c]])
                j = self.nxt("sq", 2)
                S.op("act", lambda e, j=j: e.activation(out=sq[j][:32, :n], in_=cf[:32, 5, :n], func=AF.Square),
                     reads=[cfb[5]], writes=[sqb[j]])
                pmi = 2 + self.nxt("p1m", 2)
                pm, pmb = self.ps[pmi], self.psb[pmi]
                S.op("pe", lambda e, j=j, pm=pm: e.matmul(pm[:32, :n], lhsT=self.cstb[:32, 0, 0:32], rhs=sq[j][:32, :n], start=True, stop=True),
                     reads=[sqb[j], self.cstb_buf], writes=[pmb])
                r = rsq(pm, pmb, n, 32, 1.0 / 32)
                a = self.nxt("qn", 3)
                S.op("dve", lambda e, a=a, r=r: e.scalar_tensor_tensor(
                    out=qn[a][:32, :n], in0=cf[:32, 5, :n], scalar=mg[:32, 7:8], in1=rs2[r][:32, :n], op0=ALU.mult, op1=ALU.mult),
                    reads=[cfb[5], rs2b[r], mgb], writes=[qnb[a]])
                pri = 4 + self.nxt("p1r", 2)
                pr, prb = self.ps[pri], self.psb[pri]
                S.op("pe", lambda e, a=a, pr=pr: e.matmul(pr[:32, :n], lhsT=self.cstb[:32, 7, 0:32], rhs=qn[a][:32, :n], start=True, stop=True),
                     reads=[qnb[a], self.cstb_buf], writes=[prb])
                u = self.nxt("t1", 2)
                self._mla_krope(L, S, a, u, pr, prb, qn, qnb, t1, t1b, t2, t2b, qo, qob, n, t0, i)
                p1q_banks = [0, 1, 6]
                hst = {}

                def qP(h):
                    pb = p1q_banks[self.nxt("p1q3", 3)]
                    pq, pqb = self.ps[pb], self.psb[pb]
                    for kc in range(3):
                        S.op("pe", lambda e, kc=kc: e.matmul(
                            pq[:96, :n], lhsT=Wuq[:, kc, h * 96:(h + 1) * 96], rhs=cn[:, kc, :n], start=(kc == 0), stop=(kc == 2)),
                            reads=[Wuqb, cnb[kc]], writes=[pqb])
                    hst[h] = dict(pq=pq, pqb=pqb)

                def qN(h):
                    pq, pqb = hst[h]["pq"], hst[h]["pqb"]
                    j = self.nxt("sq", 2)
                    S.op("act", lambda e: e.activation(out=sq[j][:96, :n], in_=pq[:96, :n], func=AF.Square),
                         reads=[pqb], writes=[sqb[j]])
                    pmi = 2 + self.nxt("p1m", 2)
                    pm, pmb = self.ps[pmi], self.psb[pmi]
                    S.op("pe", lambda e: e.matmul(pm[:96, :n], lhsT=blkm[:96, 0:96], rhs=sq[j][:96, :n], start=True, stop=True),
                         reads=[sqb[j], self.cstb_buf], writes=[pmb])
                    r = rsq(pm, pmb, n, 96, 1.0)
                    a = self.nxt("qn", 3)
                    S.op("dve", lambda e: e.scalar_tensor_tensor(
                        out=qn[a][:96, :n], in0=pq[:96, :n], scalar=mg[:96, 5:6], in1=rs2[r][:96, :n], op0=ALU.mult, op1=ALU.mult),
                        reads=[pqb, rs2b[r], mgb], writes=[qnb[a]])
                    hst[h]["a"] = a

                def qR(h):
                    a = hst[h]["a"]
                    pri = 4 + self.nxt("p1r", 2)
                    pr, prb = self.ps[pri], self.psb[pri]
                    S.op("pe", lambda e: e.matmul(pr[:96, :n], lhsT=Rmm[:96, 0:96], rhs=qn[a][:96, :n], start=True, stop=True),
                         reads=[qnb[a], self.cstb_buf], writes=[prb])
                    u = self.nxt("t1", 2)
                    S.op("dve", lambda e: e.tensor_tensor(out=t1[u][:96, :n], in0=qn[a][:96, :n], in1=cs[i][:96, 0, :n], op=ALU.mult),
                         reads=[qnb[a], csb[i]], writes=[t1b[u]])
                    S.op("dve", lambda e: e.tensor_tensor(out=t2[u][:96, :n], in0=pr[:96, :n], in1=cs[i][:96, 1, :n], op=ALU.mult),
                         reads=[prb, csb[i]], writes=[t2b[u]])
                    o = self.nxt("qo", 3)
                    S.op("pool", lambda e: e.tensor_tensor(out=qo[o][:96, :n], in0=t1[u][:96, :n], in1=t2[u][:96, :n], op=ALU.add),
                         reads=[t1b[u], t2b[u]], writes=[qob[o]])
                    S.dma("sp", self.q_s[h, 0:96, t0:t0 + n], qo[o][:96, :n], reads=[qob[o]])

                for it_ in range(16 + 2):
                    if it_ < 16:
                        qP(it_)
                    if 0 <= it_ - 1 < 16:
                        qN(it_ - 1)
                    if 0 <= it_ - 2 < 16:
                        qR(it_ - 2)

                kst = {}

                def kP(c):
                    pb = p1q_banks[self.nxt("p1q3", 3)]
                    pq, pqb = self.ps[pb], self.psb[pb]
                    for kc in range(2):
                        S.op("pe", lambda e, kc=kc: e.matmul(
                            pq[:, :n], lhsT=Wuk[:, kc, 2 * c:2 * c + 2, :].rearrange("p h d -> p (h d)"), rhs=cn[:, 3 + kc, :n],
                            start=(kc == 0), stop=(kc == 1)),
                            reads=[Wukb, cnb[3 + kc]], writes=[pqb])
                    kst[c] = (pq, pqb)

                def kN(c):
                    pq, pqb = kst[c]
                    j = self.nxt("sq", 2)
                    S.op("act", lambda e: e.activation(out=sq[j][:, :n], in_=pq[:, :n], func=AF.Square),
                         reads=[pqb], writes=[sqb[j]])
                    pmi = 2 + self.nxt("p1m", 2)
                    pm, pmb = self.ps[pmi], self.psb[pmi]
                    S.op("pe", lambda e: e.matmul(pm[:, :n], lhsT=blk64, rhs=sq[j][:, :n], start=True, stop=True),
                         reads=[sqb[j], self.cstb_buf], writes=[pmb])
                    kst[c] = (pq, pqb, pm, pmb)

                def kR(c):
                    pq, pqb, pm, pmb = kst[c]
                    r = rsq(pm, pmb, n, 128, 1.0)
                    o = self.nxt("qo", 3)
                    S.op("dve", lambda e: e.scalar_tensor_tensor(
                        out=qo[o][:, :n], in0=pq[:, :n], scalar=mg[:, 6:7], in1=rs2[r][:, :n], op0=ALU.mult, op1=ALU.mult),
                        reads=[pqb, rs2b[r], mgb], writes=[qob[o]])
                    for h2 in range(2):
                        S.dma("sp", self.k_s[2 * c + h2, 0:64, t0:t0 + n], qo[o][h2 * 64:(h2 + 1) * 64, :n], reads=[qob[o]])

                for it_ in range(8 + 2):
                    if it_ < 8:
                        kP(it_)
                    if 0 <= it_ - 1 < 8:
                        kN(it_ - 1)
                    if 0 <= it_ - 2 < 8:
                        kR(it_ - 2)
                for tt in range(n // 128):
                    kt = (t0 // 128) + tt
                    for cbk in range(2):
                        pvi = 4 + self.nxt("p1r", 2)
                        pv, pvb = self.ps[pvi], self.psb[pvi]
                        for kc in range(2):
                            S.op("pe", lambda e, kc=kc, pv=pv, tt=tt, cbk=cbk: e.matmul(
                                pv[:, :512], lhsT=cn[:, 3 + kc, tt * 128:(tt + 1) * 128],
                                rhs=Wuv[:, kc, cbk * 8:(cbk + 1) * 8, :].rearrange("p h d -> p (h d)"), start=(kc == 0), stop=(kc == 1)),
                                reads=[Wuvb, cnb[3 + kc]], writes=[pvb])
                        vi = self.nxt("vt", 3)
                        S.op("act", lambda e, vi=vi, pv=pv: e.activation(out=vt[vi][:, :512], in_=pv[:, :512], func=AF.Copy),
                             reads=[pvb], writes=[vtb[vi]])
                        dst = self.v_s[cbk * 8:(cbk + 1) * 8, :, kt, :].rearrange("h p d -> p h d")
                        S.dma("sp", dst, vt[vi][:, :512].rearrange("p (h d) -> p h d", d=64), reads=[vtb[vi]])

    def _mla_krope(self, L, S, a, u, pr, prb, qn, qnb, t1, t1b, t2, t2b, qo, qob, n, t0, i):
        kt_ = self.krt[i]
        kb_ = self.krtb[i]
        S.op("pool", lambda e: e.tensor_tensor(out=t1[u][:32, :n], in0=qn[a][:32, :n], in1=kt_[:32, 0, :n], op=ALU.mult),
             reads=[qnb[a], kb_], writes=[t1b[u]])
        S.op("dve", lambda e: e.tensor_tensor(out=t2[u][:32, :n], in0=pr[:32, :n], in1=kt_[:32, 1, :n], op=ALU.mult),
             reads=[prb, kb_], writes=[t2b[u]])
        o = self.nxt("qo", 3)
        S.op("pool", lambda e: e.tensor_tensor(out=qo[o][:32, :n], in0=t1[u][:32, :n], in1=t2[u][:32, :n], op=ALU.add),
             reads=[t1b[u], t2b[u]], writes=[qob[o]])
        S.dma("sp", self.kr_s[:, t0:t0 + n], qo[o][:32, :n], reads=[qob[o]])

    def phase2(self, L, need_ctx):
        nc, S = self.nc, self.S
        kind = L % 4
        dq = 96 if kind == 2 else 64
        dv = 128 if kind == 3 else 64
        scale = dq ** -0.5
        ones_b = self.cstb[:, 0, :]
        with ExitStack() as es:
            nkv = 2 if kind == 3 else 1
            kT = [[self.sb(es, f"kT{i}_{j}", [128, T], BF16) for j in range(nkv)] for i in range(2)]
            kTb = [[Buf(t) for t in row] for row in kT]
            V = [self.sb(es, f"V{i}", [128, NKT, 128], BF16) for i in range(2)]
            Vb = [Buf(t) for t in V]
            qt = [self.sb(es, f"qt{i}", [128, 512], BF16) for i in range(3)]; qtb = [Buf(t) for t in qt]
            for row in kT:
                for t_ in row:
                    pass
            for i_, row in enumerate(kT):
                for j_, t_ in enumerate(row):
                    S.op("pool", lambda e, t_=t_: e.memset(t_[dq:128, :], 0.0), writes=[kTb[i_][j_]])
            for i_, t_ in enumerate(qt):
                S.op("pool", lambda e, t_=t_: e.memset(t_[dq:128, :], 0.0), writes=[qtb[i_]])
            LOOK = 4 if dv == 64 else 3
            sbanks = [0, 1, 2, 5, 6] if dv == 64 else [0, 1, 2, 7]
            NPT = LOOK + 2
            pt = [self.sb(es, f"pt{i}", [128, 512], BF16) for i in range(NPT)]; ptb = [Buf(t) for t in pt]
            rd = [self.sb(es, f"rd{i}", [128, 512], F32) for i in range(2)]; rdb = [Buf(t) for t in rd]
            ot = [self.sb(es, f"ot{i}", [128, 512], BF16) for i in range(2)]; otb = [Buf(t) for t in ot]
            if dv != 64:
                pp = [self.sb(es, f"pp{i}", [128, 512], BF16) for i in range(4)]; ppb = [Buf(t) for t in pp]
                pq4 = [self.sb(es, f"pq4{i}", [128, 512], BF16) for i in range(3)]; pq4b = [Buf(t) for t in pq4]
            if kind == 0:
                bt = [self.sb(es, f"bt{i}", [128, 6, 512], F32) for i in range(2)]; btb = [Buf(t) for t in bt]
                tb = [self.sb(es, f"tb{i}", [128, 512], F32) for i in range(3)]; tbb = [Buf(t) for t in tb]
            if kind == 1:
                mk = self.sb(es, "mk", [128, 6, 512], BF16); mkb = Buf(mk)
                S.dma("pool", mk[:, :, :], self.sw_mask, writes=[mkb])
                snk = self.sb(es, "snk", [128, 16], F32); snkb = Buf(snk)
                S.dma("sp", snk[:, :], self.sw_sink, writes=[snkb])
                S.op("act", lambda e: e.activation(out=snk[:, :], in_=snk[:, :], func=AF.Exp), reads=[snkb], writes=[snkb])
                p0 = [self.sb(es, f"p0{i}", [128, 512], BF16) for i in range(3)]; p0b = [Buf(t) for t in p0]
            if kind == 3:
                lam = self.sb(es, "lam", [128, 4, 64], F32); lamb = Buf(lam)
                S.dma("sp", lam[:, :, :], self.diff_lam, writes=[lamb])
                lt = self.sb(es, "lt", [128, 2, 64], F32); ltb = Buf(lt)
                ls = self.sb(es, "ls", [128, 4], F32); lsb = Buf(ls)
                S.op("dve", lambda e: e.tensor_tensor(out=lt[:, 0, :], in0=lam[:, 0, :], in1=lam[:, 1, :], op=ALU.mult), reads=[lamb], writes=[ltb])
                S.op("dve", lambda e: e.tensor_tensor(out=lt[:, 1, :], in0=lam[:, 2, :], in1=lam[:, 3, :], op=ALU.mult), reads=[lamb], writes=[ltb])
                S.op("dve", lambda e: e.tensor_reduce(out=ls[:, 0:2], in_=lt[:, :, :], axis=mybir.AxisListType.X, op=ALU.add), reads=[ltb], writes=[lsb])
                S.op("act", lambda e: e.activation(out=ls[:, 0:2], in_=ls[:, 0:2], func=AF.Exp), reads=[lsb], writes=[lsb])
                lam_init = 0.8 - 0.6 * math.exp(-0.3 * L)
                S.op("dve", lambda e: e.tensor_tensor(out=ls[:, 2:3], in0=ls[:, 1:2], in1=ls[:, 0:1], op=ALU.subtract), reads=[lsb], writes=[lsb])
                S.op("dve", lambda e: e.tensor_scalar(out=ls[:, 2:3], in0=ls[:, 2:3], scalar1=-lam_init, scalar2=None, op0=ALU.add), reads=[lsb], writes=[lsb])
                sg = self.sb(es, "sg", [128, 1], F32); sgb = Buf(sg)
                S.dma("sp", sg[:, :], self.diff_subln, writes=[sgb])
                S.op("dve", lambda e: e.tensor_scalar(out=sg[:, :], in0=sg[:, :], scalar1=1.0 - lam_init, scalar2=None, op0=ALU.mult), reads=[sgb], writes=[sgb])
                o1 = [self.sb(es, f"o1{i}", [128, 512], F32) for i in range(2)]; o1b = [Buf(t) for t in o1]
                od = [self.sb(es, f"od{i}", [128, 512], F32) for i in range(2)]; odb = [Buf(t) for t in od]
                sqd = [self.sb(es, f"sqd{i}", [128, 512], BF16) for i in range(2)]; sqdb = [Buf(t) for t in sqd]

            if kind in (0, 2):
                groups = [([h], h, [(h, 0)]) for h in range(16)]
            elif kind == 1:
                groups = [([g], g, [(4 * g + i, 0) for i in range(4)]) for g in range(4)]
            else:
                groups = [([2 * g, 2 * g + 1], g, [(2 * g, 0), (2 * g + 1, 1)]) for g in range(8)]

            qblocks = []
            for qi in range(8):
                tiles = []
                pat = 0
                if kind == 0:
                    r0 = 8 * qi
                    kr0 = min(max(r0 - 4, 0), 52)
                    pat = 0 if qi == 0 else (2 if qi == 7 else 1)
                    tiles = [(kr0 // 2 + j, "bias", j) for j in range(6)]
                elif kind == 1:
                    for j in range(6):
                        kt = 4 * qi - 1 + j
                        if 0 <= kt < 32:
                            tiles.append((kt, "mask", j))
                else:
                    tiles = [(kt, "plain", 0) for kt in range(32)]
                tiles += [(32, "plain", 0), (33, "plain", 0)]
                qblocks.append((qi * 512, 512, tiles, pat))
            if need_ctx:
                qblocks.append((SEQ, NCTX, [(32, "plain", 0), (33, "plain", 0)], -1))

            aug = (dv == 64)
            if aug:
                for i in range(2):
                    S.op("pool", lambda e, i=i: e.memset(V[i][:, :, 64:128], 1.0), writes=[Vb[i]])

            def load_group(gi):
                ks, vh, _ = groups[gi]
                i = gi % 2
                for j, kh in enumerate(ks):
                    S.dma("sp", kT[i][j][0:64, :], self.k_s[kh, 0:64, :], writes=[kTb[i][j]])
                    if kind == 2:
                        S.dma("sp", kT[i][j][64:96, :], self.kr_s[:, :], writes=[kTb[i][j]])
                if aug:
                    S.dma("sp", V[i][:, :, 0:64], self.v_s[vh], writes=[Vb[i]])
                else:
                    S.dma("sp", V[i][:, :, :], self.v_s2[vh], writes=[Vb[i]])

            load_group(0)
            for gi, (ks, vh, subs) in enumerate(groups):
                if gi + 1 < len(groups):
                    load_group(gi + 1)
                gb = gi % 2
                items = []
                for (q0, nq, tiles, pat) in qblocks:
                    for si, (qh, kvi) in enumerate(subs):
                        items.append(dict(q0=q0, nq=nq, tiles=tiles, pat=pat, si=si, qh=qh, kvi=kvi))
                work = [(ii, ti) for ii, it in enumerate(items) for ti in range(len(it["tiles"]))]
                st = dict(pat=None, bi=None, o1i=None)

                def prefetch(ii):
                    it = items[ii]
                    if kind == 0 and it["pat"] >= 0 and it["pat"] != st["pat"]:
                        st["pat"] = it["pat"]
                        st["bi"] = self.nxt("bt", 2)
                        S.dma("sp", bt[st["bi"]][:, :, :], self.na_bias[it["qh"], it["pat"]], writes=[btb[st["bi"]]])
                    it["bi"] = st["bi"]
                    it["qi"] = self.nxt("qt", 3)
                    S.dma("sp", qt[it["qi"]][0:dq, :it["nq"]], self.q_s[it["qh"], 0:dq, it["q0"]:it["q0"] + it["nq"]], writes=[qtb[it["qi"]]])

                def stageA(ii, ti):
                    it = items[ii]
                    nq = it["nq"]
                    if ti == 0:
                        if ii == 0:
                            prefetch(0)
                        if ii + 1 < len(items):
                            prefetch(ii + 1)
                        it["ni"] = 3 + self.nxt("p2n", 2)
                        it["di"] = 5 + self.nxt("p2d", 2)
                        it["pis"] = {}
                        it["pps"] = {}
                    kt, mode, ref = it["tiles"][ti]
                    qi_, bi_, kvi = it["qi"], it["bi"], it["kvi"]
                    si_ = sbanks[self.nxt("p2s", len(sbanks))]
                    sp_, spb = self.ps[si_], self.psb[si_]
                    S.op("pe", lambda e: e.matmul(
                        sp_[:, :nq], lhsT=kT[gb][kvi][:, kt * 128:(kt + 1) * 128], rhs=qt[qi_][:, :nq], start=True, stop=True),
                        reads=[kTb[gb][kvi], qtb[qi_]], writes=[spb])
                    pi = self.nxt("pt", NPT)
                    it["pis"][ti] = pi
                    if mode == "plain":
                        S.op("act", lambda e: e.activation(out=pt[pi][:, :nq], in_=sp_[:, :nq], func=AF.Exp, scale=scale),
                             reads=[spb], writes=[ptb[pi]])
                    elif mode == "mask":
                        zi = self.nxt("p0", 3)
                        S.op("act", lambda e: e.activation(out=p0[zi][:, :nq], in_=sp_[:, :nq], func=AF.Exp, scale=scale),
                             reads=[spb], writes=[p0b[zi]])
                        S.op("dve", lambda e: e.tensor_tensor(out=pt[pi][:, :nq], in0=p0[zi][:, :nq], in1=mk[:, ref, :nq], op=ALU.mult),
                             reads=[p0b[zi], mkb], writes=[ptb[pi]])
                    else:
                        zi = self.nxt("tb", 3)
                        S.op("dve", lambda e: e.scalar_tensor_tensor(
                            out=tb[zi][:, :nq], in0=sp_[:, :nq], scalar=scale, in1=bt[bi_][:, ref, :nq], op0=ALU.mult, op1=ALU.add),
                            reads=[spb, btb[bi_]], writes=[tbb[zi]])
                        S.op("act", lambda e: e.activation(out=pt[pi][:, :nq], in_=tb[zi][:, :nq], func=AF.Exp),
                             reads=[tbb[zi]], writes=[ptb[pi]])

                def pairsum(ii, ti):
                    it = items[ii]
                    nq = it["nq"]
                    if aug or ti % 2 == 0:
                        return
                    pa, pb_ = it["pis"][ti - 1], it["pis"][ti]
                    pj = self.nxt("pp", 4)
                    it["pps"][ti] = pj
                    S.op("dve", lambda e: e.tensor_tensor(out=pp[pj][:, :nq], in0=pt[pa][:, :nq], in1=pt[pb_][:, :nq], op=ALU.add),
                         reads=[ptb[pa], ptb[pb_]], writes=[ppb[pj]])

                def stageB(ii, ti):
                    it = items[ii]
                    nq, qh, si = it["nq"], it["qh"], it["si"]
                    q0 = it["q0"]
                    kt, mode, ref = it["tiles"][ti]
                    pi = it["pis"][ti]
                    ni, di = it["ni"], it["di"]
                    num, numb, den, denb = self.ps[ni], self.psb[ni], self.ps[di], self.psb[di]
                    st_, sp2 = (ti == 0), (ti == len(it["tiles"]) - 1)
                    S.op("pe", lambda e: e.matmul(num[:, :nq], lhsT=V[gb][:, kt, :], rhs=pt[pi][:, :nq], start=st_, stop=sp2),
                         reads=[Vb[gb], ptb[pi]], writes=[numb])
                    if (not aug) and ti % 2 == 1:
                        pj = it["pps"][ti]
                        S.op("pe", lambda e: e.matmul(den[:, :nq], lhsT=ones_b, rhs=pp[pj][:, :nq], start=(ti == 1), stop=sp2),
                             reads=[self.cstb_buf, ppb[pj]], writes=[denb])
                    if not sp2:
                        return None
                    return lambda: finish(it, num, numb, den, denb)

                def finish(it, num, numb, den, denb):
                    nq, qh, si, q0 = it["nq"], it["qh"], it["si"], it["q0"]
                    ri = self.nxt("rd", 2)
                    if aug:
                        if kind == 1:
                            S.op("act", lambda e: e.activation(out=rd[ri][0:64, :nq], in_=num[64:128, :nq], func=AF.Ln, bias=snk[64:128, qh:qh + 1], scale=1.0),
                                 reads=[numb, snkb], writes=[rdb[ri]])
                            S.op("act", lambda e: e.activation(out=rd[ri][0:64, :nq], in_=rd[ri][0:64, :nq], func=AF.Exp, scale=-1.0), reads=[rdb[ri]], writes=[rdb[ri]])
                        elif kind == 0:
                            S.op("act", lambda e: e.activation(out=rd[ri][0:64, :nq], in_=num[64:128, :nq], func=AF.Ln), reads=[numb], writes=[rdb[ri]])
                            S.op("act", lambda e: e.activation(out=rd[ri][0:64, :nq], in_=rd[ri][0:64, :nq], func=AF.Exp, scale=-1.0), reads=[rdb[ri]], writes=[rdb[ri]])
                        else:
                            S.op("dve", lambda e: e.reciprocal(out=rd[ri][0:64, :nq], in_=num[64:128, :nq]), reads=[numb], writes=[rdb[ri]])
                        oi = self.nxt("ot", 2)
                        S.op("dve", lambda e: e.tensor_tensor(out=ot[oi][0:64, :nq], in0=num[0:64, :nq], in1=rd[ri][0:64, :nq], op=ALU.mult),
                             reads=[numb, rdb[ri]], writes=[otb[oi]])
                        S.dma("sp", self.at_s[qh * 64:(qh + 1) * 64, q0:q0 + nq], ot[oi][0:64, :nq], reads=[otb[oi]])
                        return
                    S.op("act", lambda e: e.activation(out=rd[ri][:, :nq], in_=den[:, :nq], func=AF.Ln), reads=[denb], writes=[rdb[ri]])
                    S.op("act", lambda e: e.activation(out=rd[ri][:, :nq], in_=rd[ri][:, :nq], func=AF.Exp, scale=-1.0), reads=[rdb[ri]], writes=[rdb[ri]])
                    if si == 0:
                        o1i = self.nxt("o1", 2)
                        st["o1i"] = o1i
                        S.op("dve", lambda e: e.tensor_tensor(out=o1[o1i][:, :nq], in0=num[:, :nq], in1=rd[ri][:, :nq], op=ALU.mult),
                             reads=[numb, rdb[ri]], writes=[o1b[o1i]])
                    else:
                        o1i = st["o1i"]
                        odi = self.nxt("od", 2)
                        S.op("dve", lambda e: e.tensor_tensor(out=od[odi][:, :nq], in0=num[:, :nq], in1=rd[ri][:, :nq], op=ALU.mult),
                             reads=[numb, rdb[ri]], writes=[odb[odi]])
                        S.op("dve", lambda e: e.scalar_tensor_tensor(
                            out=od[odi][:, :nq], in0=od[odi][:, :nq], scalar=ls[:, 2:3], in1=o1[o1i][:, :nq], op0=ALU.mult, op1=ALU.add),
                            reads=[odb[odi], o1b[o1i], lsb], writes=[odb[odi]])
                        qd = self.nxt("sqd", 2)
                        S.op("act", lambda e: e.activation(out=sqd[qd][:, :nq], in_=od[odi][:, :nq], func=AF.Square),
                             reads=[odb[odi]], writes=[sqdb[qd]])
                        pm, pmb = self.ps[7], self.psb[7]
                        S.op("pe", lambda e: e.matmul(pm[:, :nq], lhsT=ones_b, rhs=sqd[qd][:, :nq], start=True, stop=True),
                             reads=[sqdb[qd], self.cstb_buf], writes=[pmb])
                        r2 = self.nxt("rd", 2)
                        S.op("act", lambda e: e.activation(out=rd[r2][:, :nq], in_=pm[:, :nq], func=AF.Ln, bias=self.eps_col[:, 0:1], scale=1.0 / 128),
                             reads=[pmb, self.eps_buf], writes=[rdb[r2]])
                        S.op("act", lambda e: e.activation(out=rd[r2][:, :nq], in_=rd[r2][:, :nq], func=AF.Exp, scale=-0.5), reads=[rdb[r2]], writes=[rdb[r2]])
                        oi = self.nxt("ot", 2)
                        S.op("dve", lambda e: e.scalar_tensor_tensor(
                            out=ot[oi][:, :nq], in0=od[odi][:, :nq], scalar=sg[:, 0:1], in1=rd[r2][:, :nq], op0=ALU.mult, op1=ALU.mult),
                            reads=[odb[odi], rdb[r2], sgb], writes=[otb[oi]])
                        S.dma("sp", self.at_s[vh * 128:(vh + 1) * 128, q0:q0 + nq], ot[oi][:, :nq], reads=[otb[oi]])

                pending = []
                for idx in range(len(work) + LOOK):
                    if idx < len(work):
                        stageA(*work[idx])
                        pairsum(*work[idx])
                    while pending and pending[0][0] <= idx:
                        pending.pop(0)[1]()
                    if idx - LOOK >= 0:
                        fin = stageB(*work[idx - LOOK])
                        if fin is not None:
                            pending.append((idx + 3, fin))
                for _, fin in pending:
                    fin()

    def phase3(self, L, x_src, x_dst, need_ctx, final):
        nc, S = self.nc, self.S
        kind = L % 4
        ident = self.cst_f[:, 3, :]
        with ExitStack() as es0:
            GT = self.sb(es0, "GT", [32, T], BF16); GTb = Buf(GT)
            with ExitStack() as es, nc.named_scope(f"L{L}_p3a"):
                Wo = self.sb(es, "wo", [128, 8, D], BF16); Wob = [Buf(Wo) for _ in range(8)]
                wsrc = self.wo[kind].rearrange("(kc p) n -> p kc n", p=128)
                for kc in range(8):
                    S.dma("pool", Wo[:, kc, :], wsrc[:, kc, :], writes=[Wob[kc]])
                Wr = self.sb(es, "wr", [128, 8, 36], F32); Wrb = Buf(Wr)
                S.dma("sp", Wr[:, :, :], self.wr[L].rearrange("(kc p) n -> p kc n", p=128), writes=[Wrb])
                brt = self.sb(es, "brt", [128, 36], F32); brb = Buf(brt)
                S.dma("sp", brt[:, :], self.br[L], writes=[brb])
                xin = [self.sb(es, f"xin{i}", [128, 8, 512], F32) for i in range(2)]; xinb = [Buf(t) for t in xin]
                at = [self.sb(es, f"at{i}", [128, 8, 512], BF16) for i in range(2)]; atb = [Buf(t) for t in at]
                h2f = self.sb(es, "h2f", [128, 8, 512], F32); h2fb = Buf(h2f)
                h2b = [self.sb(es, f"h2b{i}", [128, 8, 512], BF16) for i in range(2)]; h2bb = [Buf(t) for t in h2b]
                rs = self.sb(es, "rs", [128, 512], F32); rsb = Buf(rs)
                sq = [self.sb(es, f"sq{i}", [128, 512], BF16) for i in range(2)]; sqb = [Buf(t) for t in sq]
                tmp = [self.sb(es, f"tmp{i}", [128, 512], F32) for i in range(2)]; tmpb = [Buf(t) for t in tmp]
                rt = self.sb(es, "rt", [128, 4, 256], F32); rtb = Buf(rt)
                blocks = _token_blocks(need_ctx)
                xsrc = x_src.rearrange("(kc p) t -> p kc t", p=128)
                xdst = x_dst.rearrange("(kc p) t -> p kc t", p=128)
                asrc = self.at_s.rearrange("(kc p) t -> p kc t", p=128)
                hdst = self.h2_s.rearrange("(kc p) t -> p kc t", p=128)

                def load(bi):
                    t0, n = blocks[bi]
                    i = bi % 2
                    S.dma("sp", xin[i][:, :, :n], xsrc[:, :, t0:t0 + n], writes=[xinb[i]])
                    S.dma("sp", at[i][:, :, :n], asrc[:, :, t0:t0 + n], writes=[atb[i]])

                load(0)
                for bi, (t0, n) in enumerate(blocks):
                    if bi + 1 < len(blocks):
                        load(bi + 1)
                    i = bi % 2
                    tq = 0 if t0 < SEQ else 1
                    for oc in range(8):
                        pb = self.nxt("p3y", 2)
                        py, pyb = self.ps[pb], self.psb[pb]
                        for kc in range(8):
                            S.op("pe", lambda e, kc=kc, oc=oc, py=py: e.matmul(
                                py[:, :n], lhsT=Wo[:, kc, oc * 128:(oc + 1) * 128], rhs=at[i][:, kc, :n], start=(kc == 0), stop=(kc == 7)),
                                reads=[Wob[kc], atb[i]], writes=[pyb])
                        S.op("dve", lambda e, oc=oc, py=py: e.scalar_tensor_tensor(
                            out=xin[i][:, oc, :n], in0=py[:, :n], scalar=self.modcol(L, 2, oc, tq), in1=xin[i][:, oc, :n], op0=ALU.mult, op1=ALU.add),
                            reads=[pyb, xinb[i], self.mod_buf], writes=[xinb[i]])
                    S.dma("sp", xdst[:, :, t0:t0 + n], xin[i][:, :, :n], reads=[xinb[i]])
                    hb = h2b[i]
                    self.norm_mod(L, 1, xin[i], xinb[i], n, tq, rs, rsb, sq, sqb, tmp, tmpb,
                                  [(h2f, h2fb, "act"), (hb, h2bb[i], "pool")], 7)
                    S.dma("sp", hdst[:, :, t0:t0 + n], hb[:, :, :n], reads=[h2bb[i]])
                    nt = n // 128
                    pr, prb = self.ps[2], self.psb[2]
                    for tt in range(nt):
                        for kc in range(8):
                            S.op("pe", lambda e, kc=kc, tt=tt: e.matmul(
                                pr[:, tt * 36:(tt + 1) * 36], lhsT=h2f[:, kc, tt * 128:(tt + 1) * 128], rhs=Wr[:, kc, :], start=(kc == 0), stop=(kc == 7)),
                                reads=[h2fb, Wrb], writes=[prb])
                    self.topk(pr, prb, brt, brb, rt, rtb, nt)
                    pt_, ptb_ = self.ps[3], self.psb[3]
                    for tt in range(nt):
                        S.op("pe", lambda e, tt=tt: e.transpose(pt_[0:32, tt * 128:(tt + 1) * 128], rt[:, tt, 64:96], ident),
                             reads=[rtb, self.cst_buf], writes=[ptb_])
                    S.op("act", lambda e: e.activation(out=GT[:, t0:t0 + n], in_=pt_[0:32, 0:n], func=AF.Copy),
                         reads=[ptb_], writes=[GTb])
            S.barrier()
            with ExitStack() as es, nc.named_scope(f"L{L}_p3b"):
                sel = self.sb(es, "sel", [32, NE, 128], BF16); selb = Buf(sel)
                S.dma("pool", sel[:, :, :], self.sel_in, writes=[selb])
                SBW = 2048 + (NCTX if need_ctx else 0)
                h2 = self.sb(es, "h2", [128, 8, SBW], BF16); h2bfs = [Buf(h2) for _ in range(5)]
                acc = self.sb(es, "acc", [128, 8, SBW], F32); accb = [Buf(acc) for _ in range(5)]
                NW = 3
                wgu = [self.sb(es, f"wgu{i}", [128, 8, 512], BF16) for i in range(NW)]; wgub = [Buf(t) for t in wgu]
                wdn = [self.sb(es, f"wdn{i}", [128, 2, D], BF16) for i in range(NW)]; wdnb = [Buf(t) for t in wdn]
                x1 = self.sb(es, "x1", [128, 8, 512], F32); x1b = Buf(x1)
                sgt = [self.sb(es, f"sgt{i}", [128, 512], F32) for i in range(2)]; sgtb = [Buf(t) for t in sgt]
                tt_ = [self.sb(es, f"tt{i}", [128, 512], F32) for i in range(2)]; ttb = [Buf(t) for t in tt_]
                aT = [self.sb(es, f"aT{i}", [128, 512], BF16) for i in range(4)]; aTb = [Buf(t) for t in aT]
                sbs = [(0, 2048), (2048, SBW)]
                hsrc = self.h2_s.rearrange("(kc p) t -> p kc t", p=128)
                xdst = x_dst.rearrange("(kc p) t -> p kc t", p=128)
                outd = self.out.rearrange("(kc p) t -> p kc t", p=128)

                def loadw(e):
                    i = e % NW
                    S.dma("pool", wgu[i][:, :, :], self.w_gu[L][e].rearrange("(kc p) n -> p kc n", p=128), writes=[wgub[i]])
                    S.dma("pool", wdn[i][:, :, :], self.w_dn[L][e].rearrange("(hc p) n -> p hc n", p=128), writes=[wdnb[i]])

                for (s0, sn) in sbs:
                    nb = (sn + 511) // 512
                    for b_ in range(nb):
                        c0_ = b_ * 512
                        n_ = min(512, sn - c0_)
                        S.dma("sp", h2[:, :, c0_:c0_ + n_], hsrc[:, :, s0 + c0_:s0 + c0_ + n_], writes=[h2bfs[b_]])
                    units = [(e, b) for e in range(NE) for b in range(nb)]

                    def gu_group(e, b, gidx):
                        wi = e % NW
                        b0 = b * 512
                        n = min(512, sn - b0)
                        j = (0, 2, 1, 3)[gidx]
                        pgj, pgbj = self.ps[j], self.psb[j]
                        for kc in range(8):
                            S.op("pe", lambda e_: e_.matmul(
                                pgj[:, :n], lhsT=wgu[wi][:, kc, j * 128:(j + 1) * 128], rhs=h2[:, kc, b0:b0 + n],
                                start=(kc == 0), stop=(kc == 7)), reads=[wgub[wi], h2bfs[b]], writes=[pgbj])

                    def chain(e, b):
                        b0 = b * 512
                        n = min(512, sn - b0)
                        pg = [self.ps[j] for j in range(4)]
                        pgb = [self.psb[j] for j in range(4)]
                        pgate, pgateb = self.ps[4], self.psb[4]
                        S.op("pe", lambda e_: e_.matmul(pgate[:, :n], lhsT=sel[:, e, :], rhs=GT[:, s0 + b0:s0 + b0 + n], start=True, stop=True),
                             reads=[selb, GTb], writes=[pgateb])
                        ai = []
                        for hc in range(2):
                            s_ = self.nxt("sgt", 2)
                            S.op("act", lambda e_: e_.activation(out=sgt[s_][:, :n], in_=pg[hc][:, :n], func=AF.Silu),
                                 reads=[pgb[hc]], writes=[sgtb[s_]])
                            t_ = self.nxt("tt", 2)
                            S.op("dve", lambda e_: e_.tensor_tensor(out=tt_[t_][:, :n], in0=pg[2 + hc][:, :n], in1=sgt[s_][:, :n], op=ALU.mult),
                                 reads=[pgb[2 + hc], sgtb[s_]], writes=[ttb[t_]])
                            a_ = self.nxt("aT", 4)
                            S.op("dve", lambda e_: e_.tensor_tensor(out=aT[a_][:, :n], in0=pgate[:, :n], in1=tt_[t_][:, :n], op=ALU.mult),
                                 reads=[pgateb, ttb[t_]], writes=[aTb[a_]])
                            ai.append(a_)
                        return ai

                    def down_part(e, b, ai, ocs):
                        wi = e % NW
                        b0 = b * 512
                        n = min(512, sn - b0)
                        for oc in ocs:
                            pdi = 5 + self.nxt("p3d", 3)
                            pd, pdb = self.ps[pdi], self.psb[pdi]
                            for hc in range(2):
                                S.op("pe", lambda e_: e_.matmul(
                                    pd[:, :n], lhsT=wdn[wi][:, hc, oc * 128:(oc + 1) * 128], rhs=aT[ai[hc]][:, :n],
                                    start=(hc == 0), stop=(hc == 1)), reads=[wdnb[wi], aTb[ai[hc]]], writes=[pdb])
                            if e == 0:
                                S.op("dve", lambda e_: e_.tensor_copy(out=acc[:, oc, b0:b0 + n], in_=pd[:, :n]),
                                     reads=[pdb], writes=[accb[b]])
                            else:
                                S.op("dve", lambda e_: e_.tensor_tensor(out=acc[:, oc, b0:b0 + n], in0=pd[:, :n], in1=acc[:, oc, b0:b0 + n], op=ALU.add),
                                     reads=[pdb, accb[b]], writes=[accb[b]])

                    loadw(0)
                    loadw(1)
                    loadw(2)
                    prev = None
                    for (e, b) in units:
                        for gidx in range(4):
                            gu_group(e, b, gidx)
                            if prev is not None and gidx >= 1:
                                down_part(prev[0], prev[1], prev[2], (2 * gidx - 2, 2 * gidx - 1))
                        ai = chain(e, b)
                        if prev is not None:
                            down_part(prev[0], prev[1], prev[2], (6, 7))
                            if prev[0] != e and prev[0] + NW < NE:
                                loadw(prev[0] + NW)
                        prev = (e, b, ai)
                    down_part(prev[0], prev[1], prev[2], range(8))
                    for b in range(nb):
                        b0 = b * 512
                        n = min(512, sn - b0)
                        tq = 0 if s0 + b0 < SEQ else 1
                        S.dma("sp", x1[:, :, :n], xdst[:, :, s0 + b0:s0 + b0 + n], writes=[x1b])
                        for oc in range(8):
                            S.op("dve", lambda e_, oc=oc: e_.scalar_tensor_tensor(
                                out=x1[:, oc, :n], in0=acc[:, oc, b0:b0 + n], scalar=self.modcol(L, 5, oc, tq), in1=x1[:, oc, :n], op0=ALU.mult, op1=ALU.add),
                                reads=[accb[b], x1b, self.mod_buf], writes=[x1b])
                        if final:
                            if self.last:
                                if s0 + b0 < SEQ:
                                    S.dma("sp", outd[:, :, s0 + b0:s0 + b0 + n], x1[:, :, :n], reads=[x1b])
                            else:
                                S.dma("sp", outd[:, :, s0 + b0:s0 + b0 + n], x1[:, :, :n], reads=[x1b])
                        else:
                            S.dma("sp", xdst[:, :, s0 + b0:s0 + b0 + n], x1[:, :, :n], reads=[x1b])

    def topk(self, pr, prb, brt, brb, rt, rtb, nt):
        S = self.S
        X = mybir.AxisListType.X

        def op(fn, extra=()):
            S.op("dve", fn, reads=[rtb] + list(extra), writes=[rtb])

        def bc(ap, w):
            return ap.unsqueeze(2).to_broadcast([128, nt, w])

        Lg = rt[:, 0:nt, 0:36]
        S.op("dve", lambda e: e.tensor_tensor(out=Lg, in0=pr[:, 0:nt * 36].rearrange("p (t c) -> p t c", c=36),
                                              in1=brt[:, :].unsqueeze(1).to_broadcast([128, nt, 36]), op=ALU.add),
             reads=[prb, brb], writes=[rtb])
        lg, le = rt[:, 0:nt, 0:4], rt[:, 0:nt, 4:36]
        gmax, gsum, gw = rt[:, 0:nt, 40], rt[:, 0:nt, 42], rt[:, 0:nt, 43]
        m1, m2, dd, g1, g2 = rt[:, 0:nt, 52], rt[:, 0:nt, 53], rt[:, 0:nt, 54], rt[:, 0:nt, 55], rt[:, 0:nt, 56]
        gexp, pen = rt[:, 0:nt, 44:48], rt[:, 0:nt, 48:52]
        lem, mask1, mask2, lem2 = rt[:, 0:nt, 100:132], rt[:, 0:nt, 132:164], rt[:, 0:nt, 164:196], rt[:, 0:nt, 196:228]
        G = rt[:, 0:nt, 64:96]
        op(lambda e: e.tensor_reduce(out=gmax, in_=lg, axis=X, op=ALU.max))
        op(lambda e: e.tensor_tensor(out=gexp, in0=lg, in1=bc(gmax, 4), op=ALU.subtract))
        S.op("act", lambda e: e.activation(out=gexp, in_=gexp, func=AF.Exp), reads=[rtb], writes=[rtb])
        op(lambda e: e.tensor_reduce(out=gsum, in_=gexp, axis=X, op=ALU.add))
        op(lambda e: e.reciprocal(out=gw, in_=gsum))
        op(lambda e: e.tensor_tensor(out=pen, in0=lg, in1=bc(gmax, 4), op=ALU.is_equal))
        op(lambda e: e.tensor_scalar(out=pen, in0=pen, scalar1=-1.0, scalar2=30000.0, op0=ALU.add, op1=ALU.mult))
        op(lambda e: e.tensor_tensor(out=lem.rearrange("p t (g x) -> p t g x", g=4), in0=le.rearrange("p t (g x) -> p t g x", g=4),
                                     in1=pen.unsqueeze(3).to_broadcast([128, nt, 4, 8]), op=ALU.add))
        op(lambda e: e.tensor_reduce(out=m1, in_=lem, axis=X, op=ALU.max))
        op(lambda e: e.tensor_tensor(out=mask1, in0=lem, in1=bc(m1, 32), op=ALU.is_equal))
        op(lambda e: e.scalar_tensor_tensor(out=lem2, in0=mask1, scalar=-30000.0, in1=lem, op0=ALU.mult, op1=ALU.add))
        op(lambda e: e.tensor_reduce(out=m2, in_=lem2, axis=X, op=ALU.max))
        op(lambda e: e.tensor_tensor(out=mask2, in0=lem2, in1=bc(m2, 32), op=ALU.is_equal))
        op(lambda e: e.tensor_tensor(out=dd, in0=m2, in1=m1, op=ALU.subtract))
        S.op("act", lambda e: e.activation(out=dd, in_=dd, func=AF.Exp), reads=[rtb], writes=[rtb])
        op(lambda e: e.tensor_scalar(out=dd, in0=dd, scalar1=1.0, scalar2=None, op0=ALU.add))
        op(lambda e: e.reciprocal(out=dd, in_=dd))
        op(lambda e: e.tensor_tensor(out=g1, in0=dd, in1=gw, op=ALU.mult))
        op(lambda e: e.tensor_tensor(out=g2, in0=gw, in1=g1, op=ALU.subtract))
        op(lambda e: e.tensor_tensor(out=G, in0=mask1, in1=bc(g1, 32), op=ALU.mult))
        op(lambda e: e.tensor_tensor(out=mask2, in0=mask2, in1=bc(g2, 32), op=ALU.mult))
        op(lambda e: e.tensor_tensor(out=G, in0=G, in1=mask2, op=ALU.add))


def _rope_tables():
    t = np.arange(SEQ)
    rows = (t // 64).astype(np.float32)
    cols = (t % 64).astype(np.float32)

    def ang(rot_dim):
        nf = rot_dim // 4
        inv = (np.float32(10000.0) ** (-np.arange(nf, dtype=np.float32) / np.float32(nf))).astype(np.float32)
        return np.concatenate([rows[:, None] * inv, cols[:, None] * inv], axis=-1).astype(np.float32)

    a64 = ang(64)
    r64 = np.zeros((2, 128, T), np.float32)
    r64[0, :, SEQ:] = 1.0
    idx = (np.arange(128) % 64) % 32
    r64[0, :, :SEQ] = np.cos(a64).T[idx]
    r64[1, :, :SEQ] = np.sin(a64).T[idx]
    a32 = ang(32)
    rm = np.zeros((2, 96, T), np.float32)
    rm[0] = 1.0
    idx = np.arange(32) % 16
    rm[0, 64:96, :SEQ] = np.cos(a32).T[idx]
    rm[1, 64:96, :SEQ] = np.sin(a32).T[idx]
    return r64, rm


def _consts():
    c = np.zeros((128, 8, 128), np.float32)
    c[:, 0, :] = 1.0
    for b in (0, 64):
        c[b:b + 64, 1, b:b + 64] = 1.0 / 64
        for m in range(64):
            if m < 32:
                c[b + m + 32, 2, b + m] = -1.0
            else:
                c[b + m - 32, 2, b + m] = 1.0
    c[:, 3, :] = np.eye(128, dtype=np.float32)
    c[:, 4, :] = 1.0 / 128
    c[0:64, 5, 0:64] = 1.0 / 64
    c[64:96, 5, 64:96] = 1.0 / 32
    for m in range(32):
        if m < 16:
            c[64 + m + 16, 6, 64 + m] = -1.0
            c[m + 16, 7, m] = -1.0
        else:
            c[64 + m - 16, 6, 64 + m] = 1.0
            c[m - 16, 7, m] = 1.0
    return c


def _na_bias(rpb):
    out = np.full((16, 3, 128, 6, 512), NEG, np.float32)
    rl = np.arange(8)[:, None].repeat(64, 1).reshape(-1)
    c = np.arange(64)[None, :].repeat(8, 0).reshape(-1)
    krl = np.arange(2)[:, None].repeat(64, 1).reshape(-1)
    kc = np.arange(64)[None, :].repeat(2, 0).reshape(-1)
    for pi, r0 in enumerate((0, 8, 56)):
        kr0 = min(max(r0 - 4, 0), 52)
        r = r0 + rl
        row0 = np.clip(r - 4, 0, 56)
        col0 = np.clip(c - 8, 0, 48)
        for j in range(6):
            kr = kr0 + 2 * j + krl
            vr = (kr[:, None] >= row0[None, :]) & (kr[:, None] < row0[None, :] + 8)
            vc = (kc[:, None] >= col0[None, :]) & (kc[:, None] < col0[None, :] + 16)
            valid = vr & vc
            idx = (kr[:, None] - r[None, :] + 7) * 31 + (kc[:, None] - c[None, :] + 15)
            idx = np.where(valid, idx, 0)
            g = rpb[:, idx]
            out[:, pi, :, j, :] = np.where(valid[None], g, np.float32(NEG))
    return out


def _sw_mask():
    kl = np.arange(128)[:, None, None]
    j = np.arange(6)[None, :, None]
    ql = np.arange(512)[None, None, :]
    return (np.abs(ql - (kl + 128 * (j - 1))) <= 128).astype(np.float32)


def _col(v):
    return np.ascontiguousarray(v.reshape(-1, 128).T)


def _prep_shared(inp, layers):
    f = lambda a: np.ascontiguousarray(np.asarray(a, dtype=np.float32))
    kinds = set(l % 4 for l in layers)
    d = {}
    for l in layers:
        d[f"ada_w{l}"] = f(inp["ada_w"])[l]
        d[f"w_gu{l}"] = f(inp["moe_w_gate_up"])[l]
        d[f"w_dn{l}"] = f(inp["moe_w_down"])[l]
    ab = np.stack([_col(f(inp["ada_b"])[l]) for l in range(DEPTH)])
    d["ada_b"] = np.ascontiguousarray(np.repeat(ab[..., None], 2, axis=-1))
    ng = np.stack([np.stack([_col(f(inp["norm_g"])[l, i]) for i in range(2)]) for l in range(DEPTH)])
    d["norm_g"] = np.ascontiguousarray(np.repeat(ng[..., None], 2, axis=-1))
    d["wr"] = np.ascontiguousarray(np.concatenate([f(inp["moe_w_router_group"]), f(inp["moe_w_router_expert"])], axis=-1))
    br = np.concatenate([f(inp["moe_b_router_group"]), f(inp["moe_b_router_expert"])], axis=-1)
    d["br"] = np.ascontiguousarray(np.broadcast_to(br[:, None, :], (DEPTH, 128, 36)))
    d["consts"] = _consts()
    sel = np.zeros((32, NE, 128), np.float32)
    for e in range(NE):
        sel[e, e, :] = 1.0
    d["sel"] = sel
    r64, rm = _rope_tables()
    if 0 in kinds:
        d["na_w_qkv"] = f(inp["na_w_qkv"])[0]
        d["na_w_o"] = f(inp["na_w_o"])[0]
        d["na_qk_g"] = np.ascontiguousarray(np.stack([np.tile(f(inp["na_q_norm"])[0], 2), np.tile(f(inp["na_k_norm"])[0], 2)], axis=1))
        d["na_bias"] = _na_bias(f(inp["na_rpb"])[0])
    if 1 in kinds:
        d["sw_w_qkv"] = f(inp["sw_w_qkv"])[0]
        d["sw_w_o"] = f(inp["sw_w_o"])[0]
        d["sw_qk_g"] = np.ascontiguousarray(np.stack([np.tile(f(inp["sw_q_norm"])[0], 2), np.tile(f(inp["sw_k_norm"])[0], 2)], axis=1))
        d["sw_sink"] = np.ascontiguousarray(np.broadcast_to(f(inp["sw_sink"])[0][None, :], (128, 16)))
        d["sw_mask"] = _sw_mask()
    if 1 in kinds or 3 in kinds:
        d["rope64"] = r64
    if 2 in kinds:
        d["mla_w_dqkv"] = f(inp["mla_w_dqkv"])[0]
        d["mla_w_uq"] = f(inp["mla_w_uq"])[0]
        d["mla_w_ukv"] = f(inp["mla_w_ukv"])[0]
        d["mla_w_o"] = f(inp["mla_w_o"])[0]
        mg = np.zeros((128, 8), np.float32)
        mg[:, 0:3] = _col(f(inp["mla_q_a_norm"])[0])
        mg[:, 3:5] = _col(f(inp["mla_kv_a_norm"])[0])
        mg[:96, 5] = f(inp["mla_q_norm"])[0]
        mg[:, 6] = np.tile(f(inp["mla_k_norm"])[0][:64], 2)
        mg[:32, 7] = f(inp["mla_k_norm"])[0][64:96]
        d["mla_g"] = mg
        d["rope_mla"] = rm
    if 3 in kinds:
        d["diff_w_qkv"] = f(inp["diff_w_qkv"])[0]
        d["diff_w_o"] = f(inp["diff_w_o"])[0]
        d["diff_qk_g"] = np.ascontiguousarray(np.stack([np.tile(f(inp["diff_q_norm"])[0], 2), np.tile(f(inp["diff_k_norm"])[0], 2)], axis=1))
        d["diff_lam"] = np.ascontiguousarray(np.broadcast_to(f(inp["diff_lambda"])[0][None], (128, 4, 64)))
        d["diff_subln"] = np.ascontiguousarray(f(inp["diff_subln"])[0][:, None])
    return d


def _cc(inp, b):
    c = np.asarray(inp["c"], np.float32)[b]
    cx = np.asarray(inp["c_ctx"], np.float32)
    return np.ascontiguousarray(np.stack([_col(c), _col(cx)], axis=-1))


_CACHE = {}


def _get_builder(layers, last):
    key = (tuple(layers), last)
    if key not in _CACHE:
        _CACHE[key] = Builder(list(layers), layers[0] == 0, last)
    return _CACHE[key]


def run_layers(inp, layers, xT_list, cores):
    last = layers[-1] == DEPTH - 1
    B = _get_builder(layers, last)
    shared = _prep_shared(inp, layers)
    in_maps = []
    for ci, b in enumerate(cores):
        m = {"xT_in": np.ascontiguousarray(xT_list[ci]), "cc": _cc(inp, b)}
        for k in B.in_names:
            if k not in m:
                m[k] = shared[k]
        in_maps.append(m)
    res = run_bass_kernel_spmd(B.nc, in_maps, core_ids=list(range(len(cores))))
    name = "outT" if last else "xT_out"
    return [np.asarray(r[name]) for r in res.results]


FUSED = True


def kernel(**inp):
    x = np.asarray(inp["x"], np.float32)
    ctx = np.asarray(inp["ctx"], np.float32)
    nb = x.shape[0]
    xT = [np.ascontiguousarray(np.concatenate([x[b].T, ctx[b].T], axis=1)) for b in range(nb)]
    cores = list(range(nb))
    if FUSED:
        outs = run_layers(inp, [0, 1, 2, 3], xT, cores)
    else:
        cur = xT
        for L in range(DEPTH):
            cur = run_layers(inp, [L], cur, cores)
        outs = cur
    return np.ascontiguousarray(np.stack([o.T for o in outs], axis=0)).astype(np.float32)
```
